# Optimizing a Trainium2 kernel written in Bass

```python
import math
import jax, jax.numpy as jnp
from jax import lax
import numpy as np

D_MODEL = 2048
BATCH = 2
SEQ = 16384
DEPTH = 2
DEC_BATCH = 8
DEC_SEQ = 2048
PAST_LEN = 128

EPS = 1e-6
D_MIX = D_MODEL
MLA_HEADS = 8
MLA_DN = 128
MLA_DR = 64
MLA_DV = 128
Q_LORA = 512
KV_LORA = 512
ROPE_THETA = 10000.0
MLA_SCALE = 1.0 / math.sqrt(MLA_DN + MLA_DR)
Q_BLOCK = 128
GQA_HEADS = 8
GQA_KV_HEADS = 2
GQA_GROUP = GQA_HEADS // GQA_KV_HEADS
GQA_DH = 128
WINDOW = 128
W_BLOCK = 128
GQA_SCALE = 1.0 / math.sqrt(GQA_DH)
NUM_BUCKETS = 32
MAX_DISTANCE = 128
P_QLAT = Q_LORA
P_KVLAT = KV_LORA
P_KROPE = MLA_DR
P_GQ = GQA_HEADS * GQA_DH
P_GK = GQA_KV_HEADS * GQA_DH
P_GV = GQA_KV_HEADS * GQA_DH
P_IN = P_QLAT + P_KVLAT + P_KROPE + P_GQ + P_GK + P_GV
OUT_A = MLA_HEADS * MLA_DV
OUT_B = GQA_HEADS * GQA_DH
D_FF = 5632
N_EXPERTS = 8
TOP_K = 2
N_DENSE = (DEPTH + 1) // 2
N_MOE = DEPTH // 2
NEG_BIG = -1e30

kernel_name = "hybrid_mla_swa_encoder_adaln"


def rmsnorm(x, g):
    xf = x.astype(jnp.float32)
    y = xf * lax.rsqrt(jnp.mean(xf * xf, axis=-1, keepdims=True) + EPS)
    return (y * g.astype(jnp.float32)).astype(x.dtype)


def rope_tables(S, dtype):
    pos = jnp.arange(S, dtype=jnp.float32)
    inv = 1.0 / (ROPE_THETA ** (jnp.arange(0, MLA_DR, 2, dtype=jnp.float32) / MLA_DR))
    ang = pos[:, None] * inv[None, :]
    return jnp.cos(ang).astype(dtype), jnp.sin(ang).astype(dtype)


def apply_rope(x, cos, sin):
    x1, x2 = jnp.split(x, 2, axis=-1)
    return jnp.concatenate([x1 * cos - x2 * sin, x1 * sin + x2 * cos], axis=-1)


def t5_bucket(rel):
    nb = NUM_BUCKETS // 2
    ret = (rel > 0).astype(jnp.int32) * nb
    n = jnp.abs(rel)
    max_exact = nb // 2
    nf = jnp.maximum(n, 1).astype(jnp.float32)
    large = max_exact + (jnp.log(nf / max_exact) / math.log(MAX_DISTANCE / max_exact)
                         * (nb - max_exact)).astype(jnp.int32)
    large = jnp.minimum(large, nb - 1)
    return ret + jnp.where(n < max_exact, n, large)


def mla_attention(q_nope, q_rope, k_nope, k_rope, v):
    B, S, H, _ = q_nope.shape
    nb = S // Q_BLOCK

    def block(args):
        qn, qr = args
        s = (jnp.einsum('bqhd,bkhd->bhqk', qn, k_nope)
             + jnp.einsum('bqhr,bkr->bhqk', qr, k_rope))
        p = jax.nn.softmax(s.astype(jnp.float32) * MLA_SCALE, axis=-1).astype(v.dtype)
        return jnp.einsum('bhqk,bkhd->bqhd', p, v)

    qn_b = q_nope.reshape(B, nb, Q_BLOCK, H, MLA_DN).swapaxes(0, 1)
    qr_b = q_rope.reshape(B, nb, Q_BLOCK, H, MLA_DR).swapaxes(0, 1)
    o = lax.map(block, (qn_b, qr_b))
    return o.swapaxes(0, 1).reshape(B, S, H * MLA_DV)


def window_attention(q, k, v, sink, rel_bias):
    B, S = q.shape[0], q.shape[1]
    nb = S // W_BLOCK
    qb = q.reshape(B, nb, W_BLOCK, GQA_KV_HEADS, GQA_GROUP, GQA_DH)

    def band(t):
        tp = jnp.pad(t, ((0, 0), (W_BLOCK, W_BLOCK), (0, 0), (0, 0)))
        tp = tp.reshape(B, nb + 2, W_BLOCK, GQA_KV_HEADS, GQA_DH)
        return jnp.concatenate([tp[:, :-2], tp[:, 1:-1], tp[:, 2:]], axis=2)

    kb, vb = band(k), band(v)
    s = jnp.einsum('bnqkgd,bnjkd->bnkgqj', qb, kb).astype(jnp.float32) * GQA_SCALE
    qpos = jnp.arange(W_BLOCK, dtype=jnp.int32)
    jpos = jnp.arange(3 * W_BLOCK, dtype=jnp.int32)
    rel = jpos[None, :] - W_BLOCK - qpos[:, None]
    bias = rel_bias[t5_bucket(rel)].astype(jnp.float32)
    bias = bias.transpose(2, 0, 1).reshape(GQA_KV_HEADS, GQA_GROUP, W_BLOCK, 3 * W_BLOCK)
    key_abs = jnp.arange(nb, dtype=jnp.int32)[:, None] * W_BLOCK - W_BLOCK + jpos[None, :]
    valid = (jnp.abs(rel) <= WINDOW)[None] & ((key_abs >= 0) & (key_abs < S))[:, None, :]
    s = jnp.where(valid[None, :, None, None], s + bias[None, None], NEG_BIG)
    sk = sink.astype(jnp.float32).reshape(GQA_KV_HEADS, GQA_GROUP)[None, None, :, :, None, None]
    m = jnp.maximum(jnp.max(s, axis=-1, keepdims=True), sk)
    e = jnp.exp(s - m)
    p = e / (jnp.sum(e, axis=-1, keepdims=True) + jnp.exp(sk - m))
    o = jnp.einsum('bnkgqj,bnjkd->bnqkgd', p.astype(v.dtype), vb)
    return o.reshape(B, S, GQA_HEADS * GQA_DH)


def token_mixer(h, cos, sin, rel_bias, w_in, g_q_lat, w_uq, g_kv_lat, w_ukv, sink, g_out_a, g_out_b, w_out):
    B, S, _ = h.shape
    z = h @ w_in
    offs = np.cumsum([P_QLAT, P_KVLAT, P_KROPE, P_GQ, P_GK]).tolist()
    cq, ckv, kr, gq, gk, gv = jnp.split(z, offs, axis=-1)
    q = (rmsnorm(cq, g_q_lat) @ w_uq).reshape(B, S, MLA_HEADS, MLA_DN + MLA_DR)
    q_nope, q_rope = q[..., :MLA_DN], q[..., MLA_DN:]
    kv = (rmsnorm(ckv, g_kv_lat) @ w_ukv).reshape(B, S, MLA_HEADS, MLA_DN + MLA_DV)
    k_nope, v_a = kv[..., :MLA_DN], kv[..., MLA_DN:]
    q_rope = apply_rope(q_rope, cos[None, :, None, :], sin[None, :, None, :])
    k_rope = apply_rope(kr, cos[None], sin[None])
    o_a = mla_attention(q_nope, q_rope, k_nope, k_rope, v_a)
    o_b = window_attention(gq.reshape(B, S, GQA_HEADS, GQA_DH),
                           gk.reshape(B, S, GQA_KV_HEADS, GQA_DH),
                           gv.reshape(B, S, GQA_KV_HEADS, GQA_DH), sink, rel_bias)
    o = jnp.concatenate([rmsnorm(o_a, g_out_a), rmsnorm(o_b, g_out_b)], axis=-1)
    return o @ w_out


def swiglu(h, wg, wu, wd):
    return (jax.nn.silu(h @ wg) * (h @ wu)) @ wd


def moe_swiglu(h, wr, wg, wu, wd):
    B, S, D = h.shape
    t = h.reshape(B * S, D)
    logits = (t @ wr).astype(jnp.float32)
    top_v, top_i = lax.top_k(logits, TOP_K)
    w = jax.nn.softmax(top_v, axis=-1)
    gates = jnp.sum(jax.nn.one_hot(top_i, N_EXPERTS, dtype=jnp.float32) * w[..., None], axis=1).astype(h.dtype)
    y = jnp.zeros_like(t)
    for e in range(N_EXPERTS):
        y = y + gates[:, e:e + 1] * swiglu(t, wg[e], wu[e], wd[e])
    return y.reshape(B, S, D)


def trunk(x, c, rel_bias, w_ada, b_ada, g_norm_mix, g_norm_ffn, w_in, g_q_lat, w_uq, g_kv_lat, w_ukv,
          sink, g_out_a, g_out_b, w_out, w_gate_d, w_up_d, w_down_d, w_router, w_gate_e, w_up_e,
          w_down_e, g_final):
    S = x.shape[1]
    cos, sin = rope_tables(S, x.dtype)
    cs = jax.nn.silu(c)
    for l in range(DEPTH):
        mod = cs @ w_ada[l] + b_ada[l]
        sh1, sc1, g1, sh2, sc2, g2 = [m[:, None, :] for m in jnp.split(mod, 6, axis=-1)]
        h = rmsnorm(x, g_norm_mix[l]) * (1 + sc1) + sh1
        x = x + g1 * token_mixer(h, cos, sin, rel_bias, w_in[l], g_q_lat[l], w_uq[l], g_kv_lat[l],
                                 w_ukv[l], sink[l], g_out_a[l], g_out_b[l], w_out[l])
        h = rmsnorm(x, g_norm_ffn[l]) * (1 + sc2) + sh2
        i = l // 2
        if l % 2 == 0:
            f = swiglu(h, w_gate_d[i], w_up_d[i], w_down_d[i])
        else:
            f = moe_swiglu(h, w_router[i], w_gate_e[i], w_up_e[i], w_down_e[i])
        x = x + g2 * f
    return rmsnorm(x, g_final)


def setup_inputs(seed: int = 0) -> dict:
    key = jax.random.key(seed)
    ks = jax.random.split(key, 26)
    f32 = jnp.float32

    def nrm(k, shape, scale):
        return scale * jax.random.normal(k, shape, f32)

    def gain(k, shape):
        return 1.0 + nrm(k, shape, 0.05)

    return {
        "x_prompt": nrm(ks[0], (BATCH, SEQ, D_MODEL), 1.0),
        "x_sample": nrm(ks[1], (DEC_BATCH, DEC_SEQ, D_MODEL), 1.0),
        "c_prompt": nrm(ks[2], (BATCH, D_MODEL), 1.0),
        "c_sample": nrm(ks[3], (DEC_BATCH, D_MODEL), 1.0),
        "rel_bias": nrm(ks[4], (NUM_BUCKETS, GQA_HEADS), 0.5),
        "w_ada": nrm(ks[5], (DEPTH, D_MODEL, 6 * D_MODEL), 0.5 * D_MODEL ** -0.5),
        "b_ada": nrm(ks[6], (DEPTH, 6 * D_MODEL), 0.02),
        "g_norm_mix": gain(ks[7], (DEPTH, D_MODEL)),
        "g_norm_ffn": gain(ks[8], (DEPTH, D_MODEL)),
        "w_in": nrm(ks[9], (DEPTH, D_MODEL, P_IN), D_MODEL ** -0.5),
        "g_q_lat": gain(ks[10], (DEPTH, Q_LORA)),
        "w_uq": nrm(ks[11], (DEPTH, Q_LORA, MLA_HEADS * (MLA_DN + MLA_DR)), Q_LORA ** -0.5),
        "g_kv_lat": gain(ks[12], (DEPTH, KV_LORA)),
        "w_ukv": nrm(ks[13], (DEPTH, KV_LORA, MLA_HEADS * (MLA_DN + MLA_DV)), KV_LORA ** -0.5),
        "sink": nrm(ks[14], (DEPTH, GQA_HEADS), 0.5),
        "g_out_a": gain(ks[15], (DEPTH, OUT_A)),
        "g_out_b": gain(ks[16], (DEPTH, OUT_B)),
        "w_out": nrm(ks[17], (DEPTH, D_MIX, D_MODEL), D_MIX ** -0.5),
        "w_gate_d": nrm(ks[18], (N_DENSE, D_MODEL, D_FF), D_MODEL ** -0.5),
        "w_up_d": nrm(ks[19], (N_DENSE, D_MODEL, D_FF), D_MODEL ** -0.5),
        "w_down_d": nrm(ks[20], (N_DENSE, D_FF, D_MODEL), D_FF ** -0.5),
        "w_router": nrm(ks[21], (N_MOE, D_MODEL, N_EXPERTS), D_MODEL ** -0.5),
        "w_gate_e": nrm(ks[22], (N_MOE, N_EXPERTS, D_MODEL, D_FF), D_MODEL ** -0.5),
        "w_up_e": nrm(ks[23], (N_MOE, N_EXPERTS, D_MODEL, D_FF), D_MODEL ** -0.5),
        "w_down_e": nrm(ks[24], (N_MOE, N_EXPERTS, D_FF, D_MODEL), D_FF ** -0.5),
        "g_final": gain(ks[25], (D_MODEL,)),
    }


def reference(x_prompt, x_sample, c_prompt, c_sample, rel_bias, w_ada, b_ada, g_norm_mix, g_norm_ffn,
              w_in, g_q_lat, w_uq, g_kv_lat, w_ukv, sink, g_out_a, g_out_b, w_out, w_gate_d, w_up_d,
              w_down_d, w_router, w_gate_e, w_up_e, w_down_e, g_final):
    y_prompt = trunk(x_prompt, c_prompt, rel_bias, w_ada, b_ada, g_norm_mix, g_norm_ffn, w_in, g_q_lat,
                     w_uq, g_kv_lat, w_ukv, sink, g_out_a, g_out_b, w_out, w_gate_d, w_up_d, w_down_d,
                     w_router, w_gate_e, w_up_e, w_down_e, g_final)
    y_sample = trunk(x_sample, c_sample, rel_bias, w_ada, b_ada, g_norm_mix, g_norm_ffn, w_in, g_q_lat,
                     w_uq, g_kv_lat, w_ukv, sink, g_out_a, g_out_b, w_out, w_gate_d, w_up_d, w_down_d,
                     w_router, w_gate_e, w_up_e, w_down_e, g_final)
    return (y_prompt, y_sample)
```

```python
import math
from contextlib import ExitStack
import numpy as np
import concourse.bass as bass
import concourse.mybir as mybir
from concourse.bass_utils import run_bass_kernel_spmd

F32 = mybir.dt.float32
BF16 = mybir.dt.bfloat16
AF = mybir.ActivationFunctionType
ALU = mybir.AluOpType
AX = mybir.AxisListType

D = 2048
KC = 16
EPS = 1e-6
H = 8
MLA_SCALE = 1.0 / math.sqrt(192.0)
GQA_SCALE = 1.0 / math.sqrt(128.0)
NEG = -1.0e4
T = 512


class Buf:
    def __init__(self, name, t=None):
        self.name = name
        self.t = t
        self.w = {}
        self.r = {}
        self.dsem = None
        self.dsemname = None
        self.dcnt = 0
        self.scoped = False


class Eng:
    def __init__(self, name, obj, sem, semname):
        self.name, self.obj, self.sem, self.semname = name, obj, sem, semname
        self.cnt = 0
        self.seen = {}


class Prog:
    def __init__(self, nc, es):
        self.nc, self.es = nc, es
        self.sems = {}
        self.E = {}
        for name, obj in (("pe", nc.tensor), ("act", nc.scalar), ("dve", nc.vector), ("pool", nc.gpsimd), ("sp", nc.sync)):
            sn = "c_" + name
            self.E[name] = Eng(name, obj, self.newsem(sn), sn)
        self.dbufs = []
        self.free_dsems = []
        self.nuid = 0

    def newsem(self, name):
        s = self.es.enter_context(self.nc.semaphore(name))
        self.sems[name] = s
        return s

    def tile(self, name, shape, dt, scope=None):
        self.nuid += 1
        t = (scope or self.es).enter_context(self.nc.sbuf_tensor(f"{name}_{self.nuid}", list(shape), dt))
        b = Buf(name, t)
        b.scoped = scope is not None
        return b

    def _wait(self, E, deps, defer_last=False):
        need = []
        for sn, val in deps.items():
            if E.seen.get(sn, 0) >= val:
                continue
            if sn == E.semname and E.name == "pe":
                continue
            need.append((sn, val))
            E.seen[sn] = val
        last = None
        if defer_last and need:
            last = need.pop()
        for sn, val in need:
            E.obj.wait_ge(self.sems[sn], val)
        return last

    @staticmethod
    def _merge(d, s, skip=None):
        for k, v in s.items():
            if k != skip and d.get(k, 0) < v:
                d[k] = v

    def op(self, e, fn, reads=(), writes=()):
        E = self.E[e]
        deps = {}
        for b in reads:
            self._merge(deps, b.w)
        for b in writes:
            self._merge(deps, b.w)
            self._merge(deps, b.r)
        last = self._wait(E, deps, defer_last=True)
        ins = fn(E.obj)
        if last is not None:
            ins._wait_ge(self.sems[last[0]], last[1])
        E.cnt += 1
        ins.then_inc(E.sem, 1)
        for b in writes:
            b.w[E.semname] = E.cnt
        for b in reads:
            b.r[E.semname] = E.cnt
        return ins

    def dma(self, q, out, in_, reads, writes, **kw):
        E = self.E[q]
        prim = writes[0]
        if prim.dsem is None:
            if self.free_dsems:
                prim.dsemname, prim.dsem, prim.dcnt = self.free_dsems.pop()
            else:
                self.nuid += 1
                prim.dsemname = f"d_{self.nuid}"
                prim.dsem = self.newsem(prim.dsemname)
            self.dbufs.append(prim)
        deps = {}
        for b in reads:
            self._merge(deps, b.w)
        for b in writes:
            self._merge(deps, b.r)
            self._merge(deps, b.w, skip=b.dsemname)
        last = self._wait(E, deps, defer_last=True)
        ins = E.obj.dma_start(out=out, in_=in_, **kw)
        if last is not None:
            ins._wait_ge(self.sems[last[0]], last[1])
        prim.dcnt += 16
        ins.then_inc(prim.dsem, 16)
        for b in writes:
            b.w[prim.dsemname] = prim.dcnt
        for b in reads:
            b.r[prim.dsemname] = prim.dcnt

    def barrier(self):
        deps = {}
        for E in self.E.values():
            if E.cnt:
                deps[E.semname] = E.cnt
        for b in self.dbufs:
            if b.dcnt:
                deps[b.dsemname] = b.dcnt
        for E in self.E.values():
            d = dict(deps)
            self._wait_all(E, d)

    def end_phase(self):
        self.barrier()
        keep = []
        for b in self.dbufs:
            if getattr(b, "scoped", False):
                self.free_dsems.append((b.dsemname, b.dsem, b.dcnt))
            else:
                keep.append(b)
        self.dbufs = keep

    def _wait_all(self, E, deps):
        for sn, val in deps.items():
            if E.seen.get(sn, 0) >= val:
                continue
            E.obj.wait_ge(self.sems[sn], val)
            E.seen[sn] = val


class Rot:
    def __init__(self, bufs):
        self.bufs, self.i = bufs, 0

    def next(self):
        b = self.bufs[self.i % len(self.bufs)]
        self.i += 1
        return b


def build(cfg):
    S_P, S_S, OWN, FF, NE, DEPTH = cfg["S_P"], cfg["S_S"], cfg["OWN"], cfg["FF"], cfg["NE"], cfg["DEPTH"]
    FC = FF // 128
    NTOK = S_P + S_S
    NBLK = NTOK // T
    segs = (("P", 0, S_P), ("S", S_P, S_S))
    nc = bass.Bass("TRN2", target_bir_lowering=False)

    def din(name, shape, dt=F32):
        return nc.dram_tensor(name, list(shape), dt, kind="ExternalInput").ap()

    def dscr(name, shape, dt):
        return nc.dram_tensor(name, list(shape), dt, kind="Internal").ap()

    x_in = din("x_in", [NTOK, D])
    c_in = din("c_in", [2, D])
    rope_in = din("rope_in", [64, 2, NTOK])
    em_in = din("em_in", [128, 2 * (NTOK // 128)])
    ident_in = din("ident_in", [128, 128])
    oh_in = din("oh_in", [33, 512])
    rel_bias = din("rel_bias", [32, 8])
    w_ada = din("w_ada", [DEPTH, D, 6 * D])
    b_ada = din("b_ada", [DEPTH, 6 * D])
    g_norm_mix = din("g_norm_mix", [DEPTH, D])
    g_norm_ffn = din("g_norm_ffn", [DEPTH, D])
    w_in = din("w_in", [DEPTH, D, 2624])
    w_krs = din("w_krs", [DEPTH, D, 64])
    g_q_lat = din("g_q_lat", [DEPTH, 512])
    w_uq = din("w_uq", [DEPTH, 512, 1536])
    w_uqs = din("w_uqs", [DEPTH, 512, 512])
    g_kv_lat = din("g_kv_lat", [DEPTH, 512])
    w_ukv = din("w_ukv", [DEPTH, 512, 2048])
    sink = din("sink", [DEPTH, 8])
    g_out_a = din("g_out_a", [DEPTH, 1024])
    g_out_b = din("g_out_b", [DEPTH, 1024])
    w_out = din("w_out", [DEPTH, D, D])
    w_gate_d = din("w_gate_d", [1, D, FF])
    w_up_d = din("w_up_d", [1, D, FF])
    w_down_d = din("w_down_d", [1, FF, D])
    w_router = din("w_router", [1, D, NE])
    w_gate_e = din("w_gate_e", [1, NE, D, FF])
    w_up_e = din("w_up_e", [1, NE, D, FF])
    w_down_e = din("w_down_e", [1, NE, FF, D])
    g_final = din("g_final", [D])
    NOUT = OWN + S_S
    y_out = nc.dram_tensor("y_out", [NOUT, D], F32, kind="ExternalOutput").ap()

    XT = dscr("XT", [NBLK, 128, KC, T], F32)
    QT = dscr("QT", [H, 192, NTOK], BF16)
    LAT = dscr("LAT", [576, NTOK], BF16)
    GQ = dscr("GQ", [H, 128, NTOK], BF16)
    GK = dscr("GK", [2, 128, NTOK], BF16)
    GV = dscr("GV", [NTOK, 256], BF16)
    OA = dscr("OA", [H, 128, NTOK], BF16)
    OB = dscr("OB", [H, 128, NTOK], BF16)
    TB = dscr("TB", [8, 128, 512], F32)
    NEX = 1 + NE
    WGS = dscr("WGS", [NEX, FC // 2, 128, KC * 256], BF16)
    WUS = dscr("WUS", [NEX, FC // 2, 128, KC * 256], BF16)
    WDS = dscr("WDS", [NEX, KC, 128, FC * 128], BF16)

    es = ExitStack()
    with es:
        P = Prog(nc, es)
        XTb = [Buf(f"XT{i % 6}") for i in range(6)] * (NBLK // 6 + 1)
        QTb, LATb, GQb, GKb, GVb, OAb, OBb, TBb, Yb = (Buf(n) for n in ("QT", "LAT", "GQ", "GK", "GV", "OA", "OB", "TB", "Y"))
        CONST = Buf("const")
        WSd, WSe = Buf("WSd"), Buf("WSe")
        conv = []
        for ex in range(NEX):
            Wg_ = w_gate_d[0] if ex == 0 else w_gate_e[0][ex - 1]
            Wu_ = w_up_d[0] if ex == 0 else w_up_e[0][ex - 1]
            Wd_ = w_down_d[0] if ex == 0 else w_down_e[0][ex - 1]
            wb = WSd if ex == 0 else WSe
            for fg in range(FC // 2):
                conv.append((WGS[ex, fg].rearrange("p (k n) -> p k n", n=256), Wg_[:, fg * 256:(fg + 1) * 256].rearrange("(k p) n -> p k n", p=128), wb))
                conv.append((WUS[ex, fg].rearrange("p (k n) -> p k n", n=256), Wu_[:, fg * 256:(fg + 1) * 256].rearrange("(k p) n -> p k n", p=128), wb))
            for dc in range(KC):
                conv.append((WDS[ex, dc].rearrange("p (f n) -> p f n", n=128), Wd_[:, dc * 128:(dc + 1) * 128].rearrange("(f p) n -> p f n", p=128), wb))
        conv.reverse()
        conv_per_blk = -(-len(conv) // NBLK)

        def issue_conv(n):
            for _ in range(n):
                if conv:
                    d_, s_, wb_ = conv.pop()
                    P.dma("pool", d_, s_, [CONST], [wb_])

        PS = []
        for i in range(8):
            t = es.enter_context(nc.psum_tensor(f"ps{i}", [128, 512], F32))
            PS.append(Buf(f"ps{i}", t))
        psr = Rot(PS)

        identF = P.tile("identF", [128, 128], F32)
        onesB = P.tile("onesB", [128, 128], BF16)
        onesF = P.tile("onesF", [128, 128], F32)
        vecA = [P.tile(f"vecA{l}", [128, 128], F32) for l in range(DEPTH)]
        vecB = P.tile("vecB", [128, 64], F32)
        MOD = [[P.tile(f"mod{l}{s}", [128, 96], F32) for s in range(2)] for l in range(DEPTH)]
        A1 = [[P.tile(f"a1{l}{s}", [128, 16], F32) for s in range(2)] for l in range(DEPTH)]
        A2 = [[P.tile(f"a2{l}{s}", [128, 16], F32) for s in range(2)] for l in range(DEPTH)]
        GL = P.tile("GL", [128, 64], F32)
        BT = [[P.tile(f"bt{kv}{jb}", [128, 4, 128], F32) for jb in range(3)] for kv in range(2)]
        EM = P.tile("EM", [128, 2 * (NTOK // 128)], F32)
        ESK = [P.tile(f"esk{l}", [128, 8], F32) for l in range(DEPTH)]
        SEL = P.tile("SEL", [8, NE, 128], F32)

        EPSB = P.tile("EPSB", [128, 3], F32)
        for i_, v_ in enumerate((EPS * D, EPS * 512, EPS * 1024)):
            P.op("dve", lambda e, i_=i_, v_=v_: e.memset(EPSB.t[:, i_:i_ + 1], v_), [], [EPSB])

        def epsb(v):
            i_ = {EPS * D: 0, EPS * 512: 1, EPS * 1024: 2}[v]
            return EPSB.t[:, i_:i_ + 1]
        P.dma("sp", identF.t[:], ident_in[:, :], [CONST], [identF])
        P.dma("sp", EM.t[:], em_in[:, :], [CONST], [EM])
        P.op("dve", lambda e: e.memset(onesB.t[:], 1.0), [], [onesB])
        P.op("dve", lambda e: e.memset(onesF.t[:], 1.0), [], [onesF])

        def transpose_rows(scope, rows_aps, out_buf, ncols):
            st = P.tile("stg", [128, 128], F32, scope)
            P.op("dve", lambda e: e.memset(st.t[:], 0.0), [], [st])
            r0 = 0
            for ap, r in rows_aps:
                P.dma("sp", st.t[r0:r0 + r, :], ap, [CONST], [st])
                r0 += r
            ps = psr.next()
            P.op("pe", lambda e: e.transpose(ps.t[:, 0:128], st.t[:], identF.t[:]), [st, identF], [ps])
            P.op("dve", lambda e: e.tensor_copy(out=out_buf.t[:, 0:ncols], in_=ps.t[:, 0:ncols]), [ps], [out_buf])

        def bcast_row(src_ap, n, out_ap, out_buf, scope, func=None):
            row = P.tile("row", [1, n], F32, scope)
            P.dma("sp", row.t[:], src_ap, [CONST], [row])
            ps = psr.next()
            P.op("pe", lambda e: e.matmul(ps.t[:, 0:n], onesF.t[0:1, :], row.t[:], start=True, stop=True), [onesF, row], [ps])
            if func is None:
                P.op("dve", lambda e: e.tensor_copy(out=out_ap, in_=ps.t[:, 0:n]), [ps], [out_buf])
            else:
                P.op("act", lambda e: e.activation(out=out_ap, in_=ps.t[:, 0:n], func=func), [ps], [out_buf])

        with ExitStack() as sc:
            for l in range(DEPTH):
                transpose_rows(sc, [(b_ada[l].rearrange("(c p) -> c p", p=128), 96),
                                    (g_norm_mix[l].rearrange("(c p) -> c p", p=128), 16),
                                    (g_norm_ffn[l].rearrange("(c p) -> c p", p=128), 16)], vecA[l], 128)
            rows = []
            for l in range(DEPTH):
                rows += [(g_q_lat[l].rearrange("(c p) -> c p", p=128), 4), (g_kv_lat[l].rearrange("(c p) -> c p", p=128), 4),
                         (g_out_a[l].rearrange("(c p) -> c p", p=128), 8), (g_out_b[l].rearrange("(c p) -> c p", p=128), 8)]
            rows += [(g_final.rearrange("(c p) -> c p", p=128), 16)]
            transpose_rows(sc, rows, vecB, 24 * DEPTH + 16)
            for l in range(DEPTH):
                o = 24 * l
                P.op("dve", lambda e, o=o: e.tensor_scalar_mul(out=GL.t[:, o:o + 8], in0=vecB.t[:, o:o + 8], scalar1=math.sqrt(512.0)), [vecB], [GL])
                P.op("dve", lambda e, o=o: e.tensor_scalar_mul(out=GL.t[:, o + 8:o + 24], in0=vecB.t[:, o + 8:o + 24], scalar1=math.sqrt(1024.0)), [vecB], [GL])
            o = 24 * DEPTH
            P.op("dve", lambda e: e.tensor_scalar_mul(out=GL.t[:, o:o + 16], in0=vecB.t[:, o:o + 16], scalar1=math.sqrt(float(D))), [vecB], [GL])

            for l in range(DEPTH):
                bcast_row(sink[l:l + 1, :], 8, ESK[l].t[:, :], ESK[l], sc, func=AF.Exp)
            for e_ in range(NE):
                P.op("dve", lambda e, e_=e_: e.tensor_scalar_mul(out=SEL.t[0:8, e_, :], in0=onesF.t[0:8, :], scalar1=identF.t[0:8, e_:e_ + 1]),
                     [onesF, identF], [SEL])

            rb = P.tile("rb", [33, 8], F32, sc)
            P.op("dve", lambda e: e.memset(rb.t[:], NEG), [], [rb])
            P.dma("sp", rb.t[0:32, :], rel_bias[:, :], [CONST], [rb])
            ohs = P.tile("ohs", [33, 512], F32, sc)
            P.dma("sp", ohs.t[:], oh_in[:, :], [CONST], [ohs])
            onesF33 = P.tile("ones33", [33, 128], F32, sc)
            P.op("dve", lambda e: e.memset(onesF33.t[:], 1.0), [], [onesF33])
            for h in range(8):
                lh = P.tile("lh", [33, 128], F32, sc)
                P.op("dve", lambda e, h=h, lh=lh: e.tensor_scalar_mul(out=lh.t[:], in0=onesF33.t[:], scalar1=rb.t[:, h:h + 1]), [onesF33, rb], [lh])
                ps = psr.next()
                P.op("pe", lambda e, lh=lh, ps=ps: e.matmul(ps.t[:, :], lh.t[:], ohs.t[:], start=True, stop=True), [lh, ohs], [ps])
                tr = P.tile("tr", [128, 512], F32, sc)
                P.op("dve", lambda e, ps=ps, tr=tr: e.tensor_copy(out=tr.t[:], in_=ps.t[:]), [ps], [tr])
                P.dma("sp", TB[h], tr.t[:], [tr], [TBb])
            for h in range(8):
                kv, g = h // 4, h % 4
                flat = TB[h].rearrange("p c -> (p c)")
                for jb in range(3):
                    cpr = 383 - 128 * jb
                    src = bass.AP(tensor=flat.tensor, offset=flat.offset + cpr, ap=[[511, 128], [1, 128]])
                    P.dma("sp", BT[kv][jb].t[:, g, :], src, [TBb], [BT[kv][jb]])

            cin = P.tile("cin", [2, D], F32, sc)
            P.dma("sp", cin.t[:], c_in[:, :], [CONST], [cin])
            csl = P.tile("csl", [2, D], F32, sc)
            P.op("act", lambda e: e.activation(out=csl.t[:], in_=cin.t[:], func=AF.Silu), [cin], [csl])
            csT = P.tile("csT", [128, KC, 2], BF16, sc)
            ps = psr.next()
            for k in range(KC):
                P.op("pe", lambda e, k=k: e.transpose(ps.t[:, 2 * k:2 * k + 2], csl.t[0:2, k * 128:(k + 1) * 128], identF.t[0:2, 0:2]), [csl, identF], [ps])
            P.op("dve", lambda e: e.tensor_copy(out=csT.t[:].rearrange("p k b -> p (k b)"), in_=ps.t[:, 0:2 * KC]), [ps], [csT])
            wa = Rot([P.tile(f"wa{i}", [128, KC, 512], BF16, sc) for i in range(3)])
            for l in range(DEPTH):
                psm = psr.next()
                for cg in range(24):
                    wt = wa.next()
                    P.dma("pool", wt.t[:], w_ada[l][:, cg * 512:(cg + 1) * 512].rearrange("(k p) n -> p k n", p=128), [CONST], [wt])
                    for oc in range(4):
                        occ = cg * 4 + oc
                        for k in range(KC):
                            P.op("pe", lambda e, k=k, oc=oc, occ=occ, wt=wt: e.matmul(psm.t[:, 2 * occ:2 * occ + 2], wt.t[:, k, oc * 128:(oc + 1) * 128], csT.t[:, k, :],
                                                                          start=(k == 0), stop=(k == KC - 1)), [wt, csT], [psm])
                pv = psm.t[:, 0:192].rearrange("p (c b) -> p c b", b=2)
                for s in range(2):
                    P.op("dve", lambda e, s=s, l=l: e.tensor_tensor(out=MOD[l][s].t[:], in0=pv[:, :, s], in1=vecA[l].t[:, 0:96], op=ALU.add), [psm, vecA[l]], [MOD[l][s]])
                    for (A, c0, g0) in ((A1, 16, 96), (A2, 64, 112)):
                        P.op("dve", lambda e, A=A, c0=c0, s=s, l=l: e.tensor_scalar(out=A[l][s].t[:], in0=MOD[l][s].t[:, c0:c0 + 16], scalar1=1.0, scalar2=math.sqrt(float(D)),
                                                                           op0=ALU.add, op1=ALU.mult), [MOD[l][s]], [A[l][s]])
                        P.op("dve", lambda e, A=A, g0=g0, s=s, l=l: e.tensor_tensor(out=A[l][s].t[:], in0=A[l][s].t[:], in1=vecA[l].t[:, g0:g0 + 16], op=ALU.mult), [A[l][s], vecA[l]], [A[l][s]])
            P.end_phase()

        def rstd_from_sq(srcs, nsq, Rb, sqr, eps_n):
            ps = psr.next()
            for i, (ap, b) in enumerate(srcs):
                sq = sqr.next()
                P.op("act", lambda e, ap=ap, sq=sq: e.activation(out=sq.t[:], in_=ap, func=AF.Square), [b], [sq])
                P.op("pe", lambda e, sq=sq, i=i: e.matmul(ps.t[:, :], onesB.t[:], sq.t[:], start=(i == 0), stop=(i == nsq - 1)), [onesB, sq], [ps])
            P.op("act", lambda e: e.activation(out=Rb.t[:], in_=ps.t[:], func=AF.Sqrt, bias=epsb(eps_n), scale=1.0), [ps, EPSB], [Rb])
            P.op("dve", lambda e: e.reciprocal(out=Rb.t[:], in_=Rb.t[:]), [Rb], [Rb])

        for l in range(DEPTH):
            last = (l == DEPTH - 1)
            if l == 0:
                own_blocks = list(range(NBLK))
            else:
                own_blocks = list(range(OWN // T)) + list(range(S_P // T, NBLK))
            own_set = set(own_blocks)

            with ExitStack() as sc:
                xin = Rot([P.tile(f"xin{i}", [128, D], F32, sc) for i in range(2)])
                XS = P.tile("XS", [128, KC, T], F32, sc)
                sqr = Rot([P.tile(f"sq{i}", [128, T], BF16, sc) for i in range(3)])
                Rb = P.tile("R", [128, T], F32, sc)
                tmpr = Rot([P.tile(f"tmp{i}", [128, T], F32, sc) for i in range(3)])
                hb = P.tile("h", [128, KC, T], BF16, sc)
                lat32 = P.tile("lat32", [128, 4, T], F32, sc)
                cqn = P.tile("cqn", [128, 4, T], BF16, sc)
                ckvn = P.tile("ckvn", [128, 4, T], BF16, sc)
                kr32 = P.tile("kr32", [64, 2, T], F32, sc)
                krb = P.tile("krb", [64, T], BF16, sc)
                cs = P.tile("cs", [64, 2, T], F32, sc)
                o1 = Rot([P.tile(f"o1{i}", [128, T], BF16, sc) for i in range(4)])
                gvst = P.tile("gvst", [128, 4, 256], BF16, sc)
                wp = Rot([P.tile(f"wp{i}", [128, KC, 512], BF16, sc) for i in range(3)])
                wuq = P.tile("wuq", [128, 4, 2048], BF16, sc)
                P.dma("pool", wuq.t[:, :, 0:1536], w_uq[l].rearrange("(k p) n -> p k n", p=128), [CONST], [wuq])
                P.dma("pool", wuq.t[:, :, 1536:2048], w_uqs[l].rearrange("(k p) n -> p k n", p=128), [CONST], [wuq])

                def wload(col0, ncol, extra=None):
                    wt = wp.next()
                    P.dma("pool", wt.t[:, :, 0:ncol], w_in[l][:, col0:col0 + ncol].rearrange("(k p) n -> p k n", p=128), [CONST], [wt])
                    if extra is not None:
                        P.dma("pool", wt.t[:, :, ncol:ncol + 64], extra.rearrange("(k p) n -> p k n", p=128), [CONST], [wt])
                    return wt

                def proj(wt, c0, m, ps_ap, ps):
                    for k in range(KC):
                        P.op("pe", lambda e, k=k: e.matmul(ps_ap, wt.t[:, k, c0:c0 + m], hb.t[:, k, :], start=(k == 0), stop=(k == KC - 1)), [wt, hb], [ps])

                for blk in range(NBLK):
                    seg = 0 if blk * T < S_P else 1
                    t0 = blk * T
                    need_q = blk in own_set
                    if blk > 0:
                        issue_conv(conv_per_blk)
                    if l == 0:
                        for s in range(4):
                            xt = xin.next()
                            P.dma("sp", xt.t[:], x_in[t0 + s * 128:t0 + (s + 1) * 128, :], [CONST], [xt])
                            for k4 in range(4):
                                ps = psr.next()
                                for kk in range(4):
                                    k = k4 * 4 + kk
                                    P.op("pe", lambda e, k=k, kk=kk, xt=xt, ps=ps: e.transpose(ps.t[:, kk * 128:(kk + 1) * 128], xt.t[:, k * 128:(k + 1) * 128], identF.t[:]), [xt, identF], [ps])
                                P.op("act" if k4 % 2 else "dve",
                                     lambda e, k4=k4, s=s, ps=ps: (e.copy if e is nc.scalar else e.tensor_copy)(out=XS.t[:, k4 * 4:(k4 + 1) * 4, s * 128:(s + 1) * 128],
                                                                                                       in_=ps.t[:, :].rearrange("p (k t) -> p k t", t=128)), [ps], [XS])
                        P.dma("sp", XT[blk], XS.t[:], [XS], [XTb[blk]])
                    else:
                        P.dma("sp", XS.t[:], XT[blk], [XTb[blk]], [XS])
                    P.dma("sp", cs.t[:], rope_in[:, :, t0:t0 + T], [CONST], [cs])
                    rstd_from_sq([(XS.t[:, k, :], XS) for k in range(KC)], KC, Rb, sqr, EPS * D)
                    for k in range(KC):
                        tm = tmpr.next()
                        P.op("dve", lambda e, k=k, tm=tm: e.tensor_tensor(out=tm.t[:], in0=XS.t[:, k, :], in1=Rb.t[:], op=ALU.mult), [XS, Rb], [tm])
                        P.op("act", lambda e, k=k, tm=tm: e.activation(out=hb.t[:, k, :], in_=tm.t[:], func=AF.Identity, bias=MOD[l][seg].t[:, k:k + 1], scale=A1[l][seg].t[:, k:k + 1]),
                             [tm, MOD[l][seg], A1[l][seg]], [hb])
                    for which in ((0, 1) if need_q else (1,)):
                        wt = wload(512 * which, 512)
                        for c in range(4):
                            ps = psr.next()
                            proj(wt, c * 128, 128, ps.t[:, :], ps)
                            P.op("act", lambda e, c=c, ps=ps: e.copy(out=lat32.t[:, c, :], in_=ps.t[:, :]), [ps], [lat32])
                        rstd_from_sq([(lat32.t[:, c, :], lat32) for c in range(4)], 4, Rb, sqr, EPS * 512)
                        dst = cqn if which == 0 else ckvn
                        go = 24 * l + 4 * which
                        for c in range(4):
                            tm = tmpr.next()
                            P.op("dve", lambda e, c=c, tm=tm: e.tensor_tensor(out=tm.t[:], in0=lat32.t[:, c, :], in1=Rb.t[:], op=ALU.mult), [lat32, Rb], [tm])
                            P.op("act", lambda e, c=c, tm=tm, dst=dst, go=go: e.activation(out=dst.t[:, c, :], in_=tm.t[:], func=AF.Identity, scale=GL.t[:, go + c:go + c + 1]), [tm, GL], [dst])
                    P.dma("sp", LAT[0:512, t0:t0 + T].rearrange("(c p) t -> p c t", p=128), ckvn.t[:], [ckvn], [LATb])
                    wt = wp.next()
                    P.dma("pool", wt.t[:, :, 0:64], w_in[l][:, 1024:1088].rearrange("(k p) n -> p k n", p=128), [CONST], [wt])
                    P.dma("pool", wt.t[:, :, 64:128], w_krs[l].rearrange("(k p) n -> p k n", p=128), [CONST], [wt])
                    P.dma("pool", wt.t[:, :, 128:512], w_in[l][:, 2112:2496].rearrange("(k p) n -> p k n", p=128), [CONST], [wt])
                    wt2 = wp.next()
                    P.dma("pool", wt2.t[:, :, 0:128], w_in[l][:, 2496:2624].rearrange("(k p) n -> p k n", p=128), [CONST], [wt2])
                    ps = psr.next()
                    proj(wt, 0, 64, ps.t[0:64, :], ps)
                    ps2 = psr.next()
                    proj(wt, 64, 64, ps2.t[0:64, :], ps2)
                    P.op("dve", lambda e, ps=ps: e.tensor_tensor(out=kr32.t[:, 0, :], in0=ps.t[0:64, :], in1=cs.t[:, 0, :], op=ALU.mult), [ps, cs], [kr32])
                    P.op("dve", lambda e, ps2=ps2: e.tensor_tensor(out=kr32.t[:, 1, :], in0=ps2.t[0:64, :], in1=cs.t[:, 1, :], op=ALU.mult), [ps2, cs], [kr32])
                    P.op("dve", lambda e: e.tensor_tensor(out=krb.t[:], in0=kr32.t[:, 0, :], in1=kr32.t[:, 1, :], op=ALU.add), [kr32], [krb])
                    P.dma("sp", LAT[512:576, t0:t0 + T], krb.t[:], [krb], [LATb])
                    for j in range(2):
                        ps = psr.next()
                        proj(wt, 128 + j * 128, 128, ps.t[:, :], ps)
                        ot = o1.next()
                        P.op("act", lambda e, ps=ps, ot=ot: e.copy(out=ot.t[:], in_=ps.t[:, :]), [ps], [ot])
                        P.dma("sp", GK[j, :, t0:t0 + T], ot.t[:], [ot], [GKb])
                    for s in range(4):
                        ps = psr.next()
                        for half, (wsrc, c0) in enumerate(((wt, 384), (wt2, 0))):
                            for k in range(KC):
                                P.op("pe", lambda e, k=k, s=s, half=half, wsrc=wsrc, c0=c0, ps=ps: e.matmul(ps.t[:, half * 128:(half + 1) * 128], hb.t[:, k, s * 128:(s + 1) * 128], wsrc.t[:, k, c0:c0 + 128],
                                                                                                start=(k == 0), stop=(k == KC - 1)), [hb, wsrc], [ps])
                        P.op("dve", lambda e, s=s, ps=ps: e.tensor_copy(out=gvst.t[:, s, :], in_=ps.t[:, 0:256]), [ps], [gvst])
                    P.dma("sp", GV[t0:t0 + T, :].rearrange("(s p) n -> p s n", p=128), gvst.t[:], [gvst], [GVb])
                    if not need_q:
                        continue
                    for half in range(2):
                        wt = wload(1088 + 512 * half, 512)
                        for j in range(4):
                            ps = psr.next()
                            proj(wt, j * 128, 128, ps.t[:, :], ps)
                            ot = o1.next()
                            P.op("act" if j % 2 else "dve", lambda e, ps=ps, ot=ot: (e.copy if e is nc.scalar else e.tensor_copy)(out=ot.t[:], in_=ps.t[:, :]), [ps], [ot])
                            P.dma("sp", GQ[half * 4 + j, :, t0:t0 + T], ot.t[:], [ot], [GQb])
                    for h in range(H):
                        ps = psr.next()
                        for c in range(4):
                            P.op("pe", lambda e, c=c, h=h, ps=ps: e.matmul(ps.t[:, :], wuq.t[:, c, h * 192:h * 192 + 128], cqn.t[:, c, :], start=(c == 0), stop=(c == 3)), [wuq, cqn], [ps])
                        ot = o1.next()
                        P.op("act", lambda e, ps=ps, ot=ot: e.copy(out=ot.t[:], in_=ps.t[:, :]), [ps], [ot])
                        P.dma("sp", QT[h, 0:128, t0:t0 + T], ot.t[:], [ot], [QTb])
                        ps = psr.next()
                        ps2 = psr.next()
                        for c in range(4):
                            P.op("pe", lambda e, c=c, h=h, ps=ps: e.matmul(ps.t[0:64, :], wuq.t[:, c, h * 192 + 128:h * 192 + 192], cqn.t[:, c, :], start=(c == 0), stop=(c == 3)), [wuq, cqn], [ps])
                        for c in range(4):
                            P.op("pe", lambda e, c=c, h=h, ps2=ps2: e.matmul(ps2.t[0:64, :], wuq.t[:, c, 1536 + h * 64:1536 + h * 64 + 64], cqn.t[:, c, :], start=(c == 0), stop=(c == 3)), [wuq, cqn], [ps2])
                        P.op("dve", lambda e, ps=ps: e.tensor_tensor(out=kr32.t[:, 0, :], in0=ps.t[0:64, :], in1=cs.t[:, 0, :], op=ALU.mult), [ps, cs], [kr32])
                        P.op("dve", lambda e, ps2=ps2: e.tensor_tensor(out=kr32.t[:, 1, :], in0=ps2.t[0:64, :], in1=cs.t[:, 1, :], op=ALU.mult), [ps2, cs], [kr32])
                        ot = o1.next()
                        P.op("dve", lambda e, ot=ot: e.tensor_tensor(out=ot.t[0:64, :], in0=kr32.t[:, 0, :], in1=kr32.t[:, 1, :], op=ALU.add), [kr32], [ot])
                        P.dma("sp", QT[h, 128:192, t0:t0 + T], ot.t[0:64, :], [ot], [QTb])
                issue_conv(len(conv))
                P.end_phase()

            with ExitStack() as sc:
                wukv = P.tile("wukv", [128, 4, 2048], BF16, sc)
                P.dma("pool", wukv.t[:], w_ukv[l].rearrange("(k p) n -> p k n", p=128), [CONST], [wukv])
                SMAX = max(S_P, S_S)
                krT = P.tile("krT", [64, SMAX], BF16, sc)
                KT = P.tile("KT", [128, SMAX], BF16, sc)
                VV = P.tile("VV", [128, SMAX // 128, 128], BF16, sc)
                latr = Rot([P.tile(f"lat{i}", [128, 4, T], BF16, sc) for i in range(3)])
                qnr = Rot([P.tile(f"qn{i}", [128, T], BF16, sc) for i in range(2)])
                qrr = Rot([P.tile(f"qr{i}", [64, T], BF16, sc) for i in range(2)])
                pr = Rot([P.tile(f"p{i}", [128, T], BF16, sc) for i in range(3)])
                rdr = Rot([P.tile(f"rd{i}", [128, T], F32, sc) for i in range(2)])
                obr = Rot([P.tile(f"ob{i}", [128, T], BF16, sc) for i in range(2)])
                accr = Rot([P.tile(f"acc{i}", [128, T], F32, sc) for i in range(2)])
                psS = Rot(PS[0:2])
                psO = Rot(PS[2:4])
                psD = Rot(PS[4:6])
                psK = Rot(PS[6:8])
                for (sname, s0, slen) in segs:
                    nq = (slen if (l == 0 or sname == "S") else OWN) // T
                    nkc = slen // 128
                    P.dma("sp", krT.t[:, 0:slen], LAT[512:576, s0:s0 + slen], [LATb], [krT])
                    for h in range(H):
                        for tc_ in range(slen // T):
                            lt = latr.next()
                            P.dma("sp", lt.t[:], LAT[0:512, s0 + tc_ * T:s0 + (tc_ + 1) * T].rearrange("(c p) t -> p c t", p=128), [LATb], [lt])
                            ps = psK.next()
                            for c in range(4):
                                P.op("pe", lambda e, c=c, h=h, lt=lt, ps=ps: e.matmul(ps.t[:, :], wukv.t[:, c, h * 256:h * 256 + 128], lt.t[:, c, :], start=(c == 0), stop=(c == 3)), [wukv, lt], [ps])
                            P.op("act", lambda e, ps=ps, tc_=tc_: e.copy(out=KT.t[:, tc_ * T:(tc_ + 1) * T], in_=ps.t[:, :]), [ps], [KT])
                            ps = psK.next()
                            for s in range(4):
                                for c in range(4):
                                    P.op("pe", lambda e, c=c, s=s, h=h, lt=lt, ps=ps: e.matmul(ps.t[:, s * 128:(s + 1) * 128], lt.t[:, c, s * 128:(s + 1) * 128], wukv.t[:, c, h * 256 + 128:h * 256 + 256],
                                                                                      start=(c == 0), stop=(c == 3)), [lt, wukv], [ps])
                            P.op("dve", lambda e, ps=ps, tc_=tc_: e.tensor_copy(out=VV.t[:, tc_ * 4:(tc_ + 1) * 4, :], in_=ps.t[:, :].rearrange("p (s d) -> p s d", d=128)), [ps], [VV])
                        for qb in range(nq):
                            q0 = s0 + qb * T
                            qn = qnr.next()
                            qr = qrr.next()
                            P.dma("sp", qn.t[:], QT[h, 0:128, q0:q0 + T], [QTb], [qn])
                            P.dma("sp", qr.t[:], QT[h, 128:192, q0:q0 + T], [QTb], [qr])
                            po = psO.next()
                            pd = psD.next()
                            acc = accr.next()

                            def qk(kc, qn=qn, qr=qr):
                                ps = psS.next()
                                P.op("pe", lambda e: e.matmul(ps.t[:, :], KT.t[:, kc * 128:(kc + 1) * 128], qn.t[:], start=True, stop=False), [KT, qn], [ps])
                                P.op("pe", lambda e: e.matmul(ps.t[:, :], krT.t[:, kc * 128:(kc + 1) * 128], qr.t[:], start=False, stop=True), [krT, qr], [ps])
                                return ps
                            pscur = qk(0)
                            for kc in range(nkc):
                                psn = qk(kc + 1) if kc + 1 < nkc else None
                                pt = pr.next()
                                P.op("act", lambda e, pscur=pscur, pt=pt: e.activation(out=pt.t[:], in_=pscur.t[:, :], func=AF.Exp, scale=MLA_SCALE), [pscur], [pt])
                                P.op("pe", lambda e, kc=kc, pt=pt: e.matmul(po.t[:, :], VV.t[:, kc, :], pt.t[:], start=(kc == 0), stop=(kc == nkc - 1)), [VV, pt], [po])
                                if kc == 0:
                                    P.op("dve", lambda e, pt=pt, acc=acc: e.tensor_copy(out=acc.t[:], in_=pt.t[:]), [pt], [acc])
                                else:
                                    P.op("dve", lambda e, pt=pt, acc=acc: e.tensor_tensor(out=acc.t[:], in0=acc.t[:], in1=pt.t[:], op=ALU.add), [pt, acc], [acc])
                                pscur = psn
                            P.op("pe", lambda e, acc=acc, pd=pd: e.matmul(pd.t[:, :], onesF.t[:], acc.t[:], start=True, stop=True), [onesF, acc], [pd])
                            rd = rdr.next()
                            ob = obr.next()
                            P.op("dve", lambda e, rd=rd, pd=pd: e.reciprocal(out=rd.t[:], in_=pd.t[:, :]), [pd], [rd])
                            P.op("dve", lambda e, rd=rd, ob=ob, po=po: e.tensor_tensor(out=ob.t[:], in0=po.t[:, :], in1=rd.t[:], op=ALU.mult), [po, rd], [ob])
                            P.dma("sp", OA[h, :, q0:q0 + T], ob.t[:], [ob], [OAb])
                P.end_phase()

            with ExitStack() as sc:
                SMAX = max(S_P, S_S)
                gkT = P.tile("gkT", [128, SMAX], BF16, sc)
                gvv = P.tile("gvv", [128, SMAX // 128, 128], BF16, sc)
                gqr = Rot([P.tile(f"gq{i}", [128, 4, T], BF16, sc) for i in range(2)])
                tmr = Rot([P.tile(f"wt{i}", [128, 4, 128], F32, sc) for i in range(3)])
                pr = Rot([P.tile(f"wp{i}", [128, 4, 128], BF16, sc) for i in range(3)])
                dnr = Rot([P.tile(f"dn{i}", [128, 4, 128], F32, sc) for i in range(2)])
                oor = Rot([P.tile(f"oo{i}", [128, 4, T], BF16, sc) for i in range(2)])
                psS = Rot(PS[0:3])
                psO = Rot(PS[3:5])
                psD = Rot(PS[5:7])
                for (sname, s0, slen) in segs:
                    nb = slen // 128
                    nqb = (slen if (l == 0 or sname == "S") else OWN) // 128
                    b0 = s0 // 128
                    for kv in range(2):
                        P.dma("sp", gkT.t[:, 0:slen], GK[kv, :, s0:s0 + slen], [GKb], [gkT])
                        P.dma("sp", gvv.t[:, 0:nb, :], GV[s0:s0 + slen, kv * 128:(kv + 1) * 128].rearrange("(n p) d -> p n d", p=128), [GVb], [gvv])
                        for n in range(nqb):
                            n4 = n % 4
                            if n4 == 0:
                                gq = gqr.next()
                                for g in range(4):
                                    P.dma("sp", gq.t[:, g, :], GQ[kv * 4 + g, :, s0 + n * 128:s0 + n * 128 + T], [GQb], [gq])
                                oo = oor.next()
                            po = psO.next()
                            pd = psD.next()
                            for jb in range(3):
                                m = (n + jb - 1) % nb
                                ps = psS.next()
                                P.op("pe", lambda e, m=m, n4=n4, gq=gq, ps=ps: e.matmul(ps.t[:, :].rearrange("p (g q) -> p g q", q=128), gkT.t[:, m * 128:(m + 1) * 128], gq.t[:, :, n4 * 128:(n4 + 1) * 128],
                                                                              start=True, stop=True), [gkT, gq], [ps])
                                tm = tmr.next()
                                P.op("dve", lambda e, ps=ps, tm=tm, jb=jb: e.scalar_tensor_tensor(out=tm.t[:], in0=ps.t[:, :].rearrange("p (g q) -> p g q", q=128), scalar=GQA_SCALE, in1=BT[kv][jb].t[:],
                                                                                               op0=ALU.mult, op1=ALU.add), [ps, BT[kv][jb]], [tm])
                                pt = pr.next()
                                if jb == 1:
                                    P.op("act", lambda e, tm=tm, pt=pt: e.activation(out=pt.t[:], in_=tm.t[:], func=AF.Exp), [tm], [pt])
                                else:
                                    col = 2 * (b0 + n) + (0 if jb == 0 else 1)
                                    P.op("act", lambda e, tm=tm, pt=pt, col=col: e.activation(out=pt.t[:], in_=tm.t[:], func=AF.Exp, bias=EM.t[:, col:col + 1]), [tm, EM], [pt])
                                P.op("pe", lambda e, m=m, pt=pt, jb=jb, po=po: e.matmul(po.t[:, :].rearrange("p (g q) -> p g q", q=128), gvv.t[:, m, :], pt.t[:], start=(jb == 0), stop=(jb == 2)), [gvv, pt], [po])
                                P.op("pe", lambda e, pt=pt, jb=jb, pd=pd: e.matmul(pd.t[:, :].rearrange("p (g q) -> p g q", q=128), onesB.t[:], pt.t[:], start=(jb == 0), stop=(jb == 2)), [onesB, pt], [pd])
                            dn = dnr.next()
                            for g in range(4):
                                P.op("dve", lambda e, g=g, dn=dn, pd=pd: e.tensor_scalar_add(out=dn.t[:, g, :], in0=pd.t[:, g * 128:(g + 1) * 128], scalar1=ESK[l].t[:, kv * 4 + g:kv * 4 + g + 1]), [pd, ESK[l]], [dn])
                            P.op("dve", lambda e, dn=dn: e.reciprocal(out=dn.t[:], in_=dn.t[:]), [dn], [dn])
                            P.op("dve", lambda e, dn=dn, po=po, oo=oo, n4=n4: e.tensor_tensor(out=oo.t[:, :, n4 * 128:(n4 + 1) * 128], in0=po.t[:, :].rearrange("p (g q) -> p g q", q=128), in1=dn.t[:], op=ALU.mult),
                                 [po, dn], [oo])
                            if n4 == 3:
                                for g in range(4):
                                    P.dma("sp", OB[kv * 4 + g, :, s0 + (n - 3) * 128:s0 + (n - 3) * 128 + T], oo.t[:, g, :], [oo], [OBb])
                P.end_phase()

            with ExitStack() as sc:
                XS = P.tile("XS4", [128, KC, T], F32, sc)
                sqr = Rot([P.tile(f"sq{i}", [128, T], BF16, sc) for i in range(3)])
                Rb = P.tile("R4", [128, T], F32, sc)
                tmpr = Rot([P.tile(f"tmp{i}", [128, T], F32, sc) for i in range(3)])
                hb = P.tile("h4", [128, KC, T], BF16, sc)
                act = P.tile("act", [128, max(FC, 16), T], BF16, sc)
                wgu = Rot([P.tile(f"wgu{i}", [128, KC, 256], BF16, sc) for i in range(4)])
                wdr = Rot([P.tile(f"wd{i}", [128, FC, 128], BF16, sc) for i in range(2)])
                moe = (l % 2 == 1)
                if moe:
                    gbt = P.tile("gb", [128, NE, T], BF16, sc)
                    h32r = Rot([P.tile(f"h32{i}", [128, T], F32, sc) for i in range(2)])
                    wr32 = P.tile("wr32", [128, KC, NE], F32, sc)
                    P.dma("sp", wr32.t[:], w_router[0].rearrange("(k p) n -> p k n", p=128), [CONST], [wr32])
                    lg = P.tile("lg", [128, 4, NE], F32, sc)
                    sm = P.tile("sm", [128, 16], F32, sc)
                    l2 = P.tile("l2", [128, 4, NE], F32, sc)
                    gts = P.tile("gts", [128, 4, NE], F32, sc)
                    gT = P.tile("gT", [8, T], F32, sc)
                OAt = act.t[:, 0:8, :]
                OBt = act.t[:, 8:16, :]

                for bi, blk in enumerate(own_blocks):
                    seg = 0 if blk * T < S_P else 1
                    t0 = blk * T
                    P.dma("sp", XS.t[:], XT[blk], [XTb[blk]], [XS])
                    P.dma("sp", OAt, OA[:, :, t0:t0 + T].rearrange("h p t -> p h t"), [OAb], [act])
                    P.dma("sp", OBt, OB[:, :, t0:t0 + T].rearrange("h p t -> p h t"), [OBb], [act])
                    for grp in range(2):
                        src = OAt if grp == 0 else OBt
                        ps = psr.next()
                        for c in range(8):
                            sq = sqr.next()
                            P.op("act", lambda e, c=c, sq=sq, src=src: e.activation(out=sq.t[:], in_=src[:, c, :], func=AF.Square), [act], [sq])
                            P.op("pe", lambda e, c=c, sq=sq, ps=ps: e.matmul(ps.t[:, :], onesB.t[:], sq.t[:], start=(c == 0), stop=(c == 7)), [onesB, sq], [ps])
                        P.op("act", lambda e, ps=ps: e.activation(out=Rb.t[:], in_=ps.t[:, :], func=AF.Sqrt, bias=epsb(EPS * 1024), scale=1.0), [ps, EPSB], [Rb])
                        P.op("dve", lambda e: e.reciprocal(out=Rb.t[:], in_=Rb.t[:]), [Rb], [Rb])
                        go = 24 * l + 8 + 8 * grp
                        for c in range(8):
                            tm = tmpr.next()
                            P.op("dve", lambda e, c=c, tm=tm, src=src: e.tensor_tensor(out=tm.t[:], in0=src[:, c, :], in1=Rb.t[:], op=ALU.mult), [act, Rb], [tm])
                            P.op("act", lambda e, c=c, tm=tm, go=go, grp=grp: e.activation(out=hb.t[:, grp * 8 + c, :], in_=tm.t[:], func=AF.Identity, scale=GL.t[:, go + c:go + c + 1]), [tm, GL], [hb])
                    for dg in range(8):
                        wt = wgu.next()
                        P.dma("pool", wt.t[:], w_out[l][:, dg * 256:(dg + 1) * 256].rearrange("(k p) n -> p k n", p=128), [CONST], [wt])
                        for dd in range(2):
                            dc = dg * 2 + dd
                            ps = psr.next()
                            for k in range(KC):
                                P.op("pe", lambda e, k=k, dd=dd, wt=wt, ps=ps: e.matmul(ps.t[:, :], wt.t[:, k, dd * 128:(dd + 1) * 128], hb.t[:, k, :], start=(k == 0), stop=(k == KC - 1)), [wt, hb], [ps])
                            P.op("dve", lambda e, dc=dc, ps=ps, seg=seg: e.scalar_tensor_tensor(out=XS.t[:, dc, :], in0=ps.t[:, :], scalar=MOD[l][seg].t[:, 32 + dc:33 + dc], in1=XS.t[:, dc, :],
                                                                                     op0=ALU.mult, op1=ALU.add), [ps, MOD[l][seg], XS], [XS])
                    rstd_from_sq([(XS.t[:, k, :], XS) for k in range(KC)], KC, Rb, sqr, EPS * D)
                    if moe:
                        psl = psr.next()
                    for k in range(KC):
                        tm = tmpr.next()
                        P.op("dve", lambda e, k=k, tm=tm: e.tensor_tensor(out=tm.t[:], in0=XS.t[:, k, :], in1=Rb.t[:], op=ALU.mult), [XS, Rb], [tm])
                        if not moe:
                            P.op("act", lambda e, k=k, tm=tm, seg=seg: e.activation(out=hb.t[:, k, :], in_=tm.t[:], func=AF.Identity, bias=MOD[l][seg].t[:, 48 + k:49 + k], scale=A2[l][seg].t[:, k:k + 1]),
                                 [tm, MOD[l][seg], A2[l][seg]], [hb])
                        else:
                            h32 = h32r.next()
                            P.op("act", lambda e, k=k, tm=tm, seg=seg, h32=h32: e.activation(out=h32.t[:], in_=tm.t[:], func=AF.Identity, bias=MOD[l][seg].t[:, 48 + k:49 + k], scale=A2[l][seg].t[:, k:k + 1]),
                                 [tm, MOD[l][seg], A2[l][seg]], [h32])
                            P.op("dve", lambda e, k=k, h32=h32: e.tensor_copy(out=hb.t[:, k, :], in_=h32.t[:]), [h32], [hb])
                            P.op("pe", lambda e, k=k, h32=h32: e.matmul(psl.t[0:NE, :], wr32.t[:, k, :], h32.t[:], start=(k == 0), stop=(k == KC - 1)), [h32, wr32], [psl])
                    if moe:
                        P.op("dve", lambda e: e.tensor_copy(out=gT.t[:], in_=psl.t[0:NE, :]), [psl], [gT])
                        ps = psr.next()
                        for s in range(4):
                            P.op("pe", lambda e, s=s, ps=ps: e.transpose(ps.t[:, s * NE:(s + 1) * NE], gT.t[0:NE, s * 128:(s + 1) * 128], identF.t[0:NE, 0:NE]), [gT, identF], [ps])
                        P.op("dve", lambda e, ps=ps: e.tensor_copy(out=lg.t[:], in_=ps.t[:, 0:4 * NE].rearrange("p (s n) -> p s n", n=NE)), [ps], [lg])
                        for s in range(4):
                            P.op("dve", lambda e, s=s: e.tensor_reduce(out=sm.t[:, s:s + 1], in_=lg.t[:, s, :], axis=AX.X, op=ALU.max), [lg], [sm])
                            P.op("dve", lambda e, s=s: e.tensor_scalar(out=l2.t[:, s, :], in0=lg.t[:, s, :], scalar1=sm.t[:, s:s + 1], scalar2=-1.0e30, op0=ALU.is_equal, op1=ALU.mult), [lg, sm], [l2])
                            P.op("dve", lambda e, s=s: e.tensor_tensor(out=l2.t[:, s, :], in0=l2.t[:, s, :], in1=lg.t[:, s, :], op=ALU.add), [l2, lg], [l2])
                            P.op("dve", lambda e, s=s: e.tensor_reduce(out=sm.t[:, 4 + s:5 + s], in_=l2.t[:, s, :], axis=AX.X, op=ALU.max), [l2], [sm])
                            P.op("dve", lambda e, s=s: e.tensor_scalar_mul(out=sm.t[:, 8 + s:9 + s], in0=sm.t[:, s:s + 1], scalar1=-1.0), [sm], [sm])
                            P.op("act", lambda e, s=s: e.activation(out=gts.t[:, s, :], in_=lg.t[:, s, :], func=AF.Exp, bias=sm.t[:, 8 + s:9 + s]), [lg, sm], [gts])
                            P.op("dve", lambda e, s=s: e.scalar_tensor_tensor(out=gts.t[:, s, :], in0=lg.t[:, s, :], scalar=sm.t[:, 4 + s:5 + s], in1=gts.t[:, s, :], op0=ALU.is_ge, op1=ALU.mult), [lg, sm, gts], [gts])
                            P.op("dve", lambda e, s=s: e.tensor_reduce(out=sm.t[:, 12 + s:13 + s], in_=gts.t[:, s, :], axis=AX.X, op=ALU.add), [gts], [sm])
                            P.op("dve", lambda e, s=s: e.reciprocal(out=sm.t[:, 12 + s:13 + s], in_=sm.t[:, 12 + s:13 + s]), [sm], [sm])
                            P.op("dve", lambda e, s=s: e.tensor_scalar_mul(out=gts.t[:, s, :], in0=gts.t[:, s, :], scalar1=sm.t[:, 12 + s:13 + s]), [gts, sm], [gts])
                        ps = psr.next()
                        for s in range(4):
                            P.op("pe", lambda e, s=s, ps=ps: e.transpose(ps.t[0:NE, s * 128:(s + 1) * 128], gts.t[:, s, :], identF.t[:]), [gts, identF], [ps])
                        P.op("dve", lambda e, ps=ps: e.tensor_copy(out=gT.t[:], in_=ps.t[0:NE, :]), [ps], [gT])
                        for e_ in range(NE):
                            ps = psr.next()
                            P.op("pe", lambda e, e_=e_, ps=ps: e.matmul(ps.t[:, :], SEL.t[0:NE, e_, :], gT.t[:], start=True, stop=True), [SEL, gT], [ps])
                            P.op("act", lambda e, e_=e_, ps=ps: e.copy(out=gbt.t[:, e_, :], in_=ps.t[:, :]), [ps], [gbt])
                    for ex in range(NE if moe else 1):
                        exi = (1 + ex) if moe else 0
                        wsb = WSe if moe else WSd
                        for fg in range(FC // 2):
                            wg_t = wgu.next()
                            wu_t = wgu.next()
                            P.dma("sp", wg_t.t[:], WGS[exi, fg].rearrange("p (k n) -> p k n", n=256), [wsb], [wg_t])
                            P.dma("sp", wu_t.t[:], WUS[exi, fg].rearrange("p (k n) -> p k n", n=256), [wsb], [wu_t])
                            for ff in range(2):
                                fc = fg * 2 + ff
                                pg = psr.next()
                                pu = psr.next()
                                for k in range(KC):
                                    P.op("pe", lambda e, k=k, ff=ff, wg_t=wg_t, pg=pg: e.matmul(pg.t[:, :], wg_t.t[:, k, ff * 128:(ff + 1) * 128], hb.t[:, k, :], start=(k == 0), stop=(k == KC - 1)), [wg_t, hb], [pg])
                                for k in range(KC):
                                    P.op("pe", lambda e, k=k, ff=ff, wu_t=wu_t, pu=pu: e.matmul(pu.t[:, :], wu_t.t[:, k, ff * 128:(ff + 1) * 128], hb.t[:, k, :], start=(k == 0), stop=(k == KC - 1)), [wu_t, hb], [pu])
                                tm = tmpr.next()
                                P.op("act", lambda e, pg=pg, tm=tm: e.activation(out=tm.t[:], in_=pg.t[:, :], func=AF.Silu), [pg], [tm])
                                if moe:
                                    tm2 = tmpr.next()
                                    P.op("dve", lambda e, pu=pu, tm=tm, tm2=tm2: e.tensor_tensor(out=tm2.t[:], in0=pu.t[:, :], in1=tm.t[:], op=ALU.mult), [pu, tm], [tm2])
                                    P.op("dve", lambda e, fc=fc, tm2=tm2, ex=ex: e.tensor_tensor(out=act.t[:, fc, :], in0=tm2.t[:], in1=gbt.t[:, ex, :], op=ALU.mult), [tm2, gbt], [act])
                                else:
                                    P.op("dve", lambda e, fc=fc, pu=pu, tm=tm: e.tensor_tensor(out=act.t[:, fc, :], in0=pu.t[:, :], in1=tm.t[:], op=ALU.mult), [pu, tm], [act])
                        for dc in range(KC):
                            wd_t = wdr.next()
                            P.dma("sp", wd_t.t[:], WDS[exi, dc].rearrange("p (f n) -> p f n", n=128), [wsb], [wd_t])
                            ps = psr.next()
                            for fc in range(FC):
                                P.op("pe", lambda e, fc=fc, wd_t=wd_t, ps=ps: e.matmul(ps.t[:, :], wd_t.t[:, fc, :], act.t[:, fc, :], start=(fc == 0), stop=(fc == FC - 1)), [wd_t, act], [ps])
                            P.op("dve", lambda e, dc=dc, ps=ps, seg=seg: e.scalar_tensor_tensor(out=XS.t[:, dc, :], in0=ps.t[:, :], scalar=MOD[l][seg].t[:, 80 + dc:81 + dc], in1=XS.t[:, dc, :],
                                                                                     op0=ALU.mult, op1=ALU.add), [ps, MOD[l][seg], XS], [XS])
                    if not last:
                        P.dma("sp", XT[blk], XS.t[:], [XS], [XTb[blk]])
                    elif seg == 1 or blk * T < OWN:
                        rstd_from_sq([(XS.t[:, k, :], XS) for k in range(KC)], KC, Rb, sqr, EPS * D)
                        go = 24 * DEPTH
                        for k in range(KC):
                            tm = tmpr.next()
                            P.op("dve", lambda e, k=k, tm=tm: e.tensor_tensor(out=tm.t[:], in0=XS.t[:, k, :], in1=Rb.t[:], op=ALU.mult), [XS, Rb], [tm])
                            P.op("act", lambda e, k=k, tm=tm, go=go: e.activation(out=XS.t[:, k, :], in_=tm.t[:], func=AF.Identity, scale=GL.t[:, go + k:go + k + 1]), [tm, GL, XS], [XS])
                        yo = act.t[:, 0:16, :].rearrange("p a b -> p (a b)").bitcast(F32)
                        orow = (blk * T) if seg == 0 else (OWN + blk * T - S_P)
                        for s in range(4):
                            half = s % 2
                            for k4 in range(4):
                                ps = psr.next()
                                for kk in range(4):
                                    k = k4 * 4 + kk
                                    P.op("pe", lambda e, k=k, kk=kk, s=s, ps=ps: e.transpose(ps.t[:, kk * 128:(kk + 1) * 128], XS.t[:, k, s * 128:(s + 1) * 128], identF.t[:]), [XS, identF], [ps])
                                P.op("act" if k4 % 2 else "dve", lambda e, k4=k4, half=half, ps=ps: (e.copy if e is nc.scalar else e.tensor_copy)(out=yo[:, half * 2048 + k4 * 512:half * 2048 + (k4 + 1) * 512], in_=ps.t[:, :]), [ps], [act])
                            P.dma("sp", y_out[orow + s * 128:orow + (s + 1) * 128, :], yo[:, half * 2048:(half + 1) * 2048], [act], [Yb])
                P.end_phase()

        P._wait_all(P.E["sp"], {Yb.dsemname: Yb.dcnt})
    return nc


def _host_tables(S_P, S_S, shift):
    import jax
    import jax.numpy as jnp
    cpu = jax.devices("cpu")[0]
    with jax.default_device(cpu):
        def rope(S):
            pos = jnp.arange(S, dtype=jnp.float32)
            inv = 1.0 / (10000.0 ** (jnp.arange(0, 64, 2, dtype=jnp.float32) / 64))
            ang = pos[:, None] * inv[None, :]
            return np.asarray(jnp.cos(ang)), np.asarray(jnp.sin(ang))
        cP, sP = rope(S_P)
        cS, sS = rope(S_S)
        rel = jnp.arange(-255, 256, dtype=jnp.int32)
        nb = 16
        ret = (rel > 0).astype(jnp.int32) * nb
        n = jnp.abs(rel)
        max_exact = nb // 2
        nf = jnp.maximum(n, 1).astype(jnp.float32)
        large = max_exact + (jnp.log(nf / max_exact) / math.log(128 / max_exact) * (nb - max_exact)).astype(jnp.int32)
        large = jnp.minimum(large, nb - 1)
        bucket = np.asarray(ret + jnp.where(n < max_exact, n, large))
    relv = np.arange(-255, 256)
    idx = (np.arange(S_P) + shift) % S_P
    cos = np.concatenate([cP[idx], cS], 0).T
    sin = np.concatenate([sP[idx], sS], 0).T
    rope_t = np.zeros((64, 2, S_P + S_S), np.float32)
    rope_t[0:32, 0] = cos
    rope_t[32:64, 0] = cos
    rope_t[0:32, 1] = -sin
    rope_t[32:64, 1] = sin
    nbP, nbS = S_P // 128, S_S // 128
    em = np.zeros((128, 2 * (nbP + nbS)), np.float32)
    for b in range(nbP):
        gb = (b + shift // 128) % nbP
        if gb == 0:
            em[:, 2 * b] = NEG
        if gb == nbP - 1:
            em[:, 2 * b + 1] = NEG
    em[:, 2 * nbP] = NEG
    em[:, 2 * (nbP + nbS) - 1] = NEG
    oh = np.zeros((33, 512), np.float32)
    for kp in range(511):
        r = 255 - kp
        if abs(r) <= 128:
            oh[bucket[r + 255], kp] = 1.0
        else:
            oh[32, kp] = 1.0
    oh[32, 511] = 1.0
    return rope_t, em, oh


_PERM = np.concatenate([np.arange(32, 64), np.arange(0, 32)])


def make_in_maps(inputs, cfg, n_cores=8):
    S_P, S_S, OWN = cfg["S_P"], cfg["S_S"], cfg["OWN"]
    f = lambda a: np.ascontiguousarray(np.asarray(a, dtype=np.float32))
    shared = {k: f(inputs[k]) for k in ("rel_bias", "w_ada", "b_ada", "g_norm_mix", "g_norm_ffn", "w_in", "g_q_lat", "w_uq", "g_kv_lat", "w_ukv",
                                         "sink", "g_out_a", "g_out_b", "w_out", "w_gate_d", "w_up_d", "w_down_d", "w_router", "w_gate_e", "w_up_e",
                                         "w_down_e", "g_final")}
    w_in = shared["w_in"]
    shared["w_krs"] = np.ascontiguousarray(w_in[:, :, 1024:1088][:, :, _PERM])
    wq = shared["w_uq"].reshape(w_in.shape[0], 512, 8, 192)
    shared["w_uqs"] = np.ascontiguousarray(wq[:, :, :, 128:192][:, :, :, _PERM].reshape(w_in.shape[0], 512, 512))
    shared["ident_in"] = np.eye(128, dtype=np.float32)
    xp, xs = f(inputs["x_prompt"]), f(inputs["x_sample"])
    cp, csmp = f(inputs["c_prompt"]), f(inputs["c_sample"])
    per_group = n_cores // xp.shape[0]
    maps = []
    for c in range(n_cores):
        b = c // per_group
        r = c % per_group
        shift = r * OWN
        idx = (np.arange(S_P) + shift) % S_P
        rope_t, em, oh = _host_tables(S_P, S_S, shift)
        m = dict(shared)
        m["x_in"] = np.ascontiguousarray(np.concatenate([xp[b][idx], xs[c]], 0))
        m["c_in"] = np.ascontiguousarray(np.stack([cp[b], csmp[c]], 0))
        m["rope_in"] = rope_t
        m["em_in"] = em
        m["oh_in"] = oh
        maps.append(m)
    return maps


CFG_FULL = dict(S_P=16384, S_S=2048, OWN=4096, FF=5632, NE=8, DEPTH=2)


def kernel(**inputs):
    return run(inputs, CFG_FULL)


def run(inputs, cfg):
    nc = build(cfg)
    maps = make_in_maps(inputs, cfg)
    res = run_bass_kernel_spmd(nc, maps, core_ids=list(range(8)))
    S_P, S_S, OWN = cfg["S_P"], cfg["S_S"], cfg["OWN"]
    yp = np.zeros((2, S_P, D), np.float32)
    ys = np.zeros((8, S_S, D), np.float32)
    for c in range(8):
        y = np.asarray(res.results[c]["y_out"])
        b, r = c // 4, c % 4
        yp[b, r * OWN:(r + 1) * OWN] = y[0:OWN]
        ys[c] = y[OWN:OWN + S_S]
    return (yp, ys)
```

```python
import math
from contextlib import ExitStack
import numpy as np
import concourse.bass as bass
import concourse.mybir as mybir
from concourse.bass_utils import run_bass_kernel_spmd

F32 = mybir.dt.float32
BF16 = mybir.dt.bfloat16
AF = mybir.ActivationFunctionType
ALU = mybir.AluOpType
AX = mybir.AxisListType

D = 2048
KC = 16
EPS = 1e-6
H = 8
MLA_SCALE = 1.0 / math.sqrt(192.0)
GQA_SCALE = 1.0 / math.sqrt(128.0)
NEG = -1.0e4
T = 512


class Buf:
    def __init__(self, name, t=None):
        self.name = name
        self.t = t
        self.w = {}
        self.r = {}
        self.dsem = None
        self.dsemname = None
        self.dcnt = 0
        self.scoped = False


class Eng:
    def __init__(self, name, obj, sem, semname):
        self.name, self.obj, self.sem, self.semname = name, obj, sem, semname
        self.cnt = 0
        self.seen = {}


class Prog:
    def __init__(self, nc, es):
        self.nc, self.es = nc, es
        self.sems = {}
        self.E = {}
        for name, obj in (("pe", nc.tensor), ("act", nc.scalar), ("dve", nc.vector), ("pool", nc.gpsimd), ("sp", nc.sync)):
            sn = "c_" + name
            self.E[name] = Eng(name, obj, self.newsem(sn), sn)
        self.dbufs = []
        self.free_dsems = []
        self.nuid = 0

    def newsem(self, name):
        s = self.es.enter_context(self.nc.semaphore(name))
        self.sems[name] = s
        return s

    def tile(self, name, shape, dt, scope=None):
        self.nuid += 1
        t = (scope or self.es).enter_context(self.nc.sbuf_tensor(f"{name}_{self.nuid}", list(shape), dt))
        b = Buf(name, t)
        b.scoped = scope is not None
        return b

    def _wait(self, E, deps, defer_last=False):
        need = []
        for sn, val in deps.items():
            if E.seen.get(sn, 0) >= val:
                continue
            if sn == E.semname and E.name == "pe":
                continue
            need.append((sn, val))
            E.seen[sn] = val
        last = None
        if defer_last and need:
            last = need.pop()
        for sn, val in need:
            E.obj.wait_ge(self.sems[sn], val)
        return last

    @staticmethod
    def _merge(d, s, skip=None):
        for k, v in s.items():
            if k != skip and d.get(k, 0) < v:
                d[k] = v

    def op(self, e, fn, reads=(), writes=()):
        E = self.E[e]
        deps = {}
        for b in reads:
            self._merge(deps, b.w)
        for b in writes:
            self._merge(deps, b.w)
            self._merge(deps, b.r)
        last = self._wait(E, deps, defer_last=True)
        ins = fn(E.obj)
        if last is not None:
            ins._wait_ge(self.sems[last[0]], last[1])
        E.cnt += 1
        ins.then_inc(E.sem, 1)
        for b in writes:
            b.w[E.semname] = E.cnt
        for b in reads:
            b.r[E.semname] = E.cnt
        return ins

    def dma(self, q, out, in_, reads, writes, **kw):
        E = self.E[q]
        prim = writes[0]
        if prim.dsem is None:
            if self.free_dsems:
                prim.dsemname, prim.dsem, prim.dcnt = self.free_dsems.pop()
            else:
                self.nuid += 1
                prim.dsemname = f"d_{self.nuid}"
                prim.dsem = self.newsem(prim.dsemname)
            self.dbufs.append(prim)
        deps = {}
        for b in reads:
            self._merge(deps, b.w)
        for b in writes:
            self._merge(deps, b.r)
            self._merge(deps, b.w, skip=b.dsemname)
        last = self._wait(E, deps, defer_last=True)
        ins = E.obj.dma_start(out=out, in_=in_, **kw)
        if last is not None:
            ins._wait_ge(self.sems[last[0]], last[1])
        prim.dcnt += 16
        ins.then_inc(prim.dsem, 16)
        for b in writes:
            b.w[prim.dsemname] = prim.dcnt
        for b in reads:
            b.r[prim.dsemname] = prim.dcnt

    def barrier(self):
        deps = {}
        for E in self.E.values():
            if E.cnt:
                deps[E.semname] = E.cnt
        for b in self.dbufs:
            if b.dcnt:
                deps[b.dsemname] = b.dcnt
        for E in self.E.values():
            d = dict(deps)
            self._wait_all(E, d)

    def end_phase(self):
        self.barrier()
        keep = []
        for b in self.dbufs:
            if getattr(b, "scoped", False):
                self.free_dsems.append((b.dsemname, b.dsem, b.dcnt))
            else:
                keep.append(b)
        self.dbufs = keep

    def _wait_all(self, E, deps):
        for sn, val in deps.items():
            if E.seen.get(sn, 0) >= val:
                continue
            E.obj.wait_ge(self.sems[sn], val)
            E.seen[sn] = val


class Rot:
    def __init__(self, bufs):
        self.bufs, self.i = bufs, 0

    def next(self):
        b = self.bufs[self.i % len(self.bufs)]
        self.i += 1
        return b


def build(cfg):
    S_P, S_S, OWN, FF, NE, DEPTH = cfg["S_P"], cfg["S_S"], cfg["OWN"], cfg["FF"], cfg["NE"], cfg["DEPTH"]
    FC = FF // 128
    NTOK = S_P + S_S
    NBLK = NTOK // T
    segs = (("P", 0, S_P), ("S", S_P, S_S))
    nc = bass.Bass("TRN2", target_bir_lowering=False)

    def din(name, shape, dt=F32):
        return nc.dram_tensor(name, list(shape), dt, kind="ExternalInput").ap()

    def dscr(name, shape, dt):
        return nc.dram_tensor(name, list(shape), dt, kind="Internal").ap()

    x_in = din("x_in", [NTOK, D])
    c_in = din("c_in", [2, D])
    rope_in = din("rope_in", [64, 2, NTOK])
    em_in = din("em_in", [128, 2 * (NTOK // 128)])
    ident_in = din("ident_in", [128, 128])
    oh_in = din("oh_in", [33, 512])
    rel_bias = din("rel_bias", [32, 8])
    w_ada = din("w_ada", [DEPTH, D, 6 * D])
    b_ada = din("b_ada", [DEPTH, 6 * D])
    g_norm_mix = din("g_norm_mix", [DEPTH, D])
    g_norm_ffn = din("g_norm_ffn", [DEPTH, D])
    w_in = din("w_in", [DEPTH, D, 2624])
    w_krs = din("w_krs", [DEPTH, D, 64])
    g_q_lat = din("g_q_lat", [DEPTH, 512])
    w_uq = din("w_uq", [DEPTH, 512, 1536])
    w_uqs = din("w_uqs", [DEPTH, 512, 512])
    g_kv_lat = din("g_kv_lat", [DEPTH, 512])
    w_ukv = din("w_ukv", [DEPTH, 512, 2048])
    sink = din("sink", [DEPTH, 8])
    g_out_a = din("g_out_a", [DEPTH, 1024])
    g_out_b = din("g_out_b", [DEPTH, 1024])
    w_out = din("w_out", [DEPTH, D, D])
    w_gate_d = din("w_gate_d", [1, D, FF])
    w_up_d = din("w_up_d", [1, D, FF])
    w_down_d = din("w_down_d", [1, FF, D])
    w_router = din("w_router", [1, D, NE])
    w_gate_e = din("w_gate_e", [1, NE, D, FF])
    w_up_e = din("w_up_e", [1, NE, D, FF])
    w_down_e = din("w_down_e", [1, NE, FF, D])
    g_final = din("g_final", [D])
    NOUT = OWN + S_S
    y_out = nc.dram_tensor("y_out", [NOUT, D], F32, kind="ExternalOutput").ap()

    XT = dscr("XT", [NBLK, 128, KC, T], F32)
    QT = dscr("QT", [H, 192, NTOK], BF16)
    LAT = dscr("LAT", [576, NTOK], BF16)
    GQ = dscr("GQ", [H, 128, NTOK], BF16)
    GK = dscr("GK", [2, 128, NTOK], BF16)
    GV = dscr("GV", [NTOK, 256], BF16)
    OA = dscr("OA", [H, 128, NTOK], BF16)
    OB = dscr("OB", [H, 128, NTOK], BF16)
    TB = dscr("TB", [8, 128, 512], F32)
    NEX = 1 + NE
    WGS = dscr("WGS", [NEX, FC // 2, 128, KC * 256], BF16)
    WUS = dscr("WUS", [NEX, FC // 2, 128, KC * 256], BF16)
    WDS = dscr("WDS", [NEX, KC, 128, FC * 128], BF16)

    es = ExitStack()
    with es:
        P = Prog(nc, es)
        XTb = [Buf(f"XT{i % 6}") for i in range(6)] * (NBLK // 6 + 1)
        QTb, LATb, GQb, GKb, GVb, OAb, OBb, TBb, Yb = (Buf(n) for n in ("QT", "LAT", "GQ", "GK", "GV", "OA", "OB", "TB", "Y"))
        CONST = Buf("const")
        WSd, WSe = Buf("WSd"), Buf("WSe")
        conv = []
        for ex in range(NEX):
            Wg_ = w_gate_d[0] if ex == 0 else w_gate_e[0][ex - 1]
            Wu_ = w_up_d[0] if ex == 0 else w_up_e[0][ex - 1]
            Wd_ = w_down_d[0] if ex == 0 else w_down_e[0][ex - 1]
            wb = WSd if ex == 0 else WSe
            for fg in range(FC // 2):
                conv.append((WGS[ex, fg].rearrange("p (k n) -> p k n", n=256), Wg_[:, fg * 256:(fg + 1) * 256].rearrange("(k p) n -> p k n", p=128), wb))
                conv.append((WUS[ex, fg].rearrange("p (k n) -> p k n", n=256), Wu_[:, fg * 256:(fg + 1) * 256].rearrange("(k p) n -> p k n", p=128), wb))
            for dc in range(KC):
                conv.append((WDS[ex, dc].rearrange("p (f n) -> p f n", n=128), Wd_[:, dc * 128:(dc + 1) * 128].rearrange("(f p) n -> p f n", p=128), wb))
        conv.reverse()
        conv_per_blk = -(-len(conv) // NBLK)

        def issue_conv(n):
            for _ in range(n):
                if conv:
                    d_, s_, wb_ = conv.pop()
                    P.dma("pool", d_, s_, [CONST], [wb_])

        PS = []
        for i in range(8):
            t = es.enter_context(nc.psum_tensor(f"ps{i}", [128, 512], F32))
            PS.append(Buf(f"ps{i}", t))
        psr = Rot(PS)

        identF = P.tile("identF", [128, 128], F32)
        onesB = P.tile("onesB", [128, 128], BF16)
        onesF = P.tile("onesF", [128, 128], F32)
        vecA = [P.tile(f"vecA{l}", [128, 128], F32) for l in range(DEPTH)]
        vecB = P.tile("vecB", [128, 64], F32)
        MOD = [[P.tile(f"mod{l}{s}", [128, 96], F32) for s in range(2)] for l in range(DEPTH)]
        A1 = [[P.tile(f"a1{l}{s}", [128, 16], F32) for s in range(2)] for l in range(DEPTH)]
        A2 = [[P.tile(f"a2{l}{s}", [128, 16], F32) for s in range(2)] for l in range(DEPTH)]
        GL = P.tile("GL", [128, 64], F32)
        BT = [[P.tile(f"bt{kv}{jb}", [128, 4, 128], F32) for jb in range(3)] for kv in range(2)]
        EM = P.tile("EM", [128, 2 * (NTOK // 128)], F32)
        ESK = [P.tile(f"esk{l}", [128, 8], F32) for l in range(DEPTH)]
        SEL = P.tile("SEL", [8, NE, 128], F32)

        EPSB = P.tile("EPSB", [128, 3], F32)
        for i_, v_ in enumerate((EPS * D, EPS * 512, EPS * 1024)):
            P.op("dve", lambda e, i_=i_, v_=v_: e.memset(EPSB.t[:, i_:i_ + 1], v_), [], [EPSB])

        def epsb(v):
            i_ = {EPS * D: 0, EPS * 512: 1, EPS * 1024: 2}[v]
            return EPSB.t[:, i_:i_ + 1]
        P.dma("sp", identF.t[:], ident_in[:, :], [CONST], [identF])
        P.dma("sp", EM.t[:], em_in[:, :], [CONST], [EM])
        P.op("dve", lambda e: e.memset(onesB.t[:], 1.0), [], [onesB])
        P.op("dve", lambda e: e.memset(onesF.t[:], 1.0), [], [onesF])

        def transpose_rows(scope, rows_aps, out_buf, ncols):
            st = P.tile("stg", [128, 128], F32, scope)
            P.op("dve", lambda e: e.memset(st.t[:], 0.0), [], [st])
            r0 = 0
            for ap, r in rows_aps:
                P.dma("sp", st.t[r0:r0 + r, :], ap, [CONST], [st])
                r0 += r
            ps = psr.next()
            P.op("pe", lambda e: e.transpose(ps.t[:, 0:128], st.t[:], identF.t[:]), [st, identF], [ps])
            P.op("dve", lambda e: e.tensor_copy(out=out_buf.t[:, 0:ncols], in_=ps.t[:, 0:ncols]), [ps], [out_buf])

        def bcast_row(src_ap, n, out_ap, out_buf, scope, func=None):
            row = P.tile("row", [1, n], F32, scope)
            P.dma("sp", row.t[:], src_ap, [CONST], [row])
            ps = psr.next()
            P.op("pe", lambda e: e.matmul(ps.t[:, 0:n], onesF.t[0:1, :], row.t[:], start=True, stop=True), [onesF, row], [ps])
            if func is None:
                P.op("dve", lambda e: e.tensor_copy(out=out_ap, in_=ps.t[:, 0:n]), [ps], [out_buf])
            else:
                P.op("act", lambda e: e.activation(out=out_ap, in_=ps.t[:, 0:n], func=func), [ps], [out_buf])

        with ExitStack() as sc:
            for l in range(DEPTH):
                transpose_rows(sc, [(b_ada[l].rearrange("(c p) -> c p", p=128), 96),
                                    (g_norm_mix[l].rearrange("(c p) -> c p", p=128), 16),
                                    (g_norm_ffn[l].rearrange("(c p) -> c p", p=128), 16)], vecA[l], 128)
            rows = []
            for l in range(DEPTH):
                rows += [(g_q_lat[l].rearrange("(c p) -> c p", p=128), 4), (g_kv_lat[l].rearrange("(c p) -> c p", p=128), 4),
                         (g_out_a[l].rearrange("(c p) -> c p", p=128), 8), (g_out_b[l].rearrange("(c p) -> c p", p=128), 8)]
            rows += [(g_final.rearrange("(c p) -> c p", p=128), 16)]
            transpose_rows(sc, rows, vecB, 24 * DEPTH + 16)
            for l in range(DEPTH):
                o = 24 * l
                P.op("dve", lambda e, o=o: e.tensor_scalar_mul(out=GL.t[:, o:o + 8], in0=vecB.t[:, o:o + 8], scalar1=math.sqrt(512.0)), [vecB], [GL])
                P.op("dve", lambda e, o=o: e.tensor_scalar_mul(out=GL.t[:, o + 8:o + 24], in0=vecB.t[:, o + 8:o + 24], scalar1=math.sqrt(1024.0)), [vecB], [GL])
            o = 24 * DEPTH
            P.op("dve", lambda e: e.tensor_scalar_mul(out=GL.t[:, o:o + 16], in0=vecB.t[:, o:o + 16], scalar1=math.sqrt(float(D))), [vecB], [GL])

            for l in range(DEPTH):
                bcast_row(sink[l:l + 1, :], 8, ESK[l].t[:, :], ESK[l], sc, func=AF.Exp)
            for e_ in range(NE):
                P.op("dve", lambda e, e_=e_: e.tensor_scalar_mul(out=SEL.t[0:8, e_, :], in0=onesF.t[0:8, :], scalar1=identF.t[0:8, e_:e_ + 1]),
                     [onesF, identF], [SEL])

            rb = P.tile("rb", [33, 8], F32, sc)
            P.op("dve", lambda e: e.memset(rb.t[:], NEG), [], [rb])
            P.dma("sp", rb.t[0:32, :], rel_bias[:, :], [CONST], [rb])
            ohs = P.tile("ohs", [33, 512], F32, sc)
            P.dma("sp", ohs.t[:], oh_in[:, :], [CONST], [ohs])
            onesF33 = P.tile("ones33", [33, 128], F32, sc)
            P.op("dve", lambda e: e.memset(onesF33.t[:], 1.0), [], [onesF33])
            for h in range(8):
                lh = P.tile("lh", [33, 128], F32, sc)
                P.op("dve", lambda e, h=h, lh=lh: e.tensor_scalar_mul(out=lh.t[:], in0=onesF33.t[:], scalar1=rb.t[:, h:h + 1]), [onesF33, rb], [lh])
                ps = psr.next()
                P.op("pe", lambda e, lh=lh, ps=ps: e.matmul(ps.t[:, :], lh.t[:], ohs.t[:], start=True, stop=True), [lh, ohs], [ps])
                tr = P.tile("tr", [128, 512], F32, sc)
                P.op("dve", lambda e, ps=ps, tr=tr: e.tensor_copy(out=tr.t[:], in_=ps.t[:]), [ps], [tr])
                P.dma("sp", TB[h], tr.t[:], [tr], [TBb])
            for h in range(8):
                kv, g = h // 4, h % 4
                flat = TB[h].rearrange("p c -> (p c)")
                for jb in range(3):
                    cpr = 383 - 128 * jb
                    src = bass.AP(tensor=flat.tensor, offset=flat.offset + cpr, ap=[[511, 128], [1, 128]])
                    P.dma("sp", BT[kv][jb].t[:, g, :], src, [TBb], [BT[kv][jb]])

            cin = P.tile("cin", [2, D], F32, sc)
            P.dma("sp", cin.t[:], c_in[:, :], [CONST], [cin])
            csl = P.tile("csl", [2, D], F32, sc)
            P.op("act", lambda e: e.activation(out=csl.t[:], in_=cin.t[:], func=AF.Silu), [cin], [csl])
            csT = P.tile("csT", [128, KC, 2], BF16, sc)
            ps = psr.next()
            for k in range(KC):
                P.op("pe", lambda e, k=k: e.transpose(ps.t[:, 2 * k:2 * k + 2], csl.t[0:2, k * 128:(k + 1) * 128], identF.t[0:2, 0:2]), [csl, identF], [ps])
            P.op("dve", lambda e: e.tensor_copy(out=csT.t[:].rearrange("p k b -> p (k b)"), in_=ps.t[:, 0:2 * KC]), [ps], [csT])
            wa = Rot([P.tile(f"wa{i}", [128, KC, 512], BF16, sc) for i in range(3)])
            for l in range(DEPTH):
                psm = psr.next()
                for cg in range(24):
                    wt = wa.next()
                    P.dma("pool", wt.t[:], w_ada[l][:, cg * 512:(cg + 1) * 512].rearrange("(k p) n -> p k n", p=128), [CONST], [wt])
                    for oc in range(4):
                        occ = cg * 4 + oc
                        for k in range(KC):
                            P.op("pe", lambda e, k=k, oc=oc, occ=occ, wt=wt: e.matmul(psm.t[:, 2 * occ:2 * occ + 2], wt.t[:, k, oc * 128:(oc + 1) * 128], csT.t[:, k, :],
                                                                          start=(k == 0), stop=(k == KC - 1)), [wt, csT], [psm])
                pv = psm.t[:, 0:192].rearrange("p (c b) -> p c b", b=2)
                for s in range(2):
                    P.op("dve", lambda e, s=s, l=l: e.tensor_tensor(out=MOD[l][s].t[:], in0=pv[:, :, s], in1=vecA[l].t[:, 0:96], op=ALU.add), [psm, vecA[l]], [MOD[l][s]])
                    for (A, c0, g0) in ((A1, 16, 96), (A2, 64, 112)):
                        P.op("dve", lambda e, A=A, c0=c0, s=s, l=l: e.tensor_scalar(out=A[l][s].t[:], in0=MOD[l][s].t[:, c0:c0 + 16], scalar1=1.0, scalar2=math.sqrt(float(D)),
                                                                           op0=ALU.add, op1=ALU.mult), [MOD[l][s]], [A[l][s]])
                        P.op("dve", lambda e, A=A, g0=g0, s=s, l=l: e.tensor_tensor(out=A[l][s].t[:], in0=A[l][s].t[:], in1=vecA[l].t[:, g0:g0 + 16], op=ALU.mult), [A[l][s], vecA[l]], [A[l][s]])
            P.end_phase()

        def rstd_from_sq(srcs, nsq, Rb, sqr, eps_n):
            ps = psr.next()
            for i, (ap, b) in enumerate(srcs):
                sq = sqr.next()
                P.op("act", lambda e, ap=ap, sq=sq: e.activation(out=sq.t[:], in_=ap, func=AF.Square), [b], [sq])
                P.op("pe", lambda e, sq=sq, i=i: e.matmul(ps.t[:, :], onesB.t[:], sq.t[:], start=(i == 0), stop=(i == nsq - 1)), [onesB, sq], [ps])
            P.op("act", lambda e: e.activation(out=Rb.t[:], in_=ps.t[:], func=AF.Sqrt, bias=epsb(eps_n), scale=1.0), [ps, EPSB], [Rb])
            P.op("dve", lambda e: e.reciprocal(out=Rb.t[:], in_=Rb.t[:]), [Rb], [Rb])

        for l in range(DEPTH):
            last = (l == DEPTH - 1)
            if l == 0:
                own_blocks = list(range(NBLK))
            else:
                own_blocks = list(range(OWN // T)) + list(range(S_P // T, NBLK))
            own_set = set(own_blocks)

            with ExitStack() as sc:
                xin = Rot([P.tile(f"xin{i}", [128, D], F32, sc) for i in range(2)])
                XS = P.tile("XS", [128, KC, T], F32, sc)
                sqr = Rot([P.tile(f"sq{i}", [128, T], BF16, sc) for i in range(3)])
                Rb = P.tile("R", [128, T], F32, sc)
                tmpr = Rot([P.tile(f"tmp{i}", [128, T], F32, sc) for i in range(3)])
                hb = P.tile("h", [128, KC, T], BF16, sc)
                lat32 = P.tile("lat32", [128, 4, T], F32, sc)
                cqn = P.tile("cqn", [128, 4, T], BF16, sc)
                ckvn = P.tile("ckvn", [128, 4, T], BF16, sc)
                kr32 = P.tile("kr32", [64, 2, T], F32, sc)
                krb = P.tile("krb", [64, T], BF16, sc)
                cs = P.tile("cs", [64, 2, T], F32, sc)
                o1 = Rot([P.tile(f"o1{i}", [128, T], BF16, sc) for i in range(4)])
                gvst = P.tile("gvst", [128, 4, 256], BF16, sc)
                wp = Rot([P.tile(f"wp{i}", [128, KC, 512], BF16, sc) for i in range(3)])
                wuq = P.tile("wuq", [128, 4, 2048], BF16, sc)
                P.dma("pool", wuq.t[:, :, 0:1536], w_uq[l].rearrange("(k p) n -> p k n", p=128), [CONST], [wuq])
                P.dma("pool", wuq.t[:, :, 1536:2048], w_uqs[l].rearrange("(k p) n -> p k n", p=128), [CONST], [wuq])

                def wload(col0, ncol, extra=None):
                    wt = wp.next()
                    P.dma("pool", wt.t[:, :, 0:ncol], w_in[l][:, col0:col0 + ncol].rearrange("(k p) n -> p k n", p=128), [CONST], [wt])
                    if extra is not None:
                        P.dma("pool", wt.t[:, :, ncol:ncol + 64], extra.rearrange("(k p) n -> p k n", p=128), [CONST], [wt])
                    return wt

                def proj(wt, c0, m, ps_ap, ps):
                    for k in range(KC):
                        P.op("pe", lambda e, k=k: e.matmul(ps_ap, wt.t[:, k, c0:c0 + m], hb.t[:, k, :], start=(k == 0), stop=(k == KC - 1)), [wt, hb], [ps])

                for blk in range(NBLK):
                    seg = 0 if blk * T < S_P else 1
                    t0 = blk * T
                    need_q = blk in own_set
                    if blk > 0:
                        issue_conv(conv_per_blk)
                    if l == 0:
                        for s in range(4):
                            xt = xin.next()
                            P.dma("sp", xt.t[:], x_in[t0 + s * 128:t0 + (s + 1) * 128, :], [CONST], [xt])
                            for k4 in range(4):
                                ps = psr.next()
                                for kk in range(4):
                                    k = k4 * 4 + kk
                                    P.op("pe", lambda e, k=k, kk=kk, xt=xt, ps=ps: e.transpose(ps.t[:, kk * 128:(kk + 1) * 128], xt.t[:, k * 128:(k + 1) * 128], identF.t[:]), [xt, identF], [ps])
                                P.op("act" if k4 % 2 else "dve",
                                     lambda e, k4=k4, s=s, ps=ps: (e.copy if e is nc.scalar else e.tensor_copy)(out=XS.t[:, k4 * 4:(k4 + 1) * 4, s * 128:(s + 1) * 128],
                                                                                                       in_=ps.t[:, :].rearrange("p (k t) -> p k t", t=128)), [ps], [XS])
                        P.dma("sp", XT[blk], XS.t[:], [XS], [XTb[blk]])
                    else:
                        P.dma("sp", XS.t[:], XT[blk], [XTb[blk]], [XS])
                    P.dma("sp", cs.t[:], rope_in[:, :, t0:t0 + T], [CONST], [cs])
                    rstd_from_sq([(XS.t[:, k, :], XS) for k in range(KC)], KC, Rb, sqr, EPS * D)
                    for k in range(KC):
                        tm = tmpr.next()
                        P.op("dve", lambda e, k=k, tm=tm: e.tensor_tensor(out=tm.t[:], in0=XS.t[:, k, :], in1=Rb.t[:], op=ALU.mult), [XS, Rb], [tm])
                        P.op("act", lambda e, k=k, tm=tm: e.activation(out=hb.t[:, k, :], in_=tm.t[:], func=AF.Identity, bias=MOD[l][seg].t[:, k:k + 1], scale=A1[l][seg].t[:, k:k + 1]),
                             [tm, MOD[l][seg], A1[l][seg]], [hb])
                    for which in ((0, 1) if need_q else (1,)):
                        wt = wload(512 * which, 512)
                        for c in range(4):
                            ps = psr.next()
                            proj(wt, c * 128, 128, ps.t[:, :], ps)
                            P.op("act", lambda e, c=c, ps=ps: e.copy(out=lat32.t[:, c, :], in_=ps.t[:, :]), [ps], [lat32])
                        rstd_from_sq([(lat32.t[:, c, :], lat32) for c in range(4)], 4, Rb, sqr, EPS * 512)
                        dst = cqn if which == 0 else ckvn
                        go = 24 * l + 4 * which
                        for c in range(4):
                            tm = tmpr.next()
                            P.op("dve", lambda e, c=c, tm=tm: e.tensor_tensor(out=tm.t[:], in0=lat32.t[:, c, :], in1=Rb.t[:], op=ALU.mult), [lat32, Rb], [tm])
                            P.op("act", lambda e, c=c, tm=tm, dst=dst, go=go: e.activation(out=dst.t[:, c, :], in_=tm.t[:], func=AF.Identity, scale=GL.t[:, go + c:go + c + 1]), [tm, GL], [dst])
                    P.dma("sp", LAT[0:512, t0:t0 + T].rearrange("(c p) t -> p c t", p=128), ckvn.t[:], [ckvn], [LATb])
                    wt = wp.next()
                    P.dma("pool", wt.t[:, :, 0:64], w_in[l][:, 1024:1088].rearrange("(k p) n -> p k n", p=128), [CONST], [wt])
                    P.dma("pool", wt.t[:, :, 64:128], w_krs[l].rearrange("(k p) n -> p k n", p=128), [CONST], [wt])
                    P.dma("pool", wt.t[:, :, 128:512], w_in[l][:, 2112:2496].rearrange("(k p) n -> p k n", p=128), [CONST], [wt])
                    wt2 = wp.next()
                    P.dma("pool", wt2.t[:, :, 0:128], w_in[l][:, 2496:2624].rearrange("(k p) n -> p k n", p=128), [CONST], [wt2])
                    ps = psr.next()
                    proj(wt, 0, 64, ps.t[0:64, :], ps)
                    ps2 = psr.next()
                    proj(wt, 64, 64, ps2.t[0:64, :], ps2)
                    P.op("dve", lambda e, ps=ps: e.tensor_tensor(out=kr32.t[:, 0, :], in0=ps.t[0:64, :], in1=cs.t[:, 0, :], op=ALU.mult), [ps, cs], [kr32])
                    P.op("dve", lambda e, ps2=ps2: e.tensor_tensor(out=kr32.t[:, 1, :], in0=ps2.t[0:64, :], in1=cs.t[:, 1, :], op=ALU.mult), [ps2, cs], [kr32])
                    P.op("dve", lambda e: e.tensor_tensor(out=krb.t[:], in0=kr32.t[:, 0, :], in1=kr32.t[:, 1, :], op=ALU.add), [kr32], [krb])
                    P.dma("sp", LAT[512:576, t0:t0 + T], krb.t[:], [krb], [LATb])
                    for j in range(2):
                        ps = psr.next()
                        proj(wt, 128 + j * 128, 128, ps.t[:, :], ps)
                        ot = o1.next()
                        P.op("act", lambda e, ps=ps, ot=ot: e.copy(out=ot.t[:], in_=ps.t[:, :]), [ps], [ot])
                        P.dma("sp", GK[j, :, t0:t0 + T], ot.t[:], [ot], [GKb])
                    for s in range(4):
                        ps = psr.next()
                        for half, (wsrc, c0) in enumerate(((wt, 384), (wt2, 0))):
                            for k in range(KC):
                                P.op("pe", lambda e, k=k, s=s, half=half, wsrc=wsrc, c0=c0, ps=ps: e.matmul(ps.t[:, half * 128:(half + 1) * 128], hb.t[:, k, s * 128:(s + 1) * 128], wsrc.t[:, k, c0:c0 + 128],
                                                                                                start=(k == 0), stop=(k == KC - 1)), [hb, wsrc], [ps])
                        P.op("dve", lambda e, s=s, ps=ps: e.tensor_copy(out=gvst.t[:, s, :], in_=ps.t[:, 0:256]), [ps], [gvst])
                    P.dma("sp", GV[t0:t0 + T, :].rearrange("(s p) n -> p s n", p=128), gvst.t[:], [gvst], [GVb])
                    if not need_q:
                        continue
                    for half in range(2):
                        wt = wload(1088 + 512 * half, 512)
                        for j in range(4):
                            ps = psr.next()
                            proj(wt, j * 128, 128, ps.t[:, :], ps)
                            ot = o1.next()
                            P.op("act" if j % 2 else "dve", lambda e, ps=ps, ot=ot: (e.copy if e is nc.scalar else e.tensor_copy)(out=ot.t[:], in_=ps.t[:, :]), [ps], [ot])
                            P.dma("sp", GQ[half * 4 + j, :, t0:t0 + T], ot.t[:], [ot], [GQb])
                    for h in range(H):
                        ps = psr.next()
                        for c in range(4):
                            P.op("pe", lambda e, c=c, h=h, ps=ps: e.matmul(ps.t[:, :], wuq.t[:, c, h * 192:h * 192 + 128], cqn.t[:, c, :], start=(c == 0), stop=(c == 3)), [wuq, cqn], [ps])
                        ot = o1.next()
                        P.op("act", lambda e, ps=ps, ot=ot: e.copy(out=ot.t[:], in_=ps.t[:, :]), [ps], [ot])
                        P.dma("sp", QT[h, 0:128, t0:t0 + T], ot.t[:], [ot], [QTb])
                        ps = psr.next()
                        ps2 = psr.next()
                        for c in range(4):
                            P.op("pe", lambda e, c=c, h=h, ps=ps: e.matmul(ps.t[0:64, :], wuq.t[:, c, h * 192 + 128:h * 192 + 192], cqn.t[:, c, :], start=(c == 0), stop=(c == 3)), [wuq, cqn], [ps])
                        for c in range(4):
                            P.op("pe", lambda e, c=c, h=h, ps2=ps2: e.matmul(ps2.t[0:64, :], wuq.t[:, c, 1536 + h * 64:1536 + h * 64 + 64], cqn.t[:, c, :], start=(c == 0), stop=(c == 3)), [wuq, cqn], [ps2])
                        P.op("dve", lambda e, ps=ps: e.tensor_tensor(out=kr32.t[:, 0, :], in0=ps.t[0:64, :], in1=cs.t[:, 0, :], op=ALU.mult), [ps, cs], [kr32])
                        P.op("dve", lambda e, ps2=ps2: e.tensor_tensor(out=kr32.t[:, 1, :], in0=ps2.t[0:64, :], in1=cs.t[:, 1, :], op=ALU.mult), [ps2, cs], [kr32])
                        ot = o1.next()
                        P.op("dve", lambda e, ot=ot: e.tensor_tensor(out=ot.t[0:64, :], in0=kr32.t[:, 0, :], in1=kr32.t[:, 1, :], op=ALU.add), [kr32], [ot])
                        P.dma("sp", QT[h, 128:192, t0:t0 + T], ot.t[0:64, :], [ot], [QTb])
                issue_conv(len(conv))
                P.end_phase()

            with ExitStack() as sc:
                wukv = P.tile("wukv", [128, 4, 2048], BF16, sc)
                P.dma("pool", wukv.t[:], w_ukv[l].rearrange("(k p) n -> p k n", p=128), [CONST], [wukv])
                SMAX = max(S_P, S_S)
                krT = P.tile("krT", [64, SMAX], BF16, sc)
                KT = P.tile("KT", [128, SMAX], BF16, sc)
                VV = P.tile("VV", [128, SMAX // 128, 128], BF16, sc)
                latr = Rot([P.tile(f"lat{i}", [128, 4, T], BF16, sc) for i in range(3)])
                qnr = Rot([P.tile(f"qn{i}", [128, T], BF16, sc) for i in range(2)])
                qrr = Rot([P.tile(f"qr{i}", [64, T], BF16, sc) for i in range(2)])
                pr = Rot([P.tile(f"p{i}", [128, T], BF16, sc) for i in range(3)])
                rdr = Rot([P.tile(f"rd{i}", [128, T], F32, sc) for i in range(2)])
                obr = Rot([P.tile(f"ob{i}", [128, T], BF16, sc) for i in range(2)])
                accr = Rot([P.tile(f"acc{i}", [128, T], F32, sc) for i in range(4)])
                psS = Rot(PS[0:2])
                psO = Rot(PS[2:4])
                psD = Rot(PS[4:6])
                psK = Rot(PS[6:8])
                for (sname, s0, slen) in segs:
                    nq = (slen if (l == 0 or sname == "S") else OWN) // T
                    nkc = slen // 128
                    P.dma("sp", krT.t[:, 0:slen], LAT[512:576, s0:s0 + slen], [LATb], [krT])
                    for h in range(H):
                        for tc_ in range(slen // T):
                            lt = latr.next()
                            P.dma("sp", lt.t[:], LAT[0:512, s0 + tc_ * T:s0 + (tc_ + 1) * T].rearrange("(c p) t -> p c t", p=128), [LATb], [lt])
                            ps = psK.next()
                            for c in range(4):
                                P.op("pe", lambda e, c=c, h=h, lt=lt, ps=ps: e.matmul(ps.t[:, :], wukv.t[:, c, h * 256:h * 256 + 128], lt.t[:, c, :], start=(c == 0), stop=(c == 3)), [wukv, lt], [ps])
                            P.op("act", lambda e, ps=ps, tc_=tc_: e.copy(out=KT.t[:, tc_ * T:(tc_ + 1) * T], in_=ps.t[:, :]), [ps], [KT])
                            ps = psK.next()
                            for s in range(4):
                                for c in range(4):
                                    P.op("pe", lambda e, c=c, s=s, h=h, lt=lt, ps=ps: e.matmul(ps.t[:, s * 128:(s + 1) * 128], lt.t[:, c, s * 128:(s + 1) * 128], wukv.t[:, c, h * 256 + 128:h * 256 + 256],
                                                                                      start=(c == 0), stop=(c == 3)), [lt, wukv], [ps])
                            P.op("dve", lambda e, ps=ps, tc_=tc_: e.tensor_copy(out=VV.t[:, tc_ * 4:(tc_ + 1) * 4, :], in_=ps.t[:, :].rearrange("p (s d) -> p s d", d=128)), [ps], [VV])
                        for qb in range(nq):
                            q0 = s0 + qb * T
                            qn = qnr.next()
                            qr = qrr.next()
                            P.dma("sp", qn.t[:], QT[h, 0:128, q0:q0 + T], [QTb], [qn])
                            P.dma("sp", qr.t[:], QT[h, 128:192, q0:q0 + T], [QTb], [qr])
                            po = psO.next()
                            pd = psD.next()
                            accs = (accr.next(), accr.next())

                            def qk(kc, qn=qn, qr=qr):
                                ps = psS.next()
                                P.op("pe", lambda e: e.matmul(ps.t[:, :], KT.t[:, kc * 128:(kc + 1) * 128], qn.t[:], start=True, stop=False), [KT, qn], [ps])
                                P.op("pe", lambda e: e.matmul(ps.t[:, :], krT.t[:, kc * 128:(kc + 1) * 128], qr.t[:], start=False, stop=True), [krT, qr], [ps])
                                return ps
                            pscur = qk(0)
                            for kc in range(nkc):
                                psn = qk(kc + 1) if kc + 1 < nkc else None
                                pt = pr.next()
                                P.op("act", lambda e, pscur=pscur, pt=pt: e.activation(out=pt.t[:], in_=pscur.t[:, :], func=AF.Exp, scale=MLA_SCALE), [pscur], [pt])
                                P.op("pe", lambda e, kc=kc, pt=pt: e.matmul(po.t[:, :], VV.t[:, kc, :], pt.t[:], start=(kc == 0), stop=(kc == nkc - 1)), [VV, pt], [po])
                                acc = accs[kc % 2]
                                if kc < 2:
                                    P.op("dve", lambda e, pt=pt, acc=acc: e.tensor_copy(out=acc.t[:], in_=pt.t[:]), [pt], [acc])
                                else:
                                    P.op("dve", lambda e, pt=pt, acc=acc: e.tensor_tensor(out=acc.t[:], in0=acc.t[:], in1=pt.t[:], op=ALU.add), [pt, acc], [acc])
                                pscur = psn
                            P.op("pe", lambda e, pd=pd: e.matmul(pd.t[:, :], onesF.t[:], accs[0].t[:], start=True, stop=False), [onesF, accs[0]], [pd])
                            P.op("pe", lambda e, pd=pd: e.matmul(pd.t[:, :], onesF.t[:], accs[1].t[:], start=False, stop=True), [onesF, accs[1]], [pd])
                            rd = rdr.next()
                            ob = obr.next()
                            P.op("dve", lambda e, rd=rd, pd=pd: e.reciprocal(out=rd.t[:], in_=pd.t[:, :]), [pd], [rd])
                            P.op("dve", lambda e, rd=rd, ob=ob, po=po: e.tensor_tensor(out=ob.t[:], in0=po.t[:, :], in1=rd.t[:], op=ALU.mult), [po, rd], [ob])
                            P.dma("pool", OA[h, :, q0:q0 + T], ob.t[:], [ob], [OAb])
                P.end_phase()

            with ExitStack() as sc:
                SMAX = max(S_P, S_S)
                gkT = P.tile("gkT", [128, SMAX], BF16, sc)
                gvv = P.tile("gvv", [128, SMAX // 128, 128], BF16, sc)
                gqr = Rot([P.tile(f"gq{i}", [128, 4, T], BF16, sc) for i in range(2)])
                tmr = Rot([P.tile(f"wt{i}", [128, 4, 128], F32, sc) for i in range(3)])
                pr = Rot([P.tile(f"wp{i}", [128, 4, 128], BF16, sc) for i in range(3)])
                dnr = Rot([P.tile(f"dn{i}", [128, 4, 128], F32, sc) for i in range(2)])
                oor = Rot([P.tile(f"oo{i}", [128, 4, T], BF16, sc) for i in range(2)])
                psS = Rot(PS[0:3])
                psO = Rot(PS[3:5])
                psD = Rot(PS[5:7])
                for (sname, s0, slen) in segs:
                    nb = slen // 128
                    nqb = (slen if (l == 0 or sname == "S") else OWN) // 128
                    b0 = s0 // 128
                    for kv in range(2):
                        P.dma("sp", gkT.t[:, 0:slen], GK[kv, :, s0:s0 + slen], [GKb], [gkT])
                        P.dma("sp", gvv.t[:, 0:nb, :], GV[s0:s0 + slen, kv * 128:(kv + 1) * 128].rearrange("(n p) d -> p n d", p=128), [GVb], [gvv])
                        for n in range(nqb):
                            n4 = n % 4
                            if n4 == 0:
                                gq = gqr.next()
                                for g in range(4):
                                    P.dma("sp", gq.t[:, g, :], GQ[kv * 4 + g, :, s0 + n * 128:s0 + n * 128 + T], [GQb], [gq])
                                oo = oor.next()
                            po = psO.next()
                            pd = psD.next()
                            for jb in range(3):
                                m = (n + jb - 1) % nb
                                ps = psS.next()
                                P.op("pe", lambda e, m=m, n4=n4, gq=gq, ps=ps: e.matmul(ps.t[:, :].rearrange("p (g q) -> p g q", q=128), gkT.t[:, m * 128:(m + 1) * 128], gq.t[:, :, n4 * 128:(n4 + 1) * 128],
                                                                              start=True, stop=True), [gkT, gq], [ps])
                                tm = tmr.next()
                                P.op("dve", lambda e, ps=ps, tm=tm, jb=jb: e.scalar_tensor_tensor(out=tm.t[:], in0=ps.t[:, :].rearrange("p (g q) -> p g q", q=128), scalar=GQA_SCALE, in1=BT[kv][jb].t[:],
                                                                                               op0=ALU.mult, op1=ALU.add), [ps, BT[kv][jb]], [tm])
                                pt = pr.next()
                                if jb == 1:
                                    P.op("act", lambda e, tm=tm, pt=pt: e.activation(out=pt.t[:], in_=tm.t[:], func=AF.Exp), [tm], [pt])
                                else:
                                    col = 2 * (b0 + n) + (0 if jb == 0 else 1)
                                    P.op("act", lambda e, tm=tm, pt=pt, col=col: e.activation(out=pt.t[:], in_=tm.t[:], func=AF.Exp, bias=EM.t[:, col:col + 1]), [tm, EM], [pt])
                                P.op("pe", lambda e, m=m, pt=pt, jb=jb, po=po: e.matmul(po.t[:, :].rearrange("p (g q) -> p g q", q=128), gvv.t[:, m, :], pt.t[:], start=(jb == 0), stop=(jb == 2)), [gvv, pt], [po])
                                P.op("pe", lambda e, pt=pt, jb=jb, pd=pd: e.matmul(pd.t[:, :].rearrange("p (g q) -> p g q", q=128), onesB.t[:], pt.t[:], start=(jb == 0), stop=(jb == 2)), [onesB, pt], [pd])
                            dn = dnr.next()
                            for g in range(4):
                                P.op("dve", lambda e, g=g, dn=dn, pd=pd: e.tensor_scalar_add(out=dn.t[:, g, :], in0=pd.t[:, g * 128:(g + 1) * 128], scalar1=ESK[l].t[:, kv * 4 + g:kv * 4 + g + 1]), [pd, ESK[l]], [dn])
                            P.op("dve", lambda e, dn=dn: e.reciprocal(out=dn.t[:], in_=dn.t[:]), [dn], [dn])
                            P.op("dve", lambda e, dn=dn, po=po, oo=oo, n4=n4: e.tensor_tensor(out=oo.t[:, :, n4 * 128:(n4 + 1) * 128], in0=po.t[:, :].rearrange("p (g q) -> p g q", q=128), in1=dn.t[:], op=ALU.mult),
                                 [po, dn], [oo])
                            if n4 == 3:
                                for g in range(4):
                                    P.dma("pool", OB[kv * 4 + g, :, s0 + (n - 3) * 128:s0 + (n - 3) * 128 + T], oo.t[:, g, :], [oo], [OBb])
                P.end_phase()

            with ExitStack() as sc:
                XS = P.tile("XS4", [128, KC, T], F32, sc)
                sqr = Rot([P.tile(f"sq{i}", [128, T], BF16, sc) for i in range(3)])
                Rb = P.tile("R4", [128, T], F32, sc)
                tmpr = Rot([P.tile(f"tmp{i}", [128, T], F32, sc) for i in range(3)])
                hb = P.tile("h4", [128, KC, T], BF16, sc)
                act = P.tile("act", [128, max(FC, 16), T], BF16, sc)
                wgu = Rot([P.tile(f"wgu{i}", [128, KC, 256], BF16, sc) for i in range(4)])
                wdr = Rot([P.tile(f"wd{i}", [128, FC, 128], BF16, sc) for i in range(3)])
                moe = (l % 2 == 1)
                if moe:
                    gbt = P.tile("gb", [128, NE, T], BF16, sc)
                    h32r = Rot([P.tile(f"h32{i}", [128, T], F32, sc) for i in range(2)])
                    wr32 = P.tile("wr32", [128, KC, NE], F32, sc)
                    P.dma("sp", wr32.t[:], w_router[0].rearrange("(k p) n -> p k n", p=128), [CONST], [wr32])
                    lg = P.tile("lg", [128, 4, NE], F32, sc)
                    sm = P.tile("sm", [128, 16], F32, sc)
                    l2 = P.tile("l2", [128, 4, NE], F32, sc)
                    gts = P.tile("gts", [128, 4, NE], F32, sc)
                    gT = P.tile("gT", [8, T], F32, sc)
                OAt = act.t[:, 0:8, :]
                OBt = act.t[:, 8:16, :]

                for bi, blk in enumerate(own_blocks):
                    seg = 0 if blk * T < S_P else 1
                    t0 = blk * T
                    P.dma("sp", XS.t[:], XT[blk], [XTb[blk]], [XS])
                    P.dma("sp", OAt, OA[:, :, t0:t0 + T].rearrange("h p t -> p h t"), [OAb], [act])
                    P.dma("sp", OBt, OB[:, :, t0:t0 + T].rearrange("h p t -> p h t"), [OBb], [act])
                    for grp in range(2):
                        src = OAt if grp == 0 else OBt
                        ps = psr.next()
                        for c in range(8):
                            sq = sqr.next()
                            P.op("act", lambda e, c=c, sq=sq, src=src: e.activation(out=sq.t[:], in_=src[:, c, :], func=AF.Square), [act], [sq])
                            P.op("pe", lambda e, c=c, sq=sq, ps=ps: e.matmul(ps.t[:, :], onesB.t[:], sq.t[:], start=(c == 0), stop=(c == 7)), [onesB, sq], [ps])
                        P.op("act", lambda e, ps=ps: e.activation(out=Rb.t[:], in_=ps.t[:, :], func=AF.Sqrt, bias=epsb(EPS * 1024), scale=1.0), [ps, EPSB], [Rb])
                        P.op("dve", lambda e: e.reciprocal(out=Rb.t[:], in_=Rb.t[:]), [Rb], [Rb])
                        go = 24 * l + 8 + 8 * grp
                        for c in range(8):
                            tm = tmpr.next()
                            P.op("dve", lambda e, c=c, tm=tm, src=src: e.tensor_tensor(out=tm.t[:], in0=src[:, c, :], in1=Rb.t[:], op=ALU.mult), [act, Rb], [tm])
                            P.op("act", lambda e, c=c, tm=tm, go=go, grp=grp: e.activation(out=hb.t[:, grp * 8 + c, :], in_=tm.t[:], func=AF.Identity, scale=GL.t[:, go + c:go + c + 1]), [tm, GL], [hb])
                    for dg in range(8):
                        wt = wgu.next()
                        P.dma("pool", wt.t[:], w_out[l][:, dg * 256:(dg + 1) * 256].rearrange("(k p) n -> p k n", p=128), [CONST], [wt])
                        for dd in range(2):
                            dc = dg * 2 + dd
                            ps = psr.next()
                            for k in range(KC):
                                P.op("pe", lambda e, k=k, dd=dd, wt=wt, ps=ps: e.matmul(ps.t[:, :], wt.t[:, k, dd * 128:(dd + 1) * 128], hb.t[:, k, :], start=(k == 0), stop=(k == KC - 1)), [wt, hb], [ps])
                            P.op("dve", lambda e, dc=dc, ps=ps, seg=seg: e.scalar_tensor_tensor(out=XS.t[:, dc, :], in0=ps.t[:, :], scalar=MOD[l][seg].t[:, 32 + dc:33 + dc], in1=XS.t[:, dc, :],
                                                                                     op0=ALU.mult, op1=ALU.add), [ps, MOD[l][seg], XS], [XS])
                    rstd_from_sq([(XS.t[:, k, :], XS) for k in range(KC)], KC, Rb, sqr, EPS * D)
                    if moe:
                        psl = psr.next()
                    for k in range(KC):
                        tm = tmpr.next()
                        P.op("dve", lambda e, k=k, tm=tm: e.tensor_tensor(out=tm.t[:], in0=XS.t[:, k, :], in1=Rb.t[:], op=ALU.mult), [XS, Rb], [tm])
                        if not moe:
                            P.op("act", lambda e, k=k, tm=tm, seg=seg: e.activation(out=hb.t[:, k, :], in_=tm.t[:], func=AF.Identity, bias=MOD[l][seg].t[:, 48 + k:49 + k], scale=A2[l][seg].t[:, k:k + 1]),
                                 [tm, MOD[l][seg], A2[l][seg]], [hb])
                        else:
                            h32 = h32r.next()
                            P.op("act", lambda e, k=k, tm=tm, seg=seg, h32=h32: e.activation(out=h32.t[:], in_=tm.t[:], func=AF.Identity, bias=MOD[l][seg].t[:, 48 + k:49 + k], scale=A2[l][seg].t[:, k:k + 1]),
                                 [tm, MOD[l][seg], A2[l][seg]], [h32])
                            P.op("dve", lambda e, k=k, h32=h32: e.tensor_copy(out=hb.t[:, k, :], in_=h32.t[:]), [h32], [hb])
                            P.op("pe", lambda e, k=k, h32=h32: e.matmul(psl.t[0:NE, :], wr32.t[:, k, :], h32.t[:], start=(k == 0), stop=(k == KC - 1)), [h32, wr32], [psl])
                    if moe:
                        P.op("dve", lambda e: e.tensor_copy(out=gT.t[:], in_=psl.t[0:NE, :]), [psl], [gT])
                        ps = psr.next()
                        for s in range(4):
                            P.op("pe", lambda e, s=s, ps=ps: e.transpose(ps.t[:, s * NE:(s + 1) * NE], gT.t[0:NE, s * 128:(s + 1) * 128], identF.t[0:NE, 0:NE]), [gT, identF], [ps])
                        P.op("dve", lambda e, ps=ps: e.tensor_copy(out=lg.t[:], in_=ps.t[:, 0:4 * NE].rearrange("p (s n) -> p s n", n=NE)), [ps], [lg])
                        for s in range(4):
                            P.op("dve", lambda e, s=s: e.tensor_reduce(out=sm.t[:, s:s + 1], in_=lg.t[:, s, :], axis=AX.X, op=ALU.max), [lg], [sm])
                            P.op("dve", lambda e, s=s: e.tensor_scalar(out=l2.t[:, s, :], in0=lg.t[:, s, :], scalar1=sm.t[:, s:s + 1], scalar2=-1.0e30, op0=ALU.is_equal, op1=ALU.mult), [lg, sm], [l2])
                            P.op("dve", lambda e, s=s: e.tensor_tensor(out=l2.t[:, s, :], in0=l2.t[:, s, :], in1=lg.t[:, s, :], op=ALU.add), [l2, lg], [l2])
                            P.op("dve", lambda e, s=s: e.tensor_reduce(out=sm.t[:, 4 + s:5 + s], in_=l2.t[:, s, :], axis=AX.X, op=ALU.max), [l2], [sm])
                            P.op("dve", lambda e, s=s: e.tensor_scalar_mul(out=sm.t[:, 8 + s:9 + s], in0=sm.t[:, s:s + 1], scalar1=-1.0), [sm], [sm])
                            P.op("act", lambda e, s=s: e.activation(out=gts.t[:, s, :], in_=lg.t[:, s, :], func=AF.Exp, bias=sm.t[:, 8 + s:9 + s]), [lg, sm], [gts])
                            P.op("dve", lambda e, s=s: e.scalar_tensor_tensor(out=gts.t[:, s, :], in0=lg.t[:, s, :], scalar=sm.t[:, 4 + s:5 + s], in1=gts.t[:, s, :], op0=ALU.is_ge, op1=ALU.mult), [lg, sm, gts], [gts])
                            P.op("dve", lambda e, s=s: e.tensor_reduce(out=sm.t[:, 12 + s:13 + s], in_=gts.t[:, s, :], axis=AX.X, op=ALU.add), [gts], [sm])
                            P.op("dve", lambda e, s=s: e.reciprocal(out=sm.t[:, 12 + s:13 + s], in_=sm.t[:, 12 + s:13 + s]), [sm], [sm])
                            P.op("dve", lambda e, s=s: e.tensor_scalar_mul(out=gts.t[:, s, :], in0=gts.t[:, s, :], scalar1=sm.t[:, 12 + s:13 + s]), [gts, sm], [gts])
                        ps = psr.next()
                        for s in range(4):
                            P.op("pe", lambda e, s=s, ps=ps: e.transpose(ps.t[0:NE, s * 128:(s + 1) * 128], gts.t[:, s, :], identF.t[:]), [gts, identF], [ps])
                        P.op("dve", lambda e, ps=ps: e.tensor_copy(out=gT.t[:], in_=ps.t[0:NE, :]), [ps], [gT])
                        for e_ in range(NE):
                            ps = psr.next()
                            P.op("pe", lambda e, e_=e_, ps=ps: e.matmul(ps.t[:, :], SEL.t[0:NE, e_, :], gT.t[:], start=True, stop=True), [SEL, gT], [ps])
                            P.op("act", lambda e, e_=e_, ps=ps: e.copy(out=gbt.t[:, e_, :], in_=ps.t[:, :]), [ps], [gbt])
                    for ex in range(NE if moe else 1):
                        exi = (1 + ex) if moe else 0
                        wsb = WSe if moe else WSd
                        for fg in range(FC // 2):
                            wg_t = wgu.next()
                            wu_t = wgu.next()
                            P.dma("sp", wg_t.t[:], WGS[exi, fg].rearrange("p (k n) -> p k n", n=256), [wsb], [wg_t])
                            P.dma("sp", wu_t.t[:], WUS[exi, fg].rearrange("p (k n) -> p k n", n=256), [wsb], [wu_t])
                            for ff in range(2):
                                fc = fg * 2 + ff
                                pg = psr.next()
                                pu = psr.next()
                                for k in range(KC):
                                    P.op("pe", lambda e, k=k, ff=ff, wg_t=wg_t, pg=pg: e.matmul(pg.t[:, :], wg_t.t[:, k, ff * 128:(ff + 1) * 128], hb.t[:, k, :], start=(k == 0), stop=(k == KC - 1)), [wg_t, hb], [pg])
                                for k in range(KC):
                                    P.op("pe", lambda e, k=k, ff=ff, wu_t=wu_t, pu=pu: e.matmul(pu.t[:, :], wu_t.t[:, k, ff * 128:(ff + 1) * 128], hb.t[:, k, :], start=(k == 0), stop=(k == KC - 1)), [wu_t, hb], [pu])
                                tm = tmpr.next()
                                P.op("act", lambda e, pg=pg, tm=tm: e.activation(out=tm.t[:], in_=pg.t[:, :], func=AF.Silu), [pg], [tm])
                                if moe:
                                    tm2 = tmpr.next()
                                    P.op("dve", lambda e, pu=pu, tm=tm, tm2=tm2: e.tensor_tensor(out=tm2.t[:], in0=pu.t[:, :], in1=tm.t[:], op=ALU.mult), [pu, tm], [tm2])
                                    P.op("dve", lambda e, fc=fc, tm2=tm2, ex=ex: e.tensor_tensor(out=act.t[:, fc, :], in0=tm2.t[:], in1=gbt.t[:, ex, :], op=ALU.mult), [tm2, gbt], [act])
                                else:
                                    P.op("dve", lambda e, fc=fc, pu=pu, tm=tm: e.tensor_tensor(out=act.t[:, fc, :], in0=pu.t[:, :], in1=tm.t[:], op=ALU.mult), [pu, tm], [act])
                        for dc in range(KC):
                            wd_t = wdr.next()
                            P.dma("sp", wd_t.t[:], WDS[exi, dc].rearrange("p (f n) -> p f n", n=128), [wsb], [wd_t])
                            ps = psr.next()
                            for fc in range(FC):
                                P.op("pe", lambda e, fc=fc, wd_t=wd_t, ps=ps: e.matmul(ps.t[:, :], wd_t.t[:, fc, :], act.t[:, fc, :], start=(fc == 0), stop=(fc == FC - 1)), [wd_t, act], [ps])
                            P.op("dve", lambda e, dc=dc, ps=ps, seg=seg: e.scalar_tensor_tensor(out=XS.t[:, dc, :], in0=ps.t[:, :], scalar=MOD[l][seg].t[:, 80 + dc:81 + dc], in1=XS.t[:, dc, :],
                                                                                     op0=ALU.mult, op1=ALU.add), [ps, MOD[l][seg], XS], [XS])
                    if not last:
                        P.dma("sp", XT[blk], XS.t[:], [XS], [XTb[blk]])
                    elif seg == 1 or blk * T < OWN:
                        rstd_from_sq([(XS.t[:, k, :], XS) for k in range(KC)], KC, Rb, sqr, EPS * D)
                        go = 24 * DEPTH
                        for k in range(KC):
                            tm = tmpr.next()
                            P.op("dve", lambda e, k=k, tm=tm: e.tensor_tensor(out=tm.t[:], in0=XS.t[:, k, :], in1=Rb.t[:], op=ALU.mult), [XS, Rb], [tm])
                            P.op("act", lambda e, k=k, tm=tm, go=go: e.activation(out=XS.t[:, k, :], in_=tm.t[:], func=AF.Identity, scale=GL.t[:, go + k:go + k + 1]), [tm, GL, XS], [XS])
                        yo = act.t[:, 0:16, :].rearrange("p a b -> p (a b)").bitcast(F32)
                        orow = (blk * T) if seg == 0 else (OWN + blk * T - S_P)
                        for s in range(4):
                            half = s % 2
                            for k4 in range(4):
                                ps = psr.next()
                                for kk in range(4):
                                    k = k4 * 4 + kk
                                    P.op("pe", lambda e, k=k, kk=kk, s=s, ps=ps: e.transpose(ps.t[:, kk * 128:(kk + 1) * 128], XS.t[:, k, s * 128:(s + 1) * 128], identF.t[:]), [XS, identF], [ps])
                                P.op("act" if k4 % 2 else "dve", lambda e, k4=k4, half=half, ps=ps: (e.copy if e is nc.scalar else e.tensor_copy)(out=yo[:, half * 2048 + k4 * 512:half * 2048 + (k4 + 1) * 512], in_=ps.t[:, :]), [ps], [act])
                            P.dma("sp", y_out[orow + s * 128:orow + (s + 1) * 128, :], yo[:, half * 2048:(half + 1) * 2048], [act], [Yb])
                P.end_phase()

        P._wait_all(P.E["sp"], {Yb.dsemname: Yb.dcnt})
    return nc


def _host_tables(S_P, S_S, shift):
    import jax
    import jax.numpy as jnp
    cpu = jax.devices("cpu")[0]
    with jax.default_device(cpu):
        def rope(S):
            pos = jnp.arange(S, dtype=jnp.float32)
            inv = 1.0 / (10000.0 ** (jnp.arange(0, 64, 2, dtype=jnp.float32) / 64))
            ang = pos[:, None] * inv[None, :]
            return np.asarray(jnp.cos(ang)), np.asarray(jnp.sin(ang))
        cP, sP = rope(S_P)
        cS, sS = rope(S_S)
        rel = jnp.arange(-255, 256, dtype=jnp.int32)
        nb = 16
        ret = (rel > 0).astype(jnp.int32) * nb
        n = jnp.abs(rel)
        max_exact = nb // 2
        nf = jnp.maximum(n, 1).astype(jnp.float32)
        large = max_exact + (jnp.log(nf / max_exact) / math.log(128 / max_exact) * (nb - max_exact)).astype(jnp.int32)
        large = jnp.minimum(large, nb - 1)
        bucket = np.asarray(ret + jnp.where(n < max_exact, n, large))
    relv = np.arange(-255, 256)
    idx = (np.arange(S_P) + shift) % S_P
    cos = np.concatenate([cP[idx], cS], 0).T
    sin = np.concatenate([sP[idx], sS], 0).T
    rope_t = np.zeros((64, 2, S_P + S_S), np.float32)
    rope_t[0:32, 0] = cos
    rope_t[32:64, 0] = cos
    rope_t[0:32, 1] = -sin
    rope_t[32:64, 1] = sin
    nbP, nbS = S_P // 128, S_S // 128
    em = np.zeros((128, 2 * (nbP + nbS)), np.float32)
    for b in range(nbP):
        gb = (b + shift // 128) % nbP
        if gb == 0:
            em[:, 2 * b] = NEG
        if gb == nbP - 1:
            em[:, 2 * b + 1] = NEG
    em[:, 2 * nbP] = NEG
    em[:, 2 * (nbP + nbS) - 1] = NEG
    oh = np.zeros((33, 512), np.float32)
    for kp in range(511):
        r = 255 - kp
        if abs(r) <= 128:
            oh[bucket[r + 255], kp] = 1.0
        else:
            oh[32, kp] = 1.0
    oh[32, 511] = 1.0
    return rope_t, em, oh


_PERM = np.concatenate([np.arange(32, 64), np.arange(0, 32)])


def make_in_maps(inputs, cfg, n_cores=8):
    S_P, S_S, OWN = cfg["S_P"], cfg["S_S"], cfg["OWN"]
    f = lambda a: np.ascontiguousarray(np.asarray(a, dtype=np.float32))
    shared = {k: f(inputs[k]) for k in ("rel_bias", "w_ada", "b_ada", "g_norm_mix", "g_norm_ffn", "w_in", "g_q_lat", "w_uq", "g_kv_lat", "w_ukv",
                                         "sink", "g_out_a", "g_out_b", "w_out", "w_gate_d", "w_up_d", "w_down_d", "w_router", "w_gate_e", "w_up_e",
                                         "w_down_e", "g_final")}
    w_in = shared["w_in"]
    shared["w_krs"] = np.ascontiguousarray(w_in[:, :, 1024:1088][:, :, _PERM])
    wq = shared["w_uq"].reshape(w_in.shape[0], 512, 8, 192)
    shared["w_uqs"] = np.ascontiguousarray(wq[:, :, :, 128:192][:, :, :, _PERM].reshape(w_in.shape[0], 512, 512))
    shared["ident_in"] = np.eye(128, dtype=np.float32)
    xp, xs = f(inputs["x_prompt"]), f(inputs["x_sample"])
    cp, csmp = f(inputs["c_prompt"]), f(inputs["c_sample"])
    per_group = n_cores // xp.shape[0]
    maps = []
    for c in range(n_cores):
        b = c // per_group
        r = c % per_group
        shift = r * OWN
        idx = (np.arange(S_P) + shift) % S_P
        rope_t, em, oh = _host_tables(S_P, S_S, shift)
        m = dict(shared)
        m["x_in"] = np.ascontiguousarray(np.concatenate([xp[b][idx], xs[c]], 0))
        m["c_in"] = np.ascontiguousarray(np.stack([cp[b], csmp[c]], 0))
        m["rope_in"] = rope_t
        m["em_in"] = em
        m["oh_in"] = oh
        maps.append(m)
    return maps


CFG_FULL = dict(S_P=16384, S_S=2048, OWN=4096, FF=5632, NE=8, DEPTH=2)


def kernel(**inputs):
    return run(inputs, CFG_FULL)


def run(inputs, cfg):
    nc = build(cfg)
    maps = make_in_maps(inputs, cfg)
    res = run_bass_kernel_spmd(nc, maps, core_ids=list(range(8)))
    S_P, S_S, OWN = cfg["S_P"], cfg["S_S"], cfg["OWN"]
    yp = np.zeros((2, S_P, D), np.float32)
    ys = np.zeros((8, S_S, D), np.float32)
    for c in range(8):
        y = np.asarray(res.results[c]["y_out"])
        b, r = c // 4, c % 4
        yp[b, r * OWN:(r + 1) * OWN] = y[0:OWN]
        ys[c] = y[OWN:OWN + S_S]
    return (yp, ys)
```

```python
import math
from contextlib import ExitStack
import numpy as np
import concourse.bass as bass
import concourse.mybir as mybir
from concourse.bass_utils import run_bass_kernel_spmd

F32 = mybir.dt.float32
BF16 = mybir.dt.bfloat16
AF = mybir.ActivationFunctionType
ALU = mybir.AluOpType
AX = mybir.AxisListType

D = 2048
KC = 16
EPS = 1e-6
H = 8
MLA_SCALE = 1.0 / math.sqrt(192.0)
GQA_SCALE = 1.0 / math.sqrt(128.0)
NEG = -1.0e4
T = 512


class Buf:
    def __init__(self, name, t=None):
        self.name = name
        self.t = t
        self.w = {}
        self.r = {}
        self.dsem = None
        self.dsemname = None
        self.dcnt = 0
        self.scoped = False


class Eng:
    def __init__(self, name, obj, sem, semname):
        self.name, self.obj, self.sem, self.semname = name, obj, sem, semname
        self.cnt = 0
        self.seen = {}


class Prog:
    def __init__(self, nc, es):
        self.nc, self.es = nc, es
        self.sems = {}
        self.E = {}
        for name, obj in (("pe", nc.tensor), ("act", nc.scalar), ("dve", nc.vector), ("pool", nc.gpsimd), ("sp", nc.sync)):
            sn = "c_" + name
            self.E[name] = Eng(name, obj, self.newsem(sn), sn)
        self.dbufs = []
        self.free_dsems = []
        self.nuid = 0

    def newsem(self, name):
        s = self.es.enter_context(self.nc.semaphore(name))
        self.sems[name] = s
        return s

    def tile(self, name, shape, dt, scope=None):
        self.nuid += 1
        t = (scope or self.es).enter_context(self.nc.sbuf_tensor(f"{name}_{self.nuid}", list(shape), dt))
        b = Buf(name, t)
        b.scoped = scope is not None
        return b

    def _wait(self, E, deps, defer_last=False):
        need = []
        for sn, val in deps.items():
            if E.seen.get(sn, 0) >= val:
                continue
            if sn == E.semname and E.name == "pe":
                continue
            need.append((sn, val))
            E.seen[sn] = val
        last = None
        if defer_last and need:
            last = need.pop()
        for sn, val in need:
            E.obj.wait_ge(self.sems[sn], val)
        return last

    @staticmethod
    def _merge(d, s, skip=None):
        for k, v in s.items():
            if k != skip and d.get(k, 0) < v:
                d[k] = v

    def op(self, e, fn, reads=(), writes=()):
        E = self.E[e]
        deps = {}
        for b in reads:
            self._merge(deps, b.w)
        for b in writes:
            self._merge(deps, b.w)
            self._merge(deps, b.r)
        last = self._wait(E, deps, defer_last=True)
        ins = fn(E.obj)
        if last is not None:
            ins._wait_ge(self.sems[last[0]], last[1])
        E.cnt += 1
        ins.then_inc(E.sem, 1)
        for b in writes:
            b.w[E.semname] = E.cnt
        for b in reads:
            b.r[E.semname] = E.cnt
        return ins

    def dma(self, q, out, in_, reads, writes, **kw):
        E = self.E[q]
        prim = writes[0]
        if prim.dsem is None:
            if self.free_dsems:
                prim.dsemname, prim.dsem, prim.dcnt = self.free_dsems.pop()
            else:
                self.nuid += 1
                prim.dsemname = f"d_{self.nuid}"
                prim.dsem = self.newsem(prim.dsemname)
            self.dbufs.append(prim)
        deps = {}
        for b in reads:
            self._merge(deps, b.w)
        for b in writes:
            self._merge(deps, b.r)
            self._merge(deps, b.w, skip=b.dsemname)
        last = self._wait(E, deps, defer_last=True)
        ins = E.obj.dma_start(out=out, in_=in_, **kw)
        if last is not None:
            ins._wait_ge(self.sems[last[0]], last[1])
        prim.dcnt += 16
        ins.then_inc(prim.dsem, 16)
        for b in writes:
            b.w[prim.dsemname] = prim.dcnt
        for b in reads:
            b.r[prim.dsemname] = prim.dcnt

    def barrier(self):
        deps = {}
        for E in self.E.values():
            if E.cnt:
                deps[E.semname] = E.cnt
        for b in self.dbufs:
            if b.dcnt:
                deps[b.dsemname] = b.dcnt
        for E in self.E.values():
            d = dict(deps)
            self._wait_all(E, d)

    def end_phase(self):
        self.barrier()
        keep = []
        for b in self.dbufs:
            if getattr(b, "scoped", False):
                self.free_dsems.append((b.dsemname, b.dsem, b.dcnt))
            else:
                keep.append(b)
        self.dbufs = keep

    def _wait_all(self, E, deps):
        for sn, val in deps.items():
            if E.seen.get(sn, 0) >= val:
                continue
            E.obj.wait_ge(self.sems[sn], val)
            E.seen[sn] = val


class Rot:
    def __init__(self, bufs):
        self.bufs, self.i = bufs, 0

    def next(self):
        b = self.bufs[self.i % len(self.bufs)]
        self.i += 1
        return b


def build(cfg):
    S_P, S_S, OWN, FF, NE, DEPTH = cfg["S_P"], cfg["S_S"], cfg["OWN"], cfg["FF"], cfg["NE"], cfg["DEPTH"]
    FC = FF // 128
    NTOK = S_P + S_S
    NBLK = NTOK // T
    segs = (("P", 0, S_P), ("S", S_P, S_S))
    nc = bass.Bass("TRN2", target_bir_lowering=False)

    def din(name, shape, dt=F32):
        return nc.dram_tensor(name, list(shape), dt, kind="ExternalInput").ap()

    def dscr(name, shape, dt):
        return nc.dram_tensor(name, list(shape), dt, kind="Internal").ap()

    x_in = din("x_in", [NTOK, D])
    c_in = din("c_in", [2, D])
    rope_in = din("rope_in", [64, 2, NTOK])
    em_in = din("em_in", [128, 2 * (NTOK // 128)])
    ident_in = din("ident_in", [128, 128])
    oh_in = din("oh_in", [33, 512])
    rel_bias = din("rel_bias", [32, 8])
    w_ada = din("w_ada", [DEPTH, D, 6 * D])
    b_ada = din("b_ada", [DEPTH, 6 * D])
    g_norm_mix = din("g_norm_mix", [DEPTH, D])
    g_norm_ffn = din("g_norm_ffn", [DEPTH, D])
    w_in = din("w_in", [DEPTH, D, 2624])
    w_krs = din("w_krs", [DEPTH, D, 64])
    g_q_lat = din("g_q_lat", [DEPTH, 512])
    w_uq = din("w_uq", [DEPTH, 512, 1536])
    w_uqs = din("w_uqs", [DEPTH, 512, 512])
    g_kv_lat = din("g_kv_lat", [DEPTH, 512])
    w_ukv = din("w_ukv", [DEPTH, 512, 2048])
    sink = din("sink", [DEPTH, 8])
    g_out_a = din("g_out_a", [DEPTH, 1024])
    g_out_b = din("g_out_b", [DEPTH, 1024])
    w_out = din("w_out", [DEPTH, D, D])
    w_gate_d = din("w_gate_d", [1, D, FF])
    w_up_d = din("w_up_d", [1, D, FF])
    w_down_d = din("w_down_d", [1, FF, D])
    w_router = din("w_router", [1, D, NE])
    w_gate_e = din("w_gate_e", [1, NE, D, FF])
    w_up_e = din("w_up_e", [1, NE, D, FF])
    w_down_e = din("w_down_e", [1, NE, FF, D])
    g_final = din("g_final", [D])
    NOUT = OWN + S_S
    y_out = nc.dram_tensor("y_out", [NOUT, D], F32, kind="ExternalOutput").ap()

    XT = dscr("XT", [NBLK, 128, KC, T], F32)
    QT = dscr("QT", [H, 192, NTOK], BF16)
    LAT = dscr("LAT", [576, NTOK], BF16)
    GQ = dscr("GQ", [H, 128, NTOK], BF16)
    GK = dscr("GK", [2, 128, NTOK], BF16)
    GV = dscr("GV", [NTOK, 256], BF16)
    OA = dscr("OA", [H, 128, NTOK], BF16)
    OB = dscr("OB", [H, 128, NTOK], BF16)
    TB = dscr("TB", [8, 128, 512], F32)
    NEX = 1 + NE
    WGS = dscr("WGS", [NEX, FC // 2, 128, KC * 256], BF16)
    WUS = dscr("WUS", [NEX, FC // 2, 128, KC * 256], BF16)
    WDS = dscr("WDS", [NEX, KC, 128, FC * 128], BF16)

    es = ExitStack()
    with es:
        P = Prog(nc, es)
        XTb = [Buf(f"XT{i % 6}") for i in range(6)] * (NBLK // 6 + 1)
        QTb, LATb, GQb, GKb, GVb, OAb, OBb, TBb, Yb = (Buf(n) for n in ("QT", "LAT", "GQ", "GK", "GV", "OA", "OB", "TB", "Y"))
        CONST = Buf("const")
        WSd, WSe = Buf("WSd"), Buf("WSe")
        conv = []
        for ex in range(NEX):
            Wg_ = w_gate_d[0] if ex == 0 else w_gate_e[0][ex - 1]
            Wu_ = w_up_d[0] if ex == 0 else w_up_e[0][ex - 1]
            Wd_ = w_down_d[0] if ex == 0 else w_down_e[0][ex - 1]
            wb = WSd if ex == 0 else WSe
            for fg in range(FC // 2):
                conv.append((WGS[ex, fg].rearrange("p (k n) -> p k n", n=256), Wg_[:, fg * 256:(fg + 1) * 256].rearrange("(k p) n -> p k n", p=128), wb))
                conv.append((WUS[ex, fg].rearrange("p (k n) -> p k n", n=256), Wu_[:, fg * 256:(fg + 1) * 256].rearrange("(k p) n -> p k n", p=128), wb))
            for dc in range(KC):
                conv.append((WDS[ex, dc].rearrange("p (f n) -> p f n", n=128), Wd_[:, dc * 128:(dc + 1) * 128].rearrange("(f p) n -> p f n", p=128), wb))
        conv.reverse()
        conv_per_blk = -(-len(conv) // NBLK)

        def issue_conv(n):
            for _ in range(n):
                if conv:
                    d_, s_, wb_ = conv.pop()
                    P.dma("pool", d_, s_, [CONST], [wb_])

        PS = []
        for i in range(8):
            t = es.enter_context(nc.psum_tensor(f"ps{i}", [128, 512], F32))
            PS.append(Buf(f"ps{i}", t))
        psr = Rot(PS)

        identF = P.tile("identF", [128, 128], F32)
        onesB = P.tile("onesB", [128, 128], BF16)
        onesF = P.tile("onesF", [128, 128], F32)
        vecA = [P.tile(f"vecA{l}", [128, 128], F32) for l in range(DEPTH)]
        vecB = P.tile("vecB", [128, 64], F32)
        MOD = [[P.tile(f"mod{l}{s}", [128, 96], F32) for s in range(2)] for l in range(DEPTH)]
        A1 = [[P.tile(f"a1{l}{s}", [128, 16], F32) for s in range(2)] for l in range(DEPTH)]
        A2 = [[P.tile(f"a2{l}{s}", [128, 16], F32) for s in range(2)] for l in range(DEPTH)]
        GL = P.tile("GL", [128, 64], F32)
        BT = [[P.tile(f"bt{kv}{jb}", [128, 4, 128], F32) for jb in range(3)] for kv in range(2)]
        EM = P.tile("EM", [128, 2 * (NTOK // 128)], F32)
        ESK = [P.tile(f"esk{l}", [128, 8], F32) for l in range(DEPTH)]
        SEL = P.tile("SEL", [8, NE, 128], F32)

        EPSB = P.tile("EPSB", [128, 3], F32)
        for i_, v_ in enumerate((EPS * D, EPS * 512, EPS * 1024)):
            P.op("dve", lambda e, i_=i_, v_=v_: e.memset(EPSB.t[:, i_:i_ + 1], v_), [], [EPSB])

        def epsb(v):
            i_ = {EPS * D: 0, EPS * 512: 1, EPS * 1024: 2}[v]
            return EPSB.t[:, i_:i_ + 1]
        P.dma("sp", identF.t[:], ident_in[:, :], [CONST], [identF])
        P.dma("sp", EM.t[:], em_in[:, :], [CONST], [EM])
        P.op("dve", lambda e: e.memset(onesB.t[:], 1.0), [], [onesB])
        P.op("dve", lambda e: e.memset(onesF.t[:], 1.0), [], [onesF])

        def transpose_rows(scope, rows_aps, out_buf, ncols):
            st = P.tile("stg", [128, 128], F32, scope)
            P.op("dve", lambda e: e.memset(st.t[:], 0.0), [], [st])
            r0 = 0
            for ap, r in rows_aps:
                P.dma("sp", st.t[r0:r0 + r, :], ap, [CONST], [st])
                r0 += r
            ps = psr.next()
            P.op("pe", lambda e: e.transpose(ps.t[:, 0:128], st.t[:], identF.t[:]), [st, identF], [ps])
            P.op("dve", lambda e: e.tensor_copy(out=out_buf.t[:, 0:ncols], in_=ps.t[:, 0:ncols]), [ps], [out_buf])

        def bcast_row(src_ap, n, out_ap, out_buf, scope, func=None):
            row = P.tile("row", [1, n], F32, scope)
            P.dma("sp", row.t[:], src_ap, [CONST], [row])
            ps = psr.next()
            P.op("pe", lambda e: e.matmul(ps.t[:, 0:n], onesF.t[0:1, :], row.t[:], start=True, stop=True), [onesF, row], [ps])
            if func is None:
                P.op("dve", lambda e: e.tensor_copy(out=out_ap, in_=ps.t[:, 0:n]), [ps], [out_buf])
            else:
                P.op("act", lambda e: e.activation(out=out_ap, in_=ps.t[:, 0:n], func=func), [ps], [out_buf])

        with ExitStack() as sc:
            for l in range(DEPTH):
                transpose_rows(sc, [(b_ada[l].rearrange("(c p) -> c p", p=128), 96),
                                    (g_norm_mix[l].rearrange("(c p) -> c p", p=128), 16),
                                    (g_norm_ffn[l].rearrange("(c p) -> c p", p=128), 16)], vecA[l], 128)
            rows = []
            for l in range(DEPTH):
                rows += [(g_q_lat[l].rearrange("(c p) -> c p", p=128), 4), (g_kv_lat[l].rearrange("(c p) -> c p", p=128), 4),
                         (g_out_a[l].rearrange("(c p) -> c p", p=128), 8), (g_out_b[l].rearrange("(c p) -> c p", p=128), 8)]
            rows += [(g_final.rearrange("(c p) -> c p", p=128), 16)]
            transpose_rows(sc, rows, vecB, 24 * DEPTH + 16)
            for l in range(DEPTH):
                o = 24 * l
                P.op("dve", lambda e, o=o: e.tensor_scalar_mul(out=GL.t[:, o:o + 8], in0=vecB.t[:, o:o + 8], scalar1=math.sqrt(512.0)), [vecB], [GL])
                P.op("dve", lambda e, o=o: e.tensor_scalar_mul(out=GL.t[:, o + 8:o + 24], in0=vecB.t[:, o + 8:o + 24], scalar1=math.sqrt(1024.0)), [vecB], [GL])
            o = 24 * DEPTH
            P.op("dve", lambda e: e.tensor_scalar_mul(out=GL.t[:, o:o + 16], in0=vecB.t[:, o:o + 16], scalar1=math.sqrt(float(D))), [vecB], [GL])

            for l in range(DEPTH):
                bcast_row(sink[l:l + 1, :], 8, ESK[l].t[:, :], ESK[l], sc, func=AF.Exp)
            for e_ in range(NE):
                P.op("dve", lambda e, e_=e_: e.tensor_scalar_mul(out=SEL.t[0:8, e_, :], in0=onesF.t[0:8, :], scalar1=identF.t[0:8, e_:e_ + 1]),
                     [onesF, identF], [SEL])

            rb = P.tile("rb", [33, 8], F32, sc)
            P.op("dve", lambda e: e.memset(rb.t[:], NEG), [], [rb])
            P.dma("sp", rb.t[0:32, :], rel_bias[:, :], [CONST], [rb])
            ohs = P.tile("ohs", [33, 512], F32, sc)
            P.dma("sp", ohs.t[:], oh_in[:, :], [CONST], [ohs])
            onesF33 = P.tile("ones33", [33, 128], F32, sc)
            P.op("dve", lambda e: e.memset(onesF33.t[:], 1.0), [], [onesF33])
            for h in range(8):
                lh = P.tile("lh", [33, 128], F32, sc)
                P.op("dve", lambda e, h=h, lh=lh: e.tensor_scalar_mul(out=lh.t[:], in0=onesF33.t[:], scalar1=rb.t[:, h:h + 1]), [onesF33, rb], [lh])
                ps = psr.next()
                P.op("pe", lambda e, lh=lh, ps=ps: e.matmul(ps.t[:, :], lh.t[:], ohs.t[:], start=True, stop=True), [lh, ohs], [ps])
                tr = P.tile("tr", [128, 512], F32, sc)
                P.op("dve", lambda e, ps=ps, tr=tr: e.tensor_copy(out=tr.t[:], in_=ps.t[:]), [ps], [tr])
                P.dma("sp", TB[h], tr.t[:], [tr], [TBb])
            for h in range(8):
                kv, g = h // 4, h % 4
                flat = TB[h].rearrange("p c -> (p c)")
                for jb in range(3):
                    cpr = 383 - 128 * jb
                    src = bass.AP(tensor=flat.tensor, offset=flat.offset + cpr, ap=[[511, 128], [1, 128]])
                    P.dma("sp", BT[kv][jb].t[:, g, :], src, [TBb], [BT[kv][jb]])

            cin = P.tile("cin", [2, D], F32, sc)
            P.dma("sp", cin.t[:], c_in[:, :], [CONST], [cin])
            csl = P.tile("csl", [2, D], F32, sc)
            P.op("act", lambda e: e.activation(out=csl.t[:], in_=cin.t[:], func=AF.Silu), [cin], [csl])
            csT = P.tile("csT", [128, KC, 2], BF16, sc)
            ps = psr.next()
            for k in range(KC):
                P.op("pe", lambda e, k=k: e.transpose(ps.t[:, 2 * k:2 * k + 2], csl.t[0:2, k * 128:(k + 1) * 128], identF.t[0:2, 0:2]), [csl, identF], [ps])
            P.op("dve", lambda e: e.tensor_copy(out=csT.t[:].rearrange("p k b -> p (k b)"), in_=ps.t[:, 0:2 * KC]), [ps], [csT])
            wa = Rot([P.tile(f"wa{i}", [128, KC, 512], BF16, sc) for i in range(3)])
            for l in range(DEPTH):
                psm = psr.next()
                for cg in range(24):
                    wt = wa.next()
                    P.dma("pool", wt.t[:], w_ada[l][:, cg * 512:(cg + 1) * 512].rearrange("(k p) n -> p k n", p=128), [CONST], [wt])
                    for oc in range(4):
                        occ = cg * 4 + oc
                        for k in range(KC):
                            P.op("pe", lambda e, k=k, oc=oc, occ=occ, wt=wt: e.matmul(psm.t[:, 2 * occ:2 * occ + 2], wt.t[:, k, oc * 128:(oc + 1) * 128], csT.t[:, k, :],
                                                                          start=(k == 0), stop=(k == KC - 1)), [wt, csT], [psm])
                pv = psm.t[:, 0:192].rearrange("p (c b) -> p c b", b=2)
                for s in range(2):
                    P.op("dve", lambda e, s=s, l=l: e.tensor_tensor(out=MOD[l][s].t[:], in0=pv[:, :, s], in1=vecA[l].t[:, 0:96], op=ALU.add), [psm, vecA[l]], [MOD[l][s]])
                    for (A, c0, g0) in ((A1, 16, 96), (A2, 64, 112)):
                        P.op("dve", lambda e, A=A, c0=c0, s=s, l=l: e.tensor_scalar(out=A[l][s].t[:], in0=MOD[l][s].t[:, c0:c0 + 16], scalar1=1.0, scalar2=math.sqrt(float(D)),
                                                                           op0=ALU.add, op1=ALU.mult), [MOD[l][s]], [A[l][s]])
                        P.op("dve", lambda e, A=A, g0=g0, s=s, l=l: e.tensor_tensor(out=A[l][s].t[:], in0=A[l][s].t[:], in1=vecA[l].t[:, g0:g0 + 16], op=ALU.mult), [A[l][s], vecA[l]], [A[l][s]])
            P.end_phase()

        def rstd_from_sq(srcs, nsq, Rb, sqr, eps_n):
            ps = psr.next()
            for i, (ap, b) in enumerate(srcs):
                sq = sqr.next()
                P.op("act", lambda e, ap=ap, sq=sq: e.activation(out=sq.t[:], in_=ap, func=AF.Square), [b], [sq])
                P.op("pe", lambda e, sq=sq, i=i: e.matmul(ps.t[:, :], onesB.t[:], sq.t[:], start=(i == 0), stop=(i == nsq - 1)), [onesB, sq], [ps])
            P.op("act", lambda e: e.activation(out=Rb.t[:], in_=ps.t[:], func=AF.Sqrt, bias=epsb(eps_n), scale=1.0), [ps, EPSB], [Rb])
            P.op("dve", lambda e: e.reciprocal(out=Rb.t[:], in_=Rb.t[:]), [Rb], [Rb])

        for l in range(DEPTH):
            last = (l == DEPTH - 1)
            if l == 0:
                own_blocks = list(range(NBLK))
            else:
                own_blocks = list(range(OWN // T)) + list(range(S_P // T, NBLK))
            own_set = set(own_blocks)

            with ExitStack() as sc:
                xin = Rot([P.tile(f"xin{i}", [128, D], F32, sc) for i in range(2)])
                XS = P.tile("XS", [128, KC, T], F32, sc)
                sqr = Rot([P.tile(f"sq{i}", [128, T], BF16, sc) for i in range(3)])
                Rb = P.tile("R", [128, T], F32, sc)
                tmpr = Rot([P.tile(f"tmp{i}", [128, T], F32, sc) for i in range(3)])
                hb = P.tile("h", [128, KC, T], BF16, sc)
                lat32 = P.tile("lat32", [128, 4, T], F32, sc)
                cqn = P.tile("cqn", [128, 4, T], BF16, sc)
                ckvn = P.tile("ckvn", [128, 4, T], BF16, sc)
                kr32 = P.tile("kr32", [64, 2, T], F32, sc)
                krb = P.tile("krb", [64, T], BF16, sc)
                cs = P.tile("cs", [64, 2, T], F32, sc)
                o1 = Rot([P.tile(f"o1{i}", [128, T], BF16, sc) for i in range(4)])
                gvst = P.tile("gvst", [128, 4, 256], BF16, sc)
                wp = Rot([P.tile(f"wp{i}", [128, KC, 512], BF16, sc) for i in range(3)])
                wuq = P.tile("wuq", [128, 4, 2048], BF16, sc)
                P.dma("pool", wuq.t[:, :, 0:1536], w_uq[l].rearrange("(k p) n -> p k n", p=128), [CONST], [wuq])
                P.dma("pool", wuq.t[:, :, 1536:2048], w_uqs[l].rearrange("(k p) n -> p k n", p=128), [CONST], [wuq])

                def wload(col0, ncol, extra=None):
                    wt = wp.next()
                    P.dma("pool", wt.t[:, :, 0:ncol], w_in[l][:, col0:col0 + ncol].rearrange("(k p) n -> p k n", p=128), [CONST], [wt])
                    if extra is not None:
                        P.dma("pool", wt.t[:, :, ncol:ncol + 64], extra.rearrange("(k p) n -> p k n", p=128), [CONST], [wt])
                    return wt

                def proj(wt, c0, m, ps_ap, ps):
                    for k in range(KC):
                        P.op("pe", lambda e, k=k: e.matmul(ps_ap, wt.t[:, k, c0:c0 + m], hb.t[:, k, :], start=(k == 0), stop=(k == KC - 1)), [wt, hb], [ps])

                for blk in range(NBLK):
                    seg = 0 if blk * T < S_P else 1
                    t0 = blk * T
                    need_q = blk in own_set
                    if blk > 0:
                        issue_conv(conv_per_blk)
                    if l == 0:
                        for s in range(4):
                            xt = xin.next()
                            P.dma("sp", xt.t[:], x_in[t0 + s * 128:t0 + (s + 1) * 128, :], [CONST], [xt])
                            for k4 in range(4):
                                ps = psr.next()
                                for kk in range(4):
                                    k = k4 * 4 + kk
                                    P.op("pe", lambda e, k=k, kk=kk, xt=xt, ps=ps: e.transpose(ps.t[:, kk * 128:(kk + 1) * 128], xt.t[:, k * 128:(k + 1) * 128], identF.t[:]), [xt, identF], [ps])
                                P.op("act" if k4 % 2 else "dve",
                                     lambda e, k4=k4, s=s, ps=ps: (e.copy if e is nc.scalar else e.tensor_copy)(out=XS.t[:, k4 * 4:(k4 + 1) * 4, s * 128:(s + 1) * 128],
                                                                                                       in_=ps.t[:, :].rearrange("p (k t) -> p k t", t=128)), [ps], [XS])
                        P.dma("sp", XT[blk], XS.t[:], [XS], [XTb[blk]])
                    else:
                        P.dma("sp", XS.t[:], XT[blk], [XTb[blk]], [XS])
                    P.dma("sp", cs.t[:], rope_in[:, :, t0:t0 + T], [CONST], [cs])
                    rstd_from_sq([(XS.t[:, k, :], XS) for k in range(KC)], KC, Rb, sqr, EPS * D)
                    for k in range(KC):
                        tm = tmpr.next()
                        P.op("dve", lambda e, k=k, tm=tm: e.tensor_tensor(out=tm.t[:], in0=XS.t[:, k, :], in1=Rb.t[:], op=ALU.mult), [XS, Rb], [tm])
                        P.op("act", lambda e, k=k, tm=tm: e.activation(out=hb.t[:, k, :], in_=tm.t[:], func=AF.Identity, bias=MOD[l][seg].t[:, k:k + 1], scale=A1[l][seg].t[:, k:k + 1]),
                             [tm, MOD[l][seg], A1[l][seg]], [hb])
                    for which in ((0, 1) if need_q else (1,)):
                        wt = wload(512 * which, 512)
                        for c in range(4):
                            ps = psr.next()
                            proj(wt, c * 128, 128, ps.t[:, :], ps)
                            P.op("act", lambda e, c=c, ps=ps: e.copy(out=lat32.t[:, c, :], in_=ps.t[:, :]), [ps], [lat32])
                        rstd_from_sq([(lat32.t[:, c, :], lat32) for c in range(4)], 4, Rb, sqr, EPS * 512)
                        dst = cqn if which == 0 else ckvn
                        go = 24 * l + 4 * which
                        for c in range(4):
                            tm = tmpr.next()
                            P.op("dve", lambda e, c=c, tm=tm: e.tensor_tensor(out=tm.t[:], in0=lat32.t[:, c, :], in1=Rb.t[:], op=ALU.mult), [lat32, Rb], [tm])
                            P.op("act", lambda e, c=c, tm=tm, dst=dst, go=go: e.activation(out=dst.t[:, c, :], in_=tm.t[:], func=AF.Identity, scale=GL.t[:, go + c:go + c + 1]), [tm, GL], [dst])
                    P.dma("sp", LAT[0:512, t0:t0 + T].rearrange("(c p) t -> p c t", p=128), ckvn.t[:], [ckvn], [LATb])
                    wt = wp.next()
                    P.dma("pool", wt.t[:, :, 0:64], w_in[l][:, 1024:1088].rearrange("(k p) n -> p k n", p=128), [CONST], [wt])
                    P.dma("pool", wt.t[:, :, 64:128], w_krs[l].rearrange("(k p) n -> p k n", p=128), [CONST], [wt])
                    P.dma("pool", wt.t[:, :, 128:512], w_in[l][:, 2112:2496].rearrange("(k p) n -> p k n", p=128), [CONST], [wt])
                    wt2 = wp.next()
                    P.dma("pool", wt2.t[:, :, 0:128], w_in[l][:, 2496:2624].rearrange("(k p) n -> p k n", p=128), [CONST], [wt2])
                    ps = psr.next()
                    proj(wt, 0, 64, ps.t[0:64, :], ps)
                    ps2 = psr.next()
                    proj(wt, 64, 64, ps2.t[0:64, :], ps2)
                    P.op("dve", lambda e, ps=ps: e.tensor_tensor(out=kr32.t[:, 0, :], in0=ps.t[0:64, :], in1=cs.t[:, 0, :], op=ALU.mult), [ps, cs], [kr32])
                    P.op("dve", lambda e, ps2=ps2: e.tensor_tensor(out=kr32.t[:, 1, :], in0=ps2.t[0:64, :], in1=cs.t[:, 1, :], op=ALU.mult), [ps2, cs], [kr32])
                    P.op("dve", lambda e: e.tensor_tensor(out=krb.t[:], in0=kr32.t[:, 0, :], in1=kr32.t[:, 1, :], op=ALU.add), [kr32], [krb])
                    P.dma("sp", LAT[512:576, t0:t0 + T], krb.t[:], [krb], [LATb])
                    for j in range(2):
                        ps = psr.next()
                        proj(wt, 128 + j * 128, 128, ps.t[:, :], ps)
                        ot = o1.next()
                        P.op("act", lambda e, ps=ps, ot=ot: e.copy(out=ot.t[:], in_=ps.t[:, :]), [ps], [ot])
                        P.dma("sp", GK[j, :, t0:t0 + T], ot.t[:], [ot], [GKb])
                    for s in range(4):
                        ps = psr.next()
                        for half, (wsrc, c0) in enumerate(((wt, 384), (wt2, 0))):
                            for k in range(KC):
                                P.op("pe", lambda e, k=k, s=s, half=half, wsrc=wsrc, c0=c0, ps=ps: e.matmul(ps.t[:, half * 128:(half + 1) * 128], hb.t[:, k, s * 128:(s + 1) * 128], wsrc.t[:, k, c0:c0 + 128],
                                                                                                start=(k == 0), stop=(k == KC - 1)), [hb, wsrc], [ps])
                        P.op("dve", lambda e, s=s, ps=ps: e.tensor_copy(out=gvst.t[:, s, :], in_=ps.t[:, 0:256]), [ps], [gvst])
                    P.dma("sp", GV[t0:t0 + T, :].rearrange("(s p) n -> p s n", p=128), gvst.t[:], [gvst], [GVb])
                    if not need_q:
                        continue
                    for half in range(2):
                        wt = wload(1088 + 512 * half, 512)
                        for j in range(4):
                            ps = psr.next()
                            proj(wt, j * 128, 128, ps.t[:, :], ps)
                            ot = o1.next()
                            P.op("act" if j % 2 else "dve", lambda e, ps=ps, ot=ot: (e.copy if e is nc.scalar else e.tensor_copy)(out=ot.t[:], in_=ps.t[:, :]), [ps], [ot])
                            P.dma("sp", GQ[half * 4 + j, :, t0:t0 + T], ot.t[:], [ot], [GQb])
                    for h in range(H):
                        ps = psr.next()
                        for c in range(4):
                            P.op("pe", lambda e, c=c, h=h, ps=ps: e.matmul(ps.t[:, :], wuq.t[:, c, h * 192:h * 192 + 128], cqn.t[:, c, :], start=(c == 0), stop=(c == 3)), [wuq, cqn], [ps])
                        ot = o1.next()
                        P.op("act", lambda e, ps=ps, ot=ot: e.copy(out=ot.t[:], in_=ps.t[:, :]), [ps], [ot])
                        P.dma("sp", QT[h, 0:128, t0:t0 + T], ot.t[:], [ot], [QTb])
                        ps = psr.next()
                        ps2 = psr.next()
                        for c in range(4):
                            P.op("pe", lambda e, c=c, h=h, ps=ps: e.matmul(ps.t[0:64, :], wuq.t[:, c, h * 192 + 128:h * 192 + 192], cqn.t[:, c, :], start=(c == 0), stop=(c == 3)), [wuq, cqn], [ps])
                        for c in range(4):
                            P.op("pe", lambda e, c=c, h=h, ps2=ps2: e.matmul(ps2.t[0:64, :], wuq.t[:, c, 1536 + h * 64:1536 + h * 64 + 64], cqn.t[:, c, :], start=(c == 0), stop=(c == 3)), [wuq, cqn], [ps2])
                        P.op("dve", lambda e, ps=ps: e.tensor_tensor(out=kr32.t[:, 0, :], in0=ps.t[0:64, :], in1=cs.t[:, 0, :], op=ALU.mult), [ps, cs], [kr32])
                        P.op("dve", lambda e, ps2=ps2: e.tensor_tensor(out=kr32.t[:, 1, :], in0=ps2.t[0:64, :], in1=cs.t[:, 1, :], op=ALU.mult), [ps2, cs], [kr32])
                        ot = o1.next()
                        P.op("dve", lambda e, ot=ot: e.tensor_tensor(out=ot.t[0:64, :], in0=kr32.t[:, 0, :], in1=kr32.t[:, 1, :], op=ALU.add), [kr32], [ot])
                        P.dma("sp", QT[h, 128:192, t0:t0 + T], ot.t[0:64, :], [ot], [QTb])
                issue_conv(len(conv))
                P.end_phase()

            with ExitStack() as sc:
                wukv = P.tile("wukv", [128, 4, 2048], BF16, sc)
                P.dma("pool", wukv.t[:], w_ukv[l].rearrange("(k p) n -> p k n", p=128), [CONST], [wukv])
                SMAX = max(S_P, S_S)
                krT = P.tile("krT", [64, SMAX], BF16, sc)
                KT = P.tile("KT", [128, SMAX], BF16, sc)
                VV = P.tile("VV", [128, SMAX // 128, 128], BF16, sc)
                latr = Rot([P.tile(f"lat{i}", [128, 4, T], BF16, sc) for i in range(3)])
                qnr = Rot([P.tile(f"qn{i}", [128, T], BF16, sc) for i in range(2)])
                qrr = Rot([P.tile(f"qr{i}", [64, T], BF16, sc) for i in range(2)])
                pr = Rot([P.tile(f"p{i}", [128, T], BF16, sc) for i in range(3)])
                rdr = Rot([P.tile(f"rd{i}", [128, T], F32, sc) for i in range(2)])
                obr = Rot([P.tile(f"ob{i}", [128, T], BF16, sc) for i in range(2)])
                accr = Rot([P.tile(f"acc{i}", [128, T], F32, sc) for i in range(6)])
                pr = Rot([P.tile(f"pp{i}", [128, T], BF16, sc) for i in range(5)])
                psS = Rot(PS[0:4])
                psO = Rot(PS[4:6])
                psD = Rot(PS[6:7])
                psK = Rot(PS[6:8])
                for (sname, s0, slen) in segs:
                    nq = (slen if (l == 0 or sname == "S") else OWN) // T
                    nkc = slen // 128
                    P.dma("sp", krT.t[:, 0:slen], LAT[512:576, s0:s0 + slen], [LATb], [krT])
                    for h in range(H):
                        for tc_ in range(slen // T):
                            lt = latr.next()
                            P.dma("sp", lt.t[:], LAT[0:512, s0 + tc_ * T:s0 + (tc_ + 1) * T].rearrange("(c p) t -> p c t", p=128), [LATb], [lt])
                            ps = psK.next()
                            for c in range(4):
                                P.op("pe", lambda e, c=c, h=h, lt=lt, ps=ps: e.matmul(ps.t[:, :], wukv.t[:, c, h * 256:h * 256 + 128], lt.t[:, c, :], start=(c == 0), stop=(c == 3)), [wukv, lt], [ps])
                            P.op("act", lambda e, ps=ps, tc_=tc_: e.copy(out=KT.t[:, tc_ * T:(tc_ + 1) * T], in_=ps.t[:, :]), [ps], [KT])
                            ps = psK.next()
                            for s in range(4):
                                for c in range(4):
                                    P.op("pe", lambda e, c=c, s=s, h=h, lt=lt, ps=ps: e.matmul(ps.t[:, s * 128:(s + 1) * 128], lt.t[:, c, s * 128:(s + 1) * 128], wukv.t[:, c, h * 256 + 128:h * 256 + 256],
                                                                                      start=(c == 0), stop=(c == 3)), [lt, wukv], [ps])
                            P.op("dve", lambda e, ps=ps, tc_=tc_: e.tensor_copy(out=VV.t[:, tc_ * 4:(tc_ + 1) * 4, :], in_=ps.t[:, :].rearrange("p (s d) -> p s d", d=128)), [ps], [VV])
                        for qb in range(nq):
                            q0 = s0 + qb * T
                            qn = qnr.next()
                            qr = qrr.next()
                            P.dma("sp", qn.t[:], QT[h, 0:128, q0:q0 + T], [QTb], [qn])
                            P.dma("sp", qr.t[:], QT[h, 128:192, q0:q0 + T], [QTb], [qr])
                            po = psO.next()
                            pd = psD.next()
                            accs = (accr.next(), accr.next(), accr.next())

                            def qk(kc, qn=qn, qr=qr):
                                ps = psS.next()
                                P.op("pe", lambda e: e.matmul(ps.t[:, :], KT.t[:, kc * 128:(kc + 1) * 128], qn.t[:], start=True, stop=False), [KT, qn], [ps])
                                P.op("pe", lambda e: e.matmul(ps.t[:, :], krT.t[:, kc * 128:(kc + 1) * 128], qr.t[:], start=False, stop=True), [krT, qr], [ps])
                                return ps
                            pending = [qk(0), qk(1)]
                            for kc in range(nkc):
                                if kc + 2 < nkc:
                                    pending.append(qk(kc + 2))
                                pscur = pending.pop(0)
                                pt = pr.next()
                                P.op("act", lambda e, pscur=pscur, pt=pt: e.activation(out=pt.t[:], in_=pscur.t[:, :], func=AF.Exp, scale=MLA_SCALE), [pscur], [pt])
                                P.op("pe", lambda e, kc=kc, pt=pt: e.matmul(po.t[:, :], VV.t[:, kc, :], pt.t[:], start=(kc == 0), stop=(kc == nkc - 1)), [VV, pt], [po])
                                acc = accs[kc % 3]
                                aeng = "pool" if kc % 3 == 2 else "dve"
                                if kc < 3:
                                    P.op(aeng, lambda e, pt=pt, acc=acc: e.tensor_copy(out=acc.t[:], in_=pt.t[:]), [pt], [acc])
                                else:
                                    P.op(aeng, lambda e, pt=pt, acc=acc: e.tensor_tensor(out=acc.t[:], in0=acc.t[:], in1=pt.t[:], op=ALU.add), [pt, acc], [acc])
                            for ai in range(3):
                                P.op("pe", lambda e, pd=pd, ai=ai: e.matmul(pd.t[:, :], onesF.t[:], accs[ai].t[:], start=(ai == 0), stop=(ai == 2)), [onesF, accs[ai]], [pd])
                            rd = rdr.next()
                            ob = obr.next()
                            P.op("dve", lambda e, rd=rd, pd=pd: e.reciprocal(out=rd.t[:], in_=pd.t[:, :]), [pd], [rd])
                            P.op("dve", lambda e, rd=rd, ob=ob, po=po: e.tensor_tensor(out=ob.t[:], in0=po.t[:, :], in1=rd.t[:], op=ALU.mult), [po, rd], [ob])
                            P.dma("pool", OA[h, :, q0:q0 + T], ob.t[:], [ob], [OAb])
                P.end_phase()

            with ExitStack() as sc:
                SMAX = max(S_P, S_S)
                gkT = P.tile("gkT", [128, SMAX], BF16, sc)
                gvv = P.tile("gvv", [128, SMAX // 128, 128], BF16, sc)
                gqr = Rot([P.tile(f"gq{i}", [128, 4, T], BF16, sc) for i in range(2)])
                tmr = Rot([P.tile(f"wt{i}", [128, 4, 128], F32, sc) for i in range(4)])
                pr = Rot([P.tile(f"wp{i}", [128, 4, 128], BF16, sc) for i in range(6)])
                dnr = Rot([P.tile(f"dn{i}", [128, 4, 128], F32, sc) for i in range(2)])
                oor = Rot([P.tile(f"oo{i}", [128, 4, T], BF16, sc) for i in range(2)])
                psS = Rot(PS[0:3])
                psO = Rot(PS[3:5])
                psD = Rot(PS[5:7])
                for (sname, s0, slen) in segs:
                    nb = slen // 128
                    nqb = (slen if (l == 0 or sname == "S") else OWN) // 128
                    b0 = s0 // 128
                    for kv in range(2):
                        P.dma("sp", gkT.t[:, 0:slen], GK[kv, :, s0:s0 + slen], [GKb], [gkT])
                        P.dma("sp", gvv.t[:, 0:nb, :], GV[s0:s0 + slen, kv * 128:(kv + 1) * 128].rearrange("(n p) d -> p n d", p=128), [GVb], [gvv])
                        for n in range(nqb):
                            n4 = n % 4
                            if n4 == 0:
                                gq = gqr.next()
                                for g in range(4):
                                    P.dma("sp", gq.t[:, g, :], GQ[kv * 4 + g, :, s0 + n * 128:s0 + n * 128 + T], [GQb], [gq])
                                oo = oor.next()
                            po = psO.next()
                            pd = psD.next()
                            pts = []
                            for jb in range(3):
                                m = (n + jb - 1) % nb
                                ps = psS.next()
                                P.op("pe", lambda e, m=m, n4=n4, gq=gq, ps=ps: e.matmul(ps.t[:, :].rearrange("p (g q) -> p g q", q=128), gkT.t[:, m * 128:(m + 1) * 128], gq.t[:, :, n4 * 128:(n4 + 1) * 128],
                                                                              start=True, stop=True), [gkT, gq], [ps])
                                tm = tmr.next()
                                P.op("dve", lambda e, ps=ps, tm=tm, jb=jb: e.scalar_tensor_tensor(out=tm.t[:], in0=ps.t[:, :].rearrange("p (g q) -> p g q", q=128), scalar=GQA_SCALE, in1=BT[kv][jb].t[:],
                                                                                               op0=ALU.mult, op1=ALU.add), [ps, BT[kv][jb]], [tm])
                                pt = pr.next()
                                if jb == 1:
                                    P.op("act", lambda e, tm=tm, pt=pt: e.activation(out=pt.t[:], in_=tm.t[:], func=AF.Exp), [tm], [pt])
                                else:
                                    col = 2 * (b0 + n) + (0 if jb == 0 else 1)
                                    P.op("act", lambda e, tm=tm, pt=pt, col=col: e.activation(out=pt.t[:], in_=tm.t[:], func=AF.Exp, bias=EM.t[:, col:col + 1]), [tm, EM], [pt])
                                pts.append((m, pt))
                            for jb, (m, pt) in enumerate(pts):
                                P.op("pe", lambda e, m=m, pt=pt, jb=jb, po=po: e.matmul(po.t[:, :].rearrange("p (g q) -> p g q", q=128), gvv.t[:, m, :], pt.t[:], start=(jb == 0), stop=(jb == 2)), [gvv, pt], [po])
                                P.op("pe", lambda e, pt=pt, jb=jb, pd=pd: e.matmul(pd.t[:, :].rearrange("p (g q) -> p g q", q=128), onesB.t[:], pt.t[:], start=(jb == 0), stop=(jb == 2)), [onesB, pt], [pd])
                            dn = dnr.next()
                            for g in range(4):
                                P.op("dve", lambda e, g=g, dn=dn, pd=pd: e.tensor_scalar_add(out=dn.t[:, g, :], in0=pd.t[:, g * 128:(g + 1) * 128], scalar1=ESK[l].t[:, kv * 4 + g:kv * 4 + g + 1]), [pd, ESK[l]], [dn])
                            P.op("dve", lambda e, dn=dn: e.reciprocal(out=dn.t[:], in_=dn.t[:]), [dn], [dn])
                            P.op("dve", lambda e, dn=dn, po=po, oo=oo, n4=n4: e.tensor_tensor(out=oo.t[:, :, n4 * 128:(n4 + 1) * 128], in0=po.t[:, :].rearrange("p (g q) -> p g q", q=128), in1=dn.t[:], op=ALU.mult),
                                 [po, dn], [oo])
                            if n4 == 3:
                                for g in range(4):
                                    P.dma("pool", OB[kv * 4 + g, :, s0 + (n - 3) * 128:s0 + (n - 3) * 128 + T], oo.t[:, g, :], [oo], [OBb])
                P.end_phase()

            with ExitStack() as sc:
                XS = P.tile("XS4", [128, KC, T], F32, sc)
                sqr = Rot([P.tile(f"sq{i}", [128, T], BF16, sc) for i in range(3)])
                Rb = P.tile("R4", [128, T], F32, sc)
                tmpr = Rot([P.tile(f"tmp{i}", [128, T], F32, sc) for i in range(3)])
                hb = P.tile("h4", [128, KC, T], BF16, sc)
                act = P.tile("act", [128, max(FC, 16), T], BF16, sc)
                wgu = Rot([P.tile(f"wgu{i}", [128, KC, 256], BF16, sc) for i in range(4)])
                wdr = Rot([P.tile(f"wd{i}", [128, FC, 128], BF16, sc) for i in range(3)])
                moe = (l % 2 == 1)
                if moe:
                    gbt = P.tile("gb", [128, NE, T], BF16, sc)
                    h32r = Rot([P.tile(f"h32{i}", [128, T], F32, sc) for i in range(2)])
                    wr32 = P.tile("wr32", [128, KC, NE], F32, sc)
                    P.dma("sp", wr32.t[:], w_router[0].rearrange("(k p) n -> p k n", p=128), [CONST], [wr32])
                    lg = P.tile("lg", [128, 4, NE], F32, sc)
                    sm = P.tile("sm", [128, 16], F32, sc)
                    l2 = P.tile("l2", [128, 4, NE], F32, sc)
                    gts = P.tile("gts", [128, 4, NE], F32, sc)
                    gT = P.tile("gT", [8, T], F32, sc)
                OAt = act.t[:, 0:8, :]
                OBt = act.t[:, 8:16, :]

                for bi, blk in enumerate(own_blocks):
                    seg = 0 if blk * T < S_P else 1
                    t0 = blk * T
                    P.dma("sp", XS.t[:], XT[blk], [XTb[blk]], [XS])
                    P.dma("sp", OAt, OA[:, :, t0:t0 + T].rearrange("h p t -> p h t"), [OAb], [act])
                    P.dma("sp", OBt, OB[:, :, t0:t0 + T].rearrange("h p t -> p h t"), [OBb], [act])
                    for grp in range(2):
                        src = OAt if grp == 0 else OBt
                        ps = psr.next()
                        for c in range(8):
                            sq = sqr.next()
                            P.op("act", lambda e, c=c, sq=sq, src=src: e.activation(out=sq.t[:], in_=src[:, c, :], func=AF.Square), [act], [sq])
                            P.op("pe", lambda e, c=c, sq=sq, ps=ps: e.matmul(ps.t[:, :], onesB.t[:], sq.t[:], start=(c == 0), stop=(c == 7)), [onesB, sq], [ps])
                        P.op("act", lambda e, ps=ps: e.activation(out=Rb.t[:], in_=ps.t[:, :], func=AF.Sqrt, bias=epsb(EPS * 1024), scale=1.0), [ps, EPSB], [Rb])
                        P.op("dve", lambda e: e.reciprocal(out=Rb.t[:], in_=Rb.t[:]), [Rb], [Rb])
                        go = 24 * l + 8 + 8 * grp
                        for c in range(8):
                            tm = tmpr.next()
                            P.op("dve", lambda e, c=c, tm=tm, src=src: e.tensor_tensor(out=tm.t[:], in0=src[:, c, :], in1=Rb.t[:], op=ALU.mult), [act, Rb], [tm])
                            P.op("act", lambda e, c=c, tm=tm, go=go, grp=grp: e.activation(out=hb.t[:, grp * 8 + c, :], in_=tm.t[:], func=AF.Identity, scale=GL.t[:, go + c:go + c + 1]), [tm, GL], [hb])
                    for dg in range(8):
                        wt = wgu.next()
                        P.dma("pool", wt.t[:], w_out[l][:, dg * 256:(dg + 1) * 256].rearrange("(k p) n -> p k n", p=128), [CONST], [wt])
                        for dd in range(2):
                            dc = dg * 2 + dd
                            ps = psr.next()
                            for k in range(KC):
                                P.op("pe", lambda e, k=k, dd=dd, wt=wt, ps=ps: e.matmul(ps.t[:, :], wt.t[:, k, dd * 128:(dd + 1) * 128], hb.t[:, k, :], start=(k == 0), stop=(k == KC - 1)), [wt, hb], [ps])
                            P.op("dve", lambda e, dc=dc, ps=ps, seg=seg: e.scalar_tensor_tensor(out=XS.t[:, dc, :], in0=ps.t[:, :], scalar=MOD[l][seg].t[:, 32 + dc:33 + dc], in1=XS.t[:, dc, :],
                                                                                     op0=ALU.mult, op1=ALU.add), [ps, MOD[l][seg], XS], [XS])
                    rstd_from_sq([(XS.t[:, k, :], XS) for k in range(KC)], KC, Rb, sqr, EPS * D)
                    if moe:
                        psl = psr.next()
                    for k in range(KC):
                        tm = tmpr.next()
                        P.op("dve", lambda e, k=k, tm=tm: e.tensor_tensor(out=tm.t[:], in0=XS.t[:, k, :], in1=Rb.t[:], op=ALU.mult), [XS, Rb], [tm])
                        if not moe:
                            P.op("act", lambda e, k=k, tm=tm, seg=seg: e.activation(out=hb.t[:, k, :], in_=tm.t[:], func=AF.Identity, bias=MOD[l][seg].t[:, 48 + k:49 + k], scale=A2[l][seg].t[:, k:k + 1]),
                                 [tm, MOD[l][seg], A2[l][seg]], [hb])
                        else:
                            h32 = h32r.next()
                            P.op("act", lambda e, k=k, tm=tm, seg=seg, h32=h32: e.activation(out=h32.t[:], in_=tm.t[:], func=AF.Identity, bias=MOD[l][seg].t[:, 48 + k:49 + k], scale=A2[l][seg].t[:, k:k + 1]),
                                 [tm, MOD[l][seg], A2[l][seg]], [h32])
                            P.op("dve", lambda e, k=k, h32=h32: e.tensor_copy(out=hb.t[:, k, :], in_=h32.t[:]), [h32], [hb])
                            P.op("pe", lambda e, k=k, h32=h32: e.matmul(psl.t[0:NE, :], wr32.t[:, k, :], h32.t[:], start=(k == 0), stop=(k == KC - 1)), [h32, wr32], [psl])
                    if moe:
                        P.op("dve", lambda e: e.tensor_copy(out=gT.t[:], in_=psl.t[0:NE, :]), [psl], [gT])
                        ps = psr.next()
                        for s in range(4):
                            P.op("pe", lambda e, s=s, ps=ps: e.transpose(ps.t[:, s * NE:(s + 1) * NE], gT.t[0:NE, s * 128:(s + 1) * 128], identF.t[0:NE, 0:NE]), [gT, identF], [ps])
                        P.op("dve", lambda e, ps=ps: e.tensor_copy(out=lg.t[:], in_=ps.t[:, 0:4 * NE].rearrange("p (s n) -> p s n", n=NE)), [ps], [lg])
                        for s in range(4):
                            P.op("dve", lambda e, s=s: e.tensor_reduce(out=sm.t[:, s:s + 1], in_=lg.t[:, s, :], axis=AX.X, op=ALU.max), [lg], [sm])
                            P.op("dve", lambda e, s=s: e.tensor_scalar(out=l2.t[:, s, :], in0=lg.t[:, s, :], scalar1=sm.t[:, s:s + 1], scalar2=-1.0e30, op0=ALU.is_equal, op1=ALU.mult), [lg, sm], [l2])
                            P.op("dve", lambda e, s=s: e.tensor_tensor(out=l2.t[:, s, :], in0=l2.t[:, s, :], in1=lg.t[:, s, :], op=ALU.add), [l2, lg], [l2])
                            P.op("dve", lambda e, s=s: e.tensor_reduce(out=sm.t[:, 4 + s:5 + s], in_=l2.t[:, s, :], axis=AX.X, op=ALU.max), [l2], [sm])
                            P.op("dve", lambda e, s=s: e.tensor_scalar_mul(out=sm.t[:, 8 + s:9 + s], in0=sm.t[:, s:s + 1], scalar1=-1.0), [sm], [sm])
                            P.op("act", lambda e, s=s: e.activation(out=gts.t[:, s, :], in_=lg.t[:, s, :], func=AF.Exp, bias=sm.t[:, 8 + s:9 + s]), [lg, sm], [gts])
                            P.op("dve", lambda e, s=s: e.scalar_tensor_tensor(out=gts.t[:, s, :], in0=lg.t[:, s, :], scalar=sm.t[:, 4 + s:5 + s], in1=gts.t[:, s, :], op0=ALU.is_ge, op1=ALU.mult), [lg, sm, gts], [gts])
                            P.op("dve", lambda e, s=s: e.tensor_reduce(out=sm.t[:, 12 + s:13 + s], in_=gts.t[:, s, :], axis=AX.X, op=ALU.add), [gts], [sm])
                            P.op("dve", lambda e, s=s: e.reciprocal(out=sm.t[:, 12 + s:13 + s], in_=sm.t[:, 12 + s:13 + s]), [sm], [sm])
                            P.op("dve", lambda e, s=s: e.tensor_scalar_mul(out=gts.t[:, s, :], in0=gts.t[:, s, :], scalar1=sm.t[:, 12 + s:13 + s]), [gts, sm], [gts])
                        ps = psr.next()
                        for s in range(4):
                            P.op("pe", lambda e, s=s, ps=ps: e.transpose(ps.t[0:NE, s * 128:(s + 1) * 128], gts.t[:, s, :], identF.t[:]), [gts, identF], [ps])
                        P.op("dve", lambda e, ps=ps: e.tensor_copy(out=gT.t[:], in_=ps.t[0:NE, :]), [ps], [gT])
                        for e_ in range(NE):
                            ps = psr.next()
                            P.op("pe", lambda e, e_=e_, ps=ps: e.matmul(ps.t[:, :], SEL.t[0:NE, e_, :], gT.t[:], start=True, stop=True), [SEL, gT], [ps])
                            P.op("act", lambda e, e_=e_, ps=ps: e.copy(out=gbt.t[:, e_, :], in_=ps.t[:, :]), [ps], [gbt])
                    for ex in range(NE if moe else 1):
                        exi = (1 + ex) if moe else 0
                        wsb = WSe if moe else WSd
                        for fg in range(FC // 2):
                            wg_t = wgu.next()
                            wu_t = wgu.next()
                            P.dma("sp", wg_t.t[:], WGS[exi, fg].rearrange("p (k n) -> p k n", n=256), [wsb], [wg_t])
                            P.dma("sp", wu_t.t[:], WUS[exi, fg].rearrange("p (k n) -> p k n", n=256), [wsb], [wu_t])
                            for ff in range(2):
                                fc = fg * 2 + ff
                                pg = psr.next()
                                pu = psr.next()
                                for k in range(KC):
                                    P.op("pe", lambda e, k=k, ff=ff, wg_t=wg_t, pg=pg: e.matmul(pg.t[:, :], wg_t.t[:, k, ff * 128:(ff + 1) * 128], hb.t[:, k, :], start=(k == 0), stop=(k == KC - 1)), [wg_t, hb], [pg])
                                for k in range(KC):
                                    P.op("pe", lambda e, k=k, ff=ff, wu_t=wu_t, pu=pu: e.matmul(pu.t[:, :], wu_t.t[:, k, ff * 128:(ff + 1) * 128], hb.t[:, k, :], start=(k == 0), stop=(k == KC - 1)), [wu_t, hb], [pu])
                                tm = tmpr.next()
                                P.op("act", lambda e, pg=pg, tm=tm: e.activation(out=tm.t[:], in_=pg.t[:, :], func=AF.Silu), [pg], [tm])
                                if moe:
                                    tm2 = tmpr.next()
                                    P.op("dve", lambda e, pu=pu, tm=tm, tm2=tm2: e.tensor_tensor(out=tm2.t[:], in0=pu.t[:, :], in1=tm.t[:], op=ALU.mult), [pu, tm], [tm2])
                                    P.op("dve", lambda e, fc=fc, tm2=tm2, ex=ex: e.tensor_tensor(out=act.t[:, fc, :], in0=tm2.t[:], in1=gbt.t[:, ex, :], op=ALU.mult), [tm2, gbt], [act])
                                else:
                                    P.op("dve", lambda e, fc=fc, pu=pu, tm=tm: e.tensor_tensor(out=act.t[:, fc, :], in0=pu.t[:, :], in1=tm.t[:], op=ALU.mult), [pu, tm], [act])
                        for dc in range(KC):
                            wd_t = wdr.next()
                            P.dma("sp", wd_t.t[:], WDS[exi, dc].rearrange("p (f n) -> p f n", n=128), [wsb], [wd_t])
                            ps = psr.next()
                            for fc in range(FC):
                                P.op("pe", lambda e, fc=fc, wd_t=wd_t, ps=ps: e.matmul(ps.t[:, :], wd_t.t[:, fc, :], act.t[:, fc, :], start=(fc == 0), stop=(fc == FC - 1)), [wd_t, act], [ps])
                            P.op("dve", lambda e, dc=dc, ps=ps, seg=seg: e.scalar_tensor_tensor(out=XS.t[:, dc, :], in0=ps.t[:, :], scalar=MOD[l][seg].t[:, 80 + dc:81 + dc], in1=XS.t[:, dc, :],
                                                                                     op0=ALU.mult, op1=ALU.add), [ps, MOD[l][seg], XS], [XS])
                    if not last:
                        P.dma("sp", XT[blk], XS.t[:], [XS], [XTb[blk]])
                    elif seg == 1 or blk * T < OWN:
                        rstd_from_sq([(XS.t[:, k, :], XS) for k in range(KC)], KC, Rb, sqr, EPS * D)
                        go = 24 * DEPTH
                        for k in range(KC):
                            tm = tmpr.next()
                            P.op("dve", lambda e, k=k, tm=tm: e.tensor_tensor(out=tm.t[:], in0=XS.t[:, k, :], in1=Rb.t[:], op=ALU.mult), [XS, Rb], [tm])
                            P.op("act", lambda e, k=k, tm=tm, go=go: e.activation(out=XS.t[:, k, :], in_=tm.t[:], func=AF.Identity, scale=GL.t[:, go + k:go + k + 1]), [tm, GL, XS], [XS])
                        yo = act.t[:, 0:16, :].rearrange("p a b -> p (a b)").bitcast(F32)
                        orow = (blk * T) if seg == 0 else (OWN + blk * T - S_P)
                        for s in range(4):
                            half = s % 2
                            for k4 in range(4):
                                ps = psr.next()
                                for kk in range(4):
                                    k = k4 * 4 + kk
                                    P.op("pe", lambda e, k=k, kk=kk, s=s, ps=ps: e.transpose(ps.t[:, kk * 128:(kk + 1) * 128], XS.t[:, k, s * 128:(s + 1) * 128], identF.t[:]), [XS, identF], [ps])
                                P.op("act" if k4 % 2 else "dve", lambda e, k4=k4, half=half, ps=ps: (e.copy if e is nc.scalar else e.tensor_copy)(out=yo[:, half * 2048 + k4 * 512:half * 2048 + (k4 + 1) * 512], in_=ps.t[:, :]), [ps], [act])
                            P.dma("sp", y_out[orow + s * 128:orow + (s + 1) * 128, :], yo[:, half * 2048:(half + 1) * 2048], [act], [Yb])
                P.end_phase()

        P._wait_all(P.E["sp"], {Yb.dsemname: Yb.dcnt})
    return nc


def _host_tables(S_P, S_S, shift):
    import jax
    import jax.numpy as jnp
    cpu = jax.devices("cpu")[0]
    with jax.default_device(cpu):
        def rope(S):
            pos = jnp.arange(S, dtype=jnp.float32)
            inv = 1.0 / (10000.0 ** (jnp.arange(0, 64, 2, dtype=jnp.float32) / 64))
            ang = pos[:, None] * inv[None, :]
            return np.asarray(jnp.cos(ang)), np.asarray(jnp.sin(ang))
        cP, sP = rope(S_P)
        cS, sS = rope(S_S)
        rel = jnp.arange(-255, 256, dtype=jnp.int32)
        nb = 16
        ret = (rel > 0).astype(jnp.int32) * nb
        n = jnp.abs(rel)
        max_exact = nb // 2
        nf = jnp.maximum(n, 1).astype(jnp.float32)
        large = max_exact + (jnp.log(nf / max_exact) / math.log(128 / max_exact) * (nb - max_exact)).astype(jnp.int32)
        large = jnp.minimum(large, nb - 1)
        bucket = np.asarray(ret + jnp.where(n < max_exact, n, large))
    relv = np.arange(-255, 256)
    idx = (np.arange(S_P) + shift) % S_P
    cos = np.concatenate([cP[idx], cS], 0).T
    sin = np.concatenate([sP[idx], sS], 0).T
    rope_t = np.zeros((64, 2, S_P + S_S), np.float32)
    rope_t[0:32, 0] = cos
    rope_t[32:64, 0] = cos
    rope_t[0:32, 1] = -sin
    rope_t[32:64, 1] = sin
    nbP, nbS = S_P // 128, S_S // 128
    em = np.zeros((128, 2 * (nbP + nbS)), np.float32)
    for b in range(nbP):
        gb = (b + shift // 128) % nbP
        if gb == 0:
            em[:, 2 * b] = NEG
        if gb == nbP - 1:
            em[:, 2 * b + 1] = NEG
    em[:, 2 * nbP] = NEG
    em[:, 2 * (nbP + nbS) - 1] = NEG
    oh = np.zeros((33, 512), np.float32)
    for kp in range(511):
        r = 255 - kp
        if abs(r) <= 128:
            oh[bucket[r + 255], kp] = 1.0
        else:
            oh[32, kp] = 1.0
    oh[32, 511] = 1.0
    return rope_t, em, oh


_PERM = np.concatenate([np.arange(32, 64), np.arange(0, 32)])


def make_in_maps(inputs, cfg, n_cores=8):
    S_P, S_S, OWN = cfg["S_P"], cfg["S_S"], cfg["OWN"]
    f = lambda a: np.ascontiguousarray(np.asarray(a, dtype=np.float32))
    shared = {k: f(inputs[k]) for k in ("rel_bias", "w_ada", "b_ada", "g_norm_mix", "g_norm_ffn", "w_in", "g_q_lat", "w_uq", "g_kv_lat", "w_ukv",
                                         "sink", "g_out_a", "g_out_b", "w_out", "w_gate_d", "w_up_d", "w_down_d", "w_router", "w_gate_e", "w_up_e",
                                         "w_down_e", "g_final")}
    w_in = shared["w_in"]
    shared["w_krs"] = np.ascontiguousarray(w_in[:, :, 1024:1088][:, :, _PERM])
    wq = shared["w_uq"].reshape(w_in.shape[0], 512, 8, 192)
    shared["w_uqs"] = np.ascontiguousarray(wq[:, :, :, 128:192][:, :, :, _PERM].reshape(w_in.shape[0], 512, 512))
    shared["ident_in"] = np.eye(128, dtype=np.float32)
    xp, xs = f(inputs["x_prompt"]), f(inputs["x_sample"])
    cp, csmp = f(inputs["c_prompt"]), f(inputs["c_sample"])
    per_group = n_cores // xp.shape[0]
    maps = []
    for c in range(n_cores):
        b = c // per_group
        r = c % per_group
        shift = r * OWN
        idx = (np.arange(S_P) + shift) % S_P
        rope_t, em, oh = _host_tables(S_P, S_S, shift)
        m = dict(shared)
        m["x_in"] = np.ascontiguousarray(np.concatenate([xp[b][idx], xs[c]], 0))
        m["c_in"] = np.ascontiguousarray(np.stack([cp[b], csmp[c]], 0))
        m["rope_in"] = rope_t
        m["em_in"] = em
        m["oh_in"] = oh
        maps.append(m)
    return maps


CFG_FULL = dict(S_P=16384, S_S=2048, OWN=4096, FF=5632, NE=8, DEPTH=2)


def kernel(**inputs):
    return run(inputs, CFG_FULL)


def run(inputs, cfg):
    nc = build(cfg)
    maps = make_in_maps(inputs, cfg)
    res = run_bass_kernel_spmd(nc, maps, core_ids=list(range(8)))
    S_P, S_S, OWN = cfg["S_P"], cfg["S_S"], cfg["OWN"]
    yp = np.zeros((2, S_P, D), np.float32)
    ys = np.zeros((8, S_S, D), np.float32)
    for c in range(8):
        y = np.asarray(res.results[c]["y_out"])
        b, r = c // 4, c % 4
        yp[b, r * OWN:(r + 1) * OWN] = y[0:OWN]
        ys[c] = y[OWN:OWN + S_S]
    return (yp, ys)
```

```python
import math
from contextlib import ExitStack
import numpy as np
import concourse.bass as bass
import concourse.mybir as mybir
from concourse.bass_utils import run_bass_kernel_spmd

F32 = mybir.dt.float32
BF16 = mybir.dt.bfloat16
AF = mybir.ActivationFunctionType
ALU = mybir.AluOpType
AX = mybir.AxisListType

D = 2048
KC = 16
EPS = 1e-6
H = 8
MLA_SCALE = 1.0 / math.sqrt(192.0)
GQA_SCALE = 1.0 / math.sqrt(128.0)
NEG = -1.0e4
T = 512


class Buf:
    def __init__(self, name, t=None):
        self.name = name
        self.t = t
        self.w = {}
        self.r = {}
        self.dsem = None
        self.dsemname = None
        self.dcnt = 0
        self.scoped = False


class Eng:
    def __init__(self, name, obj, sem, semname):
        self.name, self.obj, self.sem, self.semname = name, obj, sem, semname
        self.cnt = 0
        self.seen = {}


class Prog:
    def __init__(self, nc, es):
        self.nc, self.es = nc, es
        self.sems = {}
        self.E = {}
        for name, obj in (("pe", nc.tensor), ("act", nc.scalar), ("dve", nc.vector), ("pool", nc.gpsimd), ("sp", nc.sync)):
            sn = "c_" + name
            self.E[name] = Eng(name, obj, self.newsem(sn), sn)
        self.dbufs = []
        self.free_dsems = []
        self.nuid = 0

    def newsem(self, name):
        s = self.es.enter_context(self.nc.semaphore(name))
        self.sems[name] = s
        return s

    def tile(self, name, shape, dt, scope=None):
        self.nuid += 1
        t = (scope or self.es).enter_context(self.nc.sbuf_tensor(f"{name}_{self.nuid}", list(shape), dt))
        b = Buf(name, t)
        b.scoped = scope is not None
        return b

    def _wait(self, E, deps, defer_last=False):
        need = []
        for sn, val in deps.items():
            if E.seen.get(sn, 0) >= val:
                continue
            if sn == E.semname and E.name == "pe":
                continue
            need.append((sn, val))
            E.seen[sn] = val
        last = None
        if defer_last and need:
            last = need.pop()
        for sn, val in need:
            E.obj.wait_ge(self.sems[sn], val)
        return last

    @staticmethod
    def _merge(d, s, skip=None):
        for k, v in s.items():
            if k != skip and d.get(k, 0) < v:
                d[k] = v

    def op(self, e, fn, reads=(), writes=()):
        E = self.E[e]
        deps = {}
        for b in reads:
            self._merge(deps, b.w)
        for b in writes:
            self._merge(deps, b.w)
            self._merge(deps, b.r)
        last = self._wait(E, deps, defer_last=True)
        ins = fn(E.obj)
        if last is not None:
            ins._wait_ge(self.sems[last[0]], last[1])
        E.cnt += 1
        ins.then_inc(E.sem, 1)
        for b in writes:
            b.w[E.semname] = E.cnt
        for b in reads:
            b.r[E.semname] = E.cnt
        return ins

    def dma(self, q, out, in_, reads, writes, **kw):
        E = self.E[q]
        prim = writes[0]
        if prim.dsem is None:
            if self.free_dsems:
                prim.dsemname, prim.dsem, prim.dcnt = self.free_dsems.pop()
            else:
                self.nuid += 1
                prim.dsemname = f"d_{self.nuid}"
                prim.dsem = self.newsem(prim.dsemname)
            self.dbufs.append(prim)
        deps = {}
        for b in reads:
            self._merge(deps, b.w)
        for b in writes:
            self._merge(deps, b.r)
            self._merge(deps, b.w, skip=b.dsemname)
        last = self._wait(E, deps, defer_last=True)
        ins = E.obj.dma_start(out=out, in_=in_, **kw)
        if last is not None:
            ins._wait_ge(self.sems[last[0]], last[1])
        prim.dcnt += 16
        ins.then_inc(prim.dsem, 16)
        for b in writes:
            b.w[prim.dsemname] = prim.dcnt
        for b in reads:
            b.r[prim.dsemname] = prim.dcnt

    def barrier(self):
        deps = {}
        for E in self.E.values():
            if E.cnt:
                deps[E.semname] = E.cnt
        for b in self.dbufs:
            if b.dcnt:
                deps[b.dsemname] = b.dcnt
        for E in self.E.values():
            d = dict(deps)
            self._wait_all(E, d)

    def end_phase(self):
        self.barrier()
        keep = []
        for b in self.dbufs:
            if getattr(b, "scoped", False):
                self.free_dsems.append((b.dsemname, b.dsem, b.dcnt))
            else:
                keep.append(b)
        self.dbufs = keep

    def _wait_all(self, E, deps):
        for sn, val in deps.items():
            if E.seen.get(sn, 0) >= val:
                continue
            E.obj.wait_ge(self.sems[sn], val)
            E.seen[sn] = val


class Rot:
    def __init__(self, bufs):
        self.bufs, self.i = bufs, 0

    def next(self):
        b = self.bufs[self.i % len(self.bufs)]
        self.i += 1
        return b


def build(cfg):
    S_P, S_S, OWN, FF, NE, DEPTH = cfg["S_P"], cfg["S_S"], cfg["OWN"], cfg["FF"], cfg["NE"], cfg["DEPTH"]
    FC = FF // 128
    NTOK = S_P + S_S
    NBLK = NTOK // T
    segs = (("P", 0, S_P), ("S", S_P, S_S))
    nc = bass.Bass("TRN2", target_bir_lowering=False)

    def din(name, shape, dt=F32):
        return nc.dram_tensor(name, list(shape), dt, kind="ExternalInput").ap()

    def dscr(name, shape, dt):
        return nc.dram_tensor(name, list(shape), dt, kind="Internal").ap()

    x_in = din("x_in", [NTOK, D])
    c_in = din("c_in", [2, D])
    rope_in = din("rope_in", [64, 2, NTOK])
    em_in = din("em_in", [128, 2 * (NTOK // 128)])
    ident_in = din("ident_in", [128, 128])
    oh_in = din("oh_in", [33, 512])
    rel_bias = din("rel_bias", [32, 8])
    w_ada = din("w_ada", [DEPTH, D, 6 * D])
    b_ada = din("b_ada", [DEPTH, 6 * D])
    g_norm_mix = din("g_norm_mix", [DEPTH, D])
    g_norm_ffn = din("g_norm_ffn", [DEPTH, D])
    w_in = din("w_in", [DEPTH, D, 2624])
    w_krs = din("w_krs", [DEPTH, D, 64])
    g_q_lat = din("g_q_lat", [DEPTH, 512])
    w_uq = din("w_uq", [DEPTH, 512, 1536])
    w_uqs = din("w_uqs", [DEPTH, 512, 512])
    g_kv_lat = din("g_kv_lat", [DEPTH, 512])
    w_ukv = din("w_ukv", [DEPTH, 512, 2048])
    sink = din("sink", [DEPTH, 8])
    g_out_a = din("g_out_a", [DEPTH, 1024])
    g_out_b = din("g_out_b", [DEPTH, 1024])
    w_out = din("w_out", [DEPTH, D, D])
    w_gate_d = din("w_gate_d", [1, D, FF])
    w_up_d = din("w_up_d", [1, D, FF])
    w_down_d = din("w_down_d", [1, FF, D])
    w_router = din("w_router", [1, D, NE])
    w_gate_e = din("w_gate_e", [1, NE, D, FF])
    w_up_e = din("w_up_e", [1, NE, D, FF])
    w_down_e = din("w_down_e", [1, NE, FF, D])
    g_final = din("g_final", [D])
    NOUT = OWN + S_S
    y_out = nc.dram_tensor("y_out", [NOUT, D], F32, kind="ExternalOutput").ap()

    XT = dscr("XT", [NBLK, 128, KC, T], F32)
    QT = dscr("QT", [H, 192, NTOK], BF16)
    LAT = dscr("LAT", [576, NTOK], BF16)
    GQ = dscr("GQ", [H, 128, NTOK], BF16)
    GK = dscr("GK", [2, 128, NTOK], BF16)
    GV = dscr("GV", [NTOK, 256], BF16)
    OA = dscr("OA", [H, 128, NTOK], BF16)
    OB = dscr("OB", [H, 128, NTOK], BF16)
    TB = dscr("TB", [8, 128, 512], F32)
    NEX = 1 + NE
    WGS = dscr("WGS", [NEX, FC // 2, 128, KC * 256], BF16)
    WUS = dscr("WUS", [NEX, FC // 2, 128, KC * 256], BF16)
    WDS = dscr("WDS", [NEX, KC, 128, FC * 128], BF16)

    es = ExitStack()
    with es:
        P = Prog(nc, es)
        XTb = [Buf(f"XT{i % 6}") for i in range(6)] * (NBLK // 6 + 1)
        QTb, LATb, GQb, GKb, GVb, OAb, OBb, TBb, Yb = (Buf(n) for n in ("QT", "LAT", "GQ", "GK", "GV", "OA", "OB", "TB", "Y"))
        CONST = Buf("const")
        WSd, WSe = Buf("WSd"), Buf("WSe")
        conv = []
        for ex in range(NEX):
            Wg_ = w_gate_d[0] if ex == 0 else w_gate_e[0][ex - 1]
            Wu_ = w_up_d[0] if ex == 0 else w_up_e[0][ex - 1]
            Wd_ = w_down_d[0] if ex == 0 else w_down_e[0][ex - 1]
            wb = WSd if ex == 0 else WSe
            for fg in range(FC // 2):
                conv.append((WGS[ex, fg].rearrange("p (k n) -> p k n", n=256), Wg_[:, fg * 256:(fg + 1) * 256].rearrange("(k p) n -> p k n", p=128), wb))
                conv.append((WUS[ex, fg].rearrange("p (k n) -> p k n", n=256), Wu_[:, fg * 256:(fg + 1) * 256].rearrange("(k p) n -> p k n", p=128), wb))
            for dc in range(KC):
                conv.append((WDS[ex, dc].rearrange("p (f n) -> p f n", n=128), Wd_[:, dc * 128:(dc + 1) * 128].rearrange("(f p) n -> p f n", p=128), wb))
        conv.reverse()
        conv_per_blk = -(-len(conv) // NBLK)

        def issue_conv(n):
            for _ in range(n):
                if conv:
                    d_, s_, wb_ = conv.pop()
                    P.dma("pool", d_, s_, [CONST], [wb_])

        PS = []
        for i in range(8):
            t = es.enter_context(nc.psum_tensor(f"ps{i}", [128, 512], F32))
            PS.append(Buf(f"ps{i}", t))
        psr = Rot(PS)

        identF = P.tile("identF", [128, 128], F32)
        onesB = P.tile("onesB", [128, 128], BF16)
        onesF = P.tile("onesF", [128, 128], F32)
        vecA = [P.tile(f"vecA{l}", [128, 128], F32) for l in range(DEPTH)]
        vecB = P.tile("vecB", [128, 64], F32)
        MOD = [[P.tile(f"mod{l}{s}", [128, 96], F32) for s in range(2)] for l in range(DEPTH)]
        A1 = [[P.tile(f"a1{l}{s}", [128, 16], F32) for s in range(2)] for l in range(DEPTH)]
        A2 = [[P.tile(f"a2{l}{s}", [128, 16], F32) for s in range(2)] for l in range(DEPTH)]
        GL = P.tile("GL", [128, 64], F32)
        BT = [[P.tile(f"bt{kv}{jb}", [128, 4, 128], F32) for jb in range(3)] for kv in range(2)]
        EM = P.tile("EM", [128, 2 * (NTOK // 128)], F32)
        ESK = [P.tile(f"esk{l}", [128, 8], F32) for l in range(DEPTH)]
        SEL = P.tile("SEL", [8, NE, 128], F32)

        EPSB = P.tile("EPSB", [128, 3], F32)
        for i_, v_ in enumerate((EPS * D, EPS * 512, EPS * 1024)):
            P.op("dve", lambda e, i_=i_, v_=v_: e.memset(EPSB.t[:, i_:i_ + 1], v_), [], [EPSB])

        def epsb(v):
            i_ = {EPS * D: 0, EPS * 512: 1, EPS * 1024: 2}[v]
            return EPSB.t[:, i_:i_ + 1]
        P.dma("sp", identF.t[:], ident_in[:, :], [CONST], [identF])
        P.dma("sp", EM.t[:], em_in[:, :], [CONST], [EM])
        P.op("dve", lambda e: e.memset(onesB.t[:], 1.0), [], [onesB])
        P.op("dve", lambda e: e.memset(onesF.t[:], 1.0), [], [onesF])

        def transpose_rows(scope, rows_aps, out_buf, ncols):
            st = P.tile("stg", [128, 128], F32, scope)
            P.op("dve", lambda e: e.memset(st.t[:], 0.0), [], [st])
            r0 = 0
            for ap, r in rows_aps:
                P.dma("sp", st.t[r0:r0 + r, :], ap, [CONST], [st])
                r0 += r
            ps = psr.next()
            P.op("pe", lambda e: e.transpose(ps.t[:, 0:128], st.t[:], identF.t[:]), [st, identF], [ps])
            P.op("dve", lambda e: e.tensor_copy(out=out_buf.t[:, 0:ncols], in_=ps.t[:, 0:ncols]), [ps], [out_buf])

        def bcast_row(src_ap, n, out_ap, out_buf, scope, func=None):
            row = P.tile("row", [1, n], F32, scope)
            P.dma("sp", row.t[:], src_ap, [CONST], [row])
            ps = psr.next()
            P.op("pe", lambda e: e.matmul(ps.t[:, 0:n], onesF.t[0:1, :], row.t[:], start=True, stop=True), [onesF, row], [ps])
            if func is None:
                P.op("dve", lambda e: e.tensor_copy(out=out_ap, in_=ps.t[:, 0:n]), [ps], [out_buf])
            else:
                P.op("act", lambda e: e.activation(out=out_ap, in_=ps.t[:, 0:n], func=func), [ps], [out_buf])

        with ExitStack() as sc:
            for l in range(DEPTH):
                transpose_rows(sc, [(b_ada[l].rearrange("(c p) -> c p", p=128), 96),
                                    (g_norm_mix[l].rearrange("(c p) -> c p", p=128), 16),
                                    (g_norm_ffn[l].rearrange("(c p) -> c p", p=128), 16)], vecA[l], 128)
            rows = []
            for l in range(DEPTH):
                rows += [(g_q_lat[l].rearrange("(c p) -> c p", p=128), 4), (g_kv_lat[l].rearrange("(c p) -> c p", p=128), 4),
                         (g_out_a[l].rearrange("(c p) -> c p", p=128), 8), (g_out_b[l].rearrange("(c p) -> c p", p=128), 8)]
            rows += [(g_final.rearrange("(c p) -> c p", p=128), 16)]
            transpose_rows(sc, rows, vecB, 24 * DEPTH + 16)
            for l in range(DEPTH):
                o = 24 * l
                P.op("dve", lambda e, o=o: e.tensor_scalar_mul(out=GL.t[:, o:o + 8], in0=vecB.t[:, o:o + 8], scalar1=math.sqrt(512.0)), [vecB], [GL])
                P.op("dve", lambda e, o=o: e.tensor_scalar_mul(out=GL.t[:, o + 8:o + 24], in0=vecB.t[:, o + 8:o + 24], scalar1=math.sqrt(1024.0)), [vecB], [GL])
            o = 24 * DEPTH
            P.op("dve", lambda e: e.tensor_scalar_mul(out=GL.t[:, o:o + 16], in0=vecB.t[:, o:o + 16], scalar1=math.sqrt(float(D))), [vecB], [GL])

            for l in range(DEPTH):
                bcast_row(sink[l:l + 1, :], 8, ESK[l].t[:, :], ESK[l], sc, func=AF.Exp)
            for e_ in range(NE):
                P.op("dve", lambda e, e_=e_: e.tensor_scalar_mul(out=SEL.t[0:8, e_, :], in0=onesF.t[0:8, :], scalar1=identF.t[0:8, e_:e_ + 1]),
                     [onesF, identF], [SEL])

            rb = P.tile("rb", [33, 8], F32, sc)
            P.op("dve", lambda e: e.memset(rb.t[:], NEG), [], [rb])
            P.dma("sp", rb.t[0:32, :], rel_bias[:, :], [CONST], [rb])
            ohs = P.tile("ohs", [33, 512], F32, sc)
            P.dma("sp", ohs.t[:], oh_in[:, :], [CONST], [ohs])
            onesF33 = P.tile("ones33", [33, 128], F32, sc)
            P.op("dve", lambda e: e.memset(onesF33.t[:], 1.0), [], [onesF33])
            for h in range(8):
                lh = P.tile("lh", [33, 128], F32, sc)
                P.op("dve", lambda e, h=h, lh=lh: e.tensor_scalar_mul(out=lh.t[:], in0=onesF33.t[:], scalar1=rb.t[:, h:h + 1]), [onesF33, rb], [lh])
                ps = psr.next()
                P.op("pe", lambda e, lh=lh, ps=ps: e.matmul(ps.t[:, :], lh.t[:], ohs.t[:], start=True, stop=True), [lh, ohs], [ps])
                tr = P.tile("tr", [128, 512], F32, sc)
                P.op("dve", lambda e, ps=ps, tr=tr: e.tensor_copy(out=tr.t[:], in_=ps.t[:]), [ps], [tr])
                P.dma("sp", TB[h], tr.t[:], [tr], [TBb])
            for h in range(8):
                kv, g = h // 4, h % 4
                flat = TB[h].rearrange("p c -> (p c)")
                for jb in range(3):
                    cpr = 383 - 128 * jb
                    src = bass.AP(tensor=flat.tensor, offset=flat.offset + cpr, ap=[[511, 128], [1, 128]])
                    P.dma("sp", BT[kv][jb].t[:, g, :], src, [TBb], [BT[kv][jb]])

            cin = P.tile("cin", [2, D], F32, sc)
            P.dma("sp", cin.t[:], c_in[:, :], [CONST], [cin])
            csl = P.tile("csl", [2, D], F32, sc)
            P.op("act", lambda e: e.activation(out=csl.t[:], in_=cin.t[:], func=AF.Silu), [cin], [csl])
            csT = P.tile("csT", [128, KC, 2], BF16, sc)
            ps = psr.next()
            for k in range(KC):
                P.op("pe", lambda e, k=k: e.transpose(ps.t[:, 2 * k:2 * k + 2], csl.t[0:2, k * 128:(k + 1) * 128], identF.t[0:2, 0:2]), [csl, identF], [ps])
            P.op("dve", lambda e: e.tensor_copy(out=csT.t[:].rearrange("p k b -> p (k b)"), in_=ps.t[:, 0:2 * KC]), [ps], [csT])
            wa = Rot([P.tile(f"wa{i}", [128, KC, 512], BF16, sc) for i in range(3)])
            for l in range(DEPTH):
                psm = psr.next()
                for cg in range(24):
                    wt = wa.next()
                    P.dma("pool", wt.t[:], w_ada[l][:, cg * 512:(cg + 1) * 512].rearrange("(k p) n -> p k n", p=128), [CONST], [wt])
                    for oc in range(4):
                        occ = cg * 4 + oc
                        for k in range(KC):
                            P.op("pe", lambda e, k=k, oc=oc, occ=occ, wt=wt: e.matmul(psm.t[:, 2 * occ:2 * occ + 2], wt.t[:, k, oc * 128:(oc + 1) * 128], csT.t[:, k, :],
                                                                          start=(k == 0), stop=(k == KC - 1)), [wt, csT], [psm])
                pv = psm.t[:, 0:192].rearrange("p (c b) -> p c b", b=2)
                for s in range(2):
                    P.op("dve", lambda e, s=s, l=l: e.tensor_tensor(out=MOD[l][s].t[:], in0=pv[:, :, s], in1=vecA[l].t[:, 0:96], op=ALU.add), [psm, vecA[l]], [MOD[l][s]])
                    for (A, c0, g0) in ((A1, 16, 96), (A2, 64, 112)):
                        P.op("dve", lambda e, A=A, c0=c0, s=s, l=l: e.tensor_scalar(out=A[l][s].t[:], in0=MOD[l][s].t[:, c0:c0 + 16], scalar1=1.0, scalar2=math.sqrt(float(D)),
                                                                           op0=ALU.add, op1=ALU.mult), [MOD[l][s]], [A[l][s]])
                        P.op("dve", lambda e, A=A, g0=g0, s=s, l=l: e.tensor_tensor(out=A[l][s].t[:], in0=A[l][s].t[:], in1=vecA[l].t[:, g0:g0 + 16], op=ALU.mult), [A[l][s], vecA[l]], [A[l][s]])
            P.end_phase()

        def rstd_from_sq(srcs, nsq, Rb, sqr, eps_n):
            ps = psr.next()
            for i, (ap, b) in enumerate(srcs):
                sq = sqr.next()
                P.op("act", lambda e, ap=ap, sq=sq: e.activation(out=sq.t[:], in_=ap, func=AF.Square), [b], [sq])
                P.op("pe", lambda e, sq=sq, i=i: e.matmul(ps.t[:, :], onesB.t[:], sq.t[:], start=(i == 0), stop=(i == nsq - 1)), [onesB, sq], [ps])
            P.op("act", lambda e: e.activation(out=Rb.t[:], in_=ps.t[:], func=AF.Sqrt, bias=epsb(eps_n), scale=1.0), [ps, EPSB], [Rb])
            P.op("dve", lambda e: e.reciprocal(out=Rb.t[:], in_=Rb.t[:]), [Rb], [Rb])

        for l in range(DEPTH):
            last = (l == DEPTH - 1)
            if l == 0:
                own_blocks = list(range(NBLK))
            else:
                own_blocks = list(range(OWN // T)) + list(range(S_P // T, NBLK))
            own_set = set(own_blocks)

            with ExitStack() as sc:
                xin = Rot([P.tile(f"xin{i}", [128, D], F32, sc) for i in range(2)])
                XS = P.tile("XS", [128, KC, T], F32, sc)
                sqr = Rot([P.tile(f"sq{i}", [128, T], BF16, sc) for i in range(3)])
                Rb = P.tile("R", [128, T], F32, sc)
                tmpr = Rot([P.tile(f"tmp{i}", [128, T], F32, sc) for i in range(3)])
                hb = P.tile("h", [128, KC, T], BF16, sc)
                lat32 = P.tile("lat32", [128, 4, T], F32, sc)
                cqn = P.tile("cqn", [128, 4, T], BF16, sc)
                ckvn = P.tile("ckvn", [128, 4, T], BF16, sc)
                kr32 = P.tile("kr32", [64, 2, T], F32, sc)
                krb = P.tile("krb", [64, T], BF16, sc)
                cs = P.tile("cs", [64, 2, T], F32, sc)
                o1 = Rot([P.tile(f"o1{i}", [128, T], BF16, sc) for i in range(4)])
                gvst = P.tile("gvst", [128, 4, 256], BF16, sc)
                wp = Rot([P.tile(f"wp{i}", [128, KC, 512], BF16, sc) for i in range(3)])
                wuq = P.tile("wuq", [128, 4, 2048], BF16, sc)
                P.dma("pool", wuq.t[:, :, 0:1536], w_uq[l].rearrange("(k p) n -> p k n", p=128), [CONST], [wuq])
                P.dma("pool", wuq.t[:, :, 1536:2048], w_uqs[l].rearrange("(k p) n -> p k n", p=128), [CONST], [wuq])

                def wload(col0, ncol, extra=None):
                    wt = wp.next()
                    P.dma("pool", wt.t[:, :, 0:ncol], w_in[l][:, col0:col0 + ncol].rearrange("(k p) n -> p k n", p=128), [CONST], [wt])
                    if extra is not None:
                        P.dma("pool", wt.t[:, :, ncol:ncol + 64], extra.rearrange("(k p) n -> p k n", p=128), [CONST], [wt])
                    return wt

                def proj(wt, c0, m, ps_ap, ps):
                    for k in range(KC):
                        P.op("pe", lambda e, k=k: e.matmul(ps_ap, wt.t[:, k, c0:c0 + m], hb.t[:, k, :], start=(k == 0), stop=(k == KC - 1)), [wt, hb], [ps])

                for blk in range(NBLK):
                    seg = 0 if blk * T < S_P else 1
                    t0 = blk * T
                    need_q = blk in own_set
                    if blk > 0 and l == 0:
                        issue_conv(2)
                    if l == 0:
                        for s in range(4):
                            xt = xin.next()
                            P.dma("sp", xt.t[:], x_in[t0 + s * 128:t0 + (s + 1) * 128, :], [CONST], [xt])
                            for k4 in range(4):
                                ps = psr.next()
                                for kk in range(4):
                                    k = k4 * 4 + kk
                                    P.op("pe", lambda e, k=k, kk=kk, xt=xt, ps=ps: e.transpose(ps.t[:, kk * 128:(kk + 1) * 128], xt.t[:, k * 128:(k + 1) * 128], identF.t[:]), [xt, identF], [ps])
                                P.op("act" if k4 % 2 else "dve",
                                     lambda e, k4=k4, s=s, ps=ps: (e.copy if e is nc.scalar else e.tensor_copy)(out=XS.t[:, k4 * 4:(k4 + 1) * 4, s * 128:(s + 1) * 128],
                                                                                                       in_=ps.t[:, :].rearrange("p (k t) -> p k t", t=128)), [ps], [XS])
                        P.dma("sp", XT[blk], XS.t[:], [XS], [XTb[blk]])
                    else:
                        P.dma("sp", XS.t[:], XT[blk], [XTb[blk]], [XS])
                    P.dma("sp", cs.t[:], rope_in[:, :, t0:t0 + T], [CONST], [cs])
                    rstd_from_sq([(XS.t[:, k, :], XS) for k in range(KC)], KC, Rb, sqr, EPS * D)
                    for k in range(KC):
                        tm = tmpr.next()
                        P.op("dve", lambda e, k=k, tm=tm: e.tensor_tensor(out=tm.t[:], in0=XS.t[:, k, :], in1=Rb.t[:], op=ALU.mult), [XS, Rb], [tm])
                        P.op("act", lambda e, k=k, tm=tm: e.activation(out=hb.t[:, k, :], in_=tm.t[:], func=AF.Identity, bias=MOD[l][seg].t[:, k:k + 1], scale=A1[l][seg].t[:, k:k + 1]),
                             [tm, MOD[l][seg], A1[l][seg]], [hb])
                    for which in ((0, 1) if need_q else (1,)):
                        wt = wload(512 * which, 512)
                        for c in range(4):
                            ps = psr.next()
                            proj(wt, c * 128, 128, ps.t[:, :], ps)
                            P.op("act", lambda e, c=c, ps=ps: e.copy(out=lat32.t[:, c, :], in_=ps.t[:, :]), [ps], [lat32])
                        rstd_from_sq([(lat32.t[:, c, :], lat32) for c in range(4)], 4, Rb, sqr, EPS * 512)
                        dst = cqn if which == 0 else ckvn
                        go = 24 * l + 4 * which
                        for c in range(4):
                            tm = tmpr.next()
                            P.op("dve", lambda e, c=c, tm=tm: e.tensor_tensor(out=tm.t[:], in0=lat32.t[:, c, :], in1=Rb.t[:], op=ALU.mult), [lat32, Rb], [tm])
                            P.op("act", lambda e, c=c, tm=tm, dst=dst, go=go: e.activation(out=dst.t[:, c, :], in_=tm.t[:], func=AF.Identity, scale=GL.t[:, go + c:go + c + 1]), [tm, GL], [dst])
                    P.dma("sp", LAT[0:512, t0:t0 + T].rearrange("(c p) t -> p c t", p=128), ckvn.t[:], [ckvn], [LATb])
                    wt = wp.next()
                    P.dma("pool", wt.t[:, :, 0:64], w_in[l][:, 1024:1088].rearrange("(k p) n -> p k n", p=128), [CONST], [wt])
                    P.dma("pool", wt.t[:, :, 64:128], w_krs[l].rearrange("(k p) n -> p k n", p=128), [CONST], [wt])
                    P.dma("pool", wt.t[:, :, 128:512], w_in[l][:, 2112:2496].rearrange("(k p) n -> p k n", p=128), [CONST], [wt])
                    wt2 = wp.next()
                    P.dma("pool", wt2.t[:, :, 0:128], w_in[l][:, 2496:2624].rearrange("(k p) n -> p k n", p=128), [CONST], [wt2])
                    ps = psr.next()
                    proj(wt, 0, 64, ps.t[0:64, :], ps)
                    ps2 = psr.next()
                    proj(wt, 64, 64, ps2.t[0:64, :], ps2)
                    P.op("dve", lambda e, ps=ps: e.tensor_tensor(out=kr32.t[:, 0, :], in0=ps.t[0:64, :], in1=cs.t[:, 0, :], op=ALU.mult), [ps, cs], [kr32])
                    P.op("dve", lambda e, ps2=ps2: e.tensor_tensor(out=kr32.t[:, 1, :], in0=ps2.t[0:64, :], in1=cs.t[:, 1, :], op=ALU.mult), [ps2, cs], [kr32])
                    P.op("dve", lambda e: e.tensor_tensor(out=krb.t[:], in0=kr32.t[:, 0, :], in1=kr32.t[:, 1, :], op=ALU.add), [kr32], [krb])
                    P.dma("sp", LAT[512:576, t0:t0 + T], krb.t[:], [krb], [LATb])
                    for j in range(2):
                        ps = psr.next()
                        proj(wt, 128 + j * 128, 128, ps.t[:, :], ps)
                        ot = o1.next()
                        P.op("act", lambda e, ps=ps, ot=ot: e.copy(out=ot.t[:], in_=ps.t[:, :]), [ps], [ot])
                        P.dma("sp", GK[j, :, t0:t0 + T], ot.t[:], [ot], [GKb])
                    for s in range(4):
                        ps = psr.next()
                        for half, (wsrc, c0) in enumerate(((wt, 384), (wt2, 0))):
                            for k in range(KC):
                                P.op("pe", lambda e, k=k, s=s, half=half, wsrc=wsrc, c0=c0, ps=ps: e.matmul(ps.t[:, half * 128:(half + 1) * 128], hb.t[:, k, s * 128:(s + 1) * 128], wsrc.t[:, k, c0:c0 + 128],
                                                                                                start=(k == 0), stop=(k == KC - 1)), [hb, wsrc], [ps])
                        P.op("dve", lambda e, s=s, ps=ps: e.tensor_copy(out=gvst.t[:, s, :], in_=ps.t[:, 0:256]), [ps], [gvst])
                    P.dma("sp", GV[t0:t0 + T, :].rearrange("(s p) n -> p s n", p=128), gvst.t[:], [gvst], [GVb])
                    if not need_q:
                        continue
                    for half in range(2):
                        wt = wload(1088 + 512 * half, 512)
                        for j in range(4):
                            ps = psr.next()
                            proj(wt, j * 128, 128, ps.t[:, :], ps)
                            ot = o1.next()
                            P.op("act" if j % 2 else "dve", lambda e, ps=ps, ot=ot: (e.copy if e is nc.scalar else e.tensor_copy)(out=ot.t[:], in_=ps.t[:, :]), [ps], [ot])
                            P.dma("sp", GQ[half * 4 + j, :, t0:t0 + T], ot.t[:], [ot], [GQb])
                    for h in range(H):
                        ps = psr.next()
                        for c in range(4):
                            P.op("pe", lambda e, c=c, h=h, ps=ps: e.matmul(ps.t[:, :], wuq.t[:, c, h * 192:h * 192 + 128], cqn.t[:, c, :], start=(c == 0), stop=(c == 3)), [wuq, cqn], [ps])
                        ot = o1.next()
                        P.op("act", lambda e, ps=ps, ot=ot: e.copy(out=ot.t[:], in_=ps.t[:, :]), [ps], [ot])
                        P.dma("sp", QT[h, 0:128, t0:t0 + T], ot.t[:], [ot], [QTb])
                        ps = psr.next()
                        ps2 = psr.next()
                        for c in range(4):
                            P.op("pe", lambda e, c=c, h=h, ps=ps: e.matmul(ps.t[0:64, :], wuq.t[:, c, h * 192 + 128:h * 192 + 192], cqn.t[:, c, :], start=(c == 0), stop=(c == 3)), [wuq, cqn], [ps])
                        for c in range(4):
                            P.op("pe", lambda e, c=c, h=h, ps2=ps2: e.matmul(ps2.t[0:64, :], wuq.t[:, c, 1536 + h * 64:1536 + h * 64 + 64], cqn.t[:, c, :], start=(c == 0), stop=(c == 3)), [wuq, cqn], [ps2])
                        P.op("dve", lambda e, ps=ps: e.tensor_tensor(out=kr32.t[:, 0, :], in0=ps.t[0:64, :], in1=cs.t[:, 0, :], op=ALU.mult), [ps, cs], [kr32])
                        P.op("dve", lambda e, ps2=ps2: e.tensor_tensor(out=kr32.t[:, 1, :], in0=ps2.t[0:64, :], in1=cs.t[:, 1, :], op=ALU.mult), [ps2, cs], [kr32])
                        ot = o1.next()
                        P.op("dve", lambda e, ot=ot: e.tensor_tensor(out=ot.t[0:64, :], in0=kr32.t[:, 0, :], in1=kr32.t[:, 1, :], op=ALU.add), [kr32], [ot])
                        P.dma("sp", QT[h, 128:192, t0:t0 + T], ot.t[0:64, :], [ot], [QTb])
                P.end_phase()

            with ExitStack() as sc:
                wukv = P.tile("wukv", [128, 4, 2048], BF16, sc)
                P.dma("pool", wukv.t[:], w_ukv[l].rearrange("(k p) n -> p k n", p=128), [CONST], [wukv])
                SMAX = max(S_P, S_S)
                krT = P.tile("krT", [64, SMAX], BF16, sc)
                KT = P.tile("KT", [128, SMAX], BF16, sc)
                VV = P.tile("VV", [128, SMAX // 128, 128], BF16, sc)
                latr = Rot([P.tile(f"lat{i}", [128, 4, T], BF16, sc) for i in range(3)])
                qnr = Rot([P.tile(f"qn{i}", [128, T], BF16, sc) for i in range(2)])
                qrr = Rot([P.tile(f"qr{i}", [64, T], BF16, sc) for i in range(2)])
                pr = Rot([P.tile(f"p{i}", [128, T], BF16, sc) for i in range(3)])
                rdr = Rot([P.tile(f"rd{i}", [128, T], F32, sc) for i in range(2)])
                obr = Rot([P.tile(f"ob{i}", [128, T], BF16, sc) for i in range(2)])
                accr = Rot([P.tile(f"acc{i}", [128, T], F32, sc) for i in range(6)])
                pr = Rot([P.tile(f"pp{i}", [128, T], BF16, sc) for i in range(5)])
                psS = Rot(PS[0:4])
                psO = Rot(PS[4:6])
                psD = Rot(PS[6:7])
                psK = Rot(PS[6:8])
                for (sname, s0, slen) in segs:
                    nq = (slen if (l == 0 or sname == "S") else OWN) // T
                    nkc = slen // 128
                    P.dma("sp", krT.t[:, 0:slen], LAT[512:576, s0:s0 + slen], [LATb], [krT])
                    for h in range(H):
                        for tc_ in range(slen // T):
                            lt = latr.next()
                            P.dma("sp", lt.t[:], LAT[0:512, s0 + tc_ * T:s0 + (tc_ + 1) * T].rearrange("(c p) t -> p c t", p=128), [LATb], [lt])
                            ps = psK.next()
                            for c in range(4):
                                P.op("pe", lambda e, c=c, h=h, lt=lt, ps=ps: e.matmul(ps.t[:, :], wukv.t[:, c, h * 256:h * 256 + 128], lt.t[:, c, :], start=(c == 0), stop=(c == 3)), [wukv, lt], [ps])
                            P.op("act", lambda e, ps=ps, tc_=tc_: e.copy(out=KT.t[:, tc_ * T:(tc_ + 1) * T], in_=ps.t[:, :]), [ps], [KT])
                            ps = psK.next()
                            for s in range(4):
                                for c in range(4):
                                    P.op("pe", lambda e, c=c, s=s, h=h, lt=lt, ps=ps: e.matmul(ps.t[:, s * 128:(s + 1) * 128], lt.t[:, c, s * 128:(s + 1) * 128], wukv.t[:, c, h * 256 + 128:h * 256 + 256],
                                                                                      start=(c == 0), stop=(c == 3)), [lt, wukv], [ps])
                            P.op("dve", lambda e, ps=ps, tc_=tc_: e.tensor_copy(out=VV.t[:, tc_ * 4:(tc_ + 1) * 4, :], in_=ps.t[:, :].rearrange("p (s d) -> p s d", d=128)), [ps], [VV])
                        for qb in range(nq):
                            q0 = s0 + qb * T
                            qn = qnr.next()
                            qr = qrr.next()
                            P.dma("sp", qn.t[:], QT[h, 0:128, q0:q0 + T], [QTb], [qn])
                            P.dma("sp", qr.t[:], QT[h, 128:192, q0:q0 + T], [QTb], [qr])
                            po = psO.next()
                            pd = psD.next()
                            accs = (accr.next(), accr.next(), accr.next())

                            def qk(kc, qn=qn, qr=qr):
                                ps = psS.next()
                                P.op("pe", lambda e: e.matmul(ps.t[:, :], KT.t[:, kc * 128:(kc + 1) * 128], qn.t[:], start=True, stop=False), [KT, qn], [ps])
                                P.op("pe", lambda e: e.matmul(ps.t[:, :], krT.t[:, kc * 128:(kc + 1) * 128], qr.t[:], start=False, stop=True), [krT, qr], [ps])
                                return ps
                            pending = [qk(0), qk(1)]
                            for kc in range(nkc):
                                if kc + 2 < nkc:
                                    pending.append(qk(kc + 2))
                                pscur = pending.pop(0)
                                pt = pr.next()
                                P.op("act", lambda e, pscur=pscur, pt=pt: e.activation(out=pt.t[:], in_=pscur.t[:, :], func=AF.Exp, scale=MLA_SCALE), [pscur], [pt])
                                P.op("pe", lambda e, kc=kc, pt=pt: e.matmul(po.t[:, :], VV.t[:, kc, :], pt.t[:], start=(kc == 0), stop=(kc == nkc - 1)), [VV, pt], [po])
                                acc = accs[kc % 3]
                                aeng = "dve"
                                if kc < 3:
                                    P.op(aeng, lambda e, pt=pt, acc=acc: e.tensor_copy(out=acc.t[:], in_=pt.t[:]), [pt], [acc])
                                else:
                                    P.op(aeng, lambda e, pt=pt, acc=acc: e.tensor_tensor(out=acc.t[:], in0=acc.t[:], in1=pt.t[:], op=ALU.add), [pt, acc], [acc])
                            for ai in range(3):
                                P.op("pe", lambda e, pd=pd, ai=ai: e.matmul(pd.t[:, :], onesF.t[:], accs[ai].t[:], start=(ai == 0), stop=(ai == 2)), [onesF, accs[ai]], [pd])
                            rd = rdr.next()
                            ob = obr.next()
                            P.op("dve", lambda e, rd=rd, pd=pd: e.reciprocal(out=rd.t[:], in_=pd.t[:, :]), [pd], [rd])
                            P.op("dve", lambda e, rd=rd, ob=ob, po=po: e.tensor_tensor(out=ob.t[:], in0=po.t[:, :], in1=rd.t[:], op=ALU.mult), [po, rd], [ob])
                            P.dma("pool", OA[h, :, q0:q0 + T], ob.t[:], [ob], [OAb])
                            issue_conv(2)
                issue_conv(len(conv))
                P.end_phase()

            with ExitStack() as sc:
                SMAX = max(S_P, S_S)
                gkT = P.tile("gkT", [128, SMAX], BF16, sc)
                gvv = P.tile("gvv", [128, SMAX // 128, 128], BF16, sc)
                gqr = Rot([P.tile(f"gq{i}", [128, 4, T], BF16, sc) for i in range(2)])
                tmr = Rot([P.tile(f"wt{i}", [128, 4, 128], F32, sc) for i in range(4)])
                pr = Rot([P.tile(f"wp{i}", [128, 4, 128], BF16, sc) for i in range(6)])
                dnr = Rot([P.tile(f"dn{i}", [128, 4, 128], F32, sc) for i in range(2)])
                oor = Rot([P.tile(f"oo{i}", [128, 4, T], BF16, sc) for i in range(2)])
                psS = Rot(PS[0:3])
                psO = Rot(PS[3:5])
                psD = Rot(PS[5:7])
                for (sname, s0, slen) in segs:
                    nb = slen // 128
                    nqb = (slen if (l == 0 or sname == "S") else OWN) // 128
                    b0 = s0 // 128
                    for kv in range(2):
                        P.dma("sp", gkT.t[:, 0:slen], GK[kv, :, s0:s0 + slen], [GKb], [gkT])
                        P.dma("sp", gvv.t[:, 0:nb, :], GV[s0:s0 + slen, kv * 128:(kv + 1) * 128].rearrange("(n p) d -> p n d", p=128), [GVb], [gvv])
                        for n in range(nqb):
                            n4 = n % 4
                            if n4 == 0:
                                gq = gqr.next()
                                for g in range(4):
                                    P.dma("sp", gq.t[:, g, :], GQ[kv * 4 + g, :, s0 + n * 128:s0 + n * 128 + T], [GQb], [gq])
                                oo = oor.next()
                            po = psO.next()
                            pd = psD.next()
                            pts = []
                            for jb in range(3):
                                m = (n + jb - 1) % nb
                                ps = psS.next()
                                P.op("pe", lambda e, m=m, n4=n4, gq=gq, ps=ps: e.matmul(ps.t[:, :].rearrange("p (g q) -> p g q", q=128), gkT.t[:, m * 128:(m + 1) * 128], gq.t[:, :, n4 * 128:(n4 + 1) * 128],
                                                                              start=True, stop=True), [gkT, gq], [ps])
                                tm = tmr.next()
                                P.op("dve", lambda e, ps=ps, tm=tm, jb=jb: e.scalar_tensor_tensor(out=tm.t[:], in0=ps.t[:, :].rearrange("p (g q) -> p g q", q=128), scalar=GQA_SCALE, in1=BT[kv][jb].t[:],
                                                                                               op0=ALU.mult, op1=ALU.add), [ps, BT[kv][jb]], [tm])
                                pt = pr.next()
                                if jb == 1:
                                    P.op("act", lambda e, tm=tm, pt=pt: e.activation(out=pt.t[:], in_=tm.t[:], func=AF.Exp), [tm], [pt])
                                else:
                                    col = 2 * (b0 + n) + (0 if jb == 0 else 1)
                                    P.op("act", lambda e, tm=tm, pt=pt, col=col: e.activation(out=pt.t[:], in_=tm.t[:], func=AF.Exp, bias=EM.t[:, col:col + 1]), [tm, EM], [pt])
                                pts.append((m, pt))
                            for jb, (m, pt) in enumerate(pts):
                                P.op("pe", lambda e, m=m, pt=pt, jb=jb, po=po: e.matmul(po.t[:, :].rearrange("p (g q) -> p g q", q=128), gvv.t[:, m, :], pt.t[:], start=(jb == 0), stop=(jb == 2)), [gvv, pt], [po])
                                P.op("pe", lambda e, pt=pt, jb=jb, pd=pd: e.matmul(pd.t[:, :].rearrange("p (g q) -> p g q", q=128), onesB.t[:], pt.t[:], start=(jb == 0), stop=(jb == 2)), [onesB, pt], [pd])
                            dn = dnr.next()
                            for g in range(4):
                                P.op("dve", lambda e, g=g, dn=dn, pd=pd: e.tensor_scalar_add(out=dn.t[:, g, :], in0=pd.t[:, g * 128:(g + 1) * 128], scalar1=ESK[l].t[:, kv * 4 + g:kv * 4 + g + 1]), [pd, ESK[l]], [dn])
                            P.op("dve", lambda e, dn=dn: e.reciprocal(out=dn.t[:], in_=dn.t[:]), [dn], [dn])
                            P.op("dve", lambda e, dn=dn, po=po, oo=oo, n4=n4: e.tensor_tensor(out=oo.t[:, :, n4 * 128:(n4 + 1) * 128], in0=po.t[:, :].rearrange("p (g q) -> p g q", q=128), in1=dn.t[:], op=ALU.mult),
                                 [po, dn], [oo])
                            if n4 == 3:
                                for g in range(4):
                                    P.dma("pool", OB[kv * 4 + g, :, s0 + (n - 3) * 128:s0 + (n - 3) * 128 + T], oo.t[:, g, :], [oo], [OBb])
                P.end_phase()

            with ExitStack() as sc:
                XS = P.tile("XS4", [128, KC, T], F32, sc)
                sqr = Rot([P.tile(f"sq{i}", [128, T], BF16, sc) for i in range(3)])
                Rb = P.tile("R4", [128, T], F32, sc)
                tmpr = Rot([P.tile(f"tmp{i}", [128, T], F32, sc) for i in range(3)])
                hb = P.tile("h4", [128, KC, T], BF16, sc)
                act = P.tile("act", [128, max(FC, 16), T], BF16, sc)
                wgu = Rot([P.tile(f"wgu{i}", [128, KC, 256], BF16, sc) for i in range(4)])
                wdr = Rot([P.tile(f"wd{i}", [128, FC, 128], BF16, sc) for i in range(3)])
                moe = (l % 2 == 1)
                if moe:
                    gbt = P.tile("gb", [128, NE, T], BF16, sc)
                    h32r = Rot([P.tile(f"h32{i}", [128, T], F32, sc) for i in range(2)])
                    wr32 = P.tile("wr32", [128, KC, NE], F32, sc)
                    P.dma("sp", wr32.t[:], w_router[0].rearrange("(k p) n -> p k n", p=128), [CONST], [wr32])
                    lg = P.tile("lg", [128, 4, NE], F32, sc)
                    sm = P.tile("sm", [128, 16], F32, sc)
                    l2 = P.tile("l2", [128, 4, NE], F32, sc)
                    gts = P.tile("gts", [128, 4, NE], F32, sc)
                    gT = P.tile("gT", [8, T], F32, sc)
                OAt = act.t[:, 0:8, :]
                OBt = act.t[:, 8:16, :]

                for bi, blk in enumerate(own_blocks):
                    seg = 0 if blk * T < S_P else 1
                    t0 = blk * T
                    P.dma("sp", XS.t[:], XT[blk], [XTb[blk]], [XS])
                    P.dma("sp", OAt, OA[:, :, t0:t0 + T].rearrange("h p t -> p h t"), [OAb], [act])
                    P.dma("sp", OBt, OB[:, :, t0:t0 + T].rearrange("h p t -> p h t"), [OBb], [act])
                    for grp in range(2):
                        src = OAt if grp == 0 else OBt
                        ps = psr.next()
                        for c in range(8):
                            sq = sqr.next()
                            P.op("act", lambda e, c=c, sq=sq, src=src: e.activation(out=sq.t[:], in_=src[:, c, :], func=AF.Square), [act], [sq])
                            P.op("pe", lambda e, c=c, sq=sq, ps=ps: e.matmul(ps.t[:, :], onesB.t[:], sq.t[:], start=(c == 0), stop=(c == 7)), [onesB, sq], [ps])
                        P.op("act", lambda e, ps=ps: e.activation(out=Rb.t[:], in_=ps.t[:, :], func=AF.Sqrt, bias=epsb(EPS * 1024), scale=1.0), [ps, EPSB], [Rb])
                        P.op("dve", lambda e: e.reciprocal(out=Rb.t[:], in_=Rb.t[:]), [Rb], [Rb])
                        go = 24 * l + 8 + 8 * grp
                        for c in range(8):
                            tm = tmpr.next()
                            P.op("dve", lambda e, c=c, tm=tm, src=src: e.tensor_tensor(out=tm.t[:], in0=src[:, c, :], in1=Rb.t[:], op=ALU.mult), [act, Rb], [tm])
                            P.op("act", lambda e, c=c, tm=tm, go=go, grp=grp: e.activation(out=hb.t[:, grp * 8 + c, :], in_=tm.t[:], func=AF.Identity, scale=GL.t[:, go + c:go + c + 1]), [tm, GL], [hb])
                    for dg in range(8):
                        wt = wgu.next()
                        P.dma("pool", wt.t[:], w_out[l][:, dg * 256:(dg + 1) * 256].rearrange("(k p) n -> p k n", p=128), [CONST], [wt])
                        for dd in range(2):
                            dc = dg * 2 + dd
                            ps = psr.next()
                            for k in range(KC):
                                P.op("pe", lambda e, k=k, dd=dd, wt=wt, ps=ps: e.matmul(ps.t[:, :], wt.t[:, k, dd * 128:(dd + 1) * 128], hb.t[:, k, :], start=(k == 0), stop=(k == KC - 1)), [wt, hb], [ps])
                            P.op("dve", lambda e, dc=dc, ps=ps, seg=seg: e.scalar_tensor_tensor(out=XS.t[:, dc, :], in0=ps.t[:, :], scalar=MOD[l][seg].t[:, 32 + dc:33 + dc], in1=XS.t[:, dc, :],
                                                                                     op0=ALU.mult, op1=ALU.add), [ps, MOD[l][seg], XS], [XS])
                    rstd_from_sq([(XS.t[:, k, :], XS) for k in range(KC)], KC, Rb, sqr, EPS * D)
                    if moe:
                        psl = psr.next()
                    for k in range(KC):
                        tm = tmpr.next()
                        P.op("dve", lambda e, k=k, tm=tm: e.tensor_tensor(out=tm.t[:], in0=XS.t[:, k, :], in1=Rb.t[:], op=ALU.mult), [XS, Rb], [tm])
                        if not moe:
                            P.op("act", lambda e, k=k, tm=tm, seg=seg: e.activation(out=hb.t[:, k, :], in_=tm.t[:], func=AF.Identity, bias=MOD[l][seg].t[:, 48 + k:49 + k], scale=A2[l][seg].t[:, k:k + 1]),
                                 [tm, MOD[l][seg], A2[l][seg]], [hb])
                        else:
                            h32 = h32r.next()
                            P.op("act", lambda e, k=k, tm=tm, seg=seg, h32=h32: e.activation(out=h32.t[:], in_=tm.t[:], func=AF.Identity, bias=MOD[l][seg].t[:, 48 + k:49 + k], scale=A2[l][seg].t[:, k:k + 1]),
                                 [tm, MOD[l][seg], A2[l][seg]], [h32])
                            P.op("dve", lambda e, k=k, h32=h32: e.tensor_copy(out=hb.t[:, k, :], in_=h32.t[:]), [h32], [hb])
                            P.op("pe", lambda e, k=k, h32=h32: e.matmul(psl.t[0:NE, :], wr32.t[:, k, :], h32.t[:], start=(k == 0), stop=(k == KC - 1)), [h32, wr32], [psl])
                    if moe:
                        P.op("dve", lambda e: e.tensor_copy(out=gT.t[:], in_=psl.t[0:NE, :]), [psl], [gT])
                        ps = psr.next()
                        for s in range(4):
                            P.op("pe", lambda e, s=s, ps=ps: e.transpose(ps.t[:, s * NE:(s + 1) * NE], gT.t[0:NE, s * 128:(s + 1) * 128], identF.t[0:NE, 0:NE]), [gT, identF], [ps])
                        P.op("dve", lambda e, ps=ps: e.tensor_copy(out=lg.t[:], in_=ps.t[:, 0:4 * NE].rearrange("p (s n) -> p s n", n=NE)), [ps], [lg])
                        for s in range(4):
                            P.op("dve", lambda e, s=s: e.tensor_reduce(out=sm.t[:, s:s + 1], in_=lg.t[:, s, :], axis=AX.X, op=ALU.max), [lg], [sm])
                            P.op("dve", lambda e, s=s: e.tensor_scalar(out=l2.t[:, s, :], in0=lg.t[:, s, :], scalar1=sm.t[:, s:s + 1], scalar2=-1.0e30, op0=ALU.is_equal, op1=ALU.mult), [lg, sm], [l2])
                            P.op("dve", lambda e, s=s: e.tensor_tensor(out=l2.t[:, s, :], in0=l2.t[:, s, :], in1=lg.t[:, s, :], op=ALU.add), [l2, lg], [l2])
                            P.op("dve", lambda e, s=s: e.tensor_reduce(out=sm.t[:, 4 + s:5 + s], in_=l2.t[:, s, :], axis=AX.X, op=ALU.max), [l2], [sm])
                            P.op("dve", lambda e, s=s: e.tensor_scalar_mul(out=sm.t[:, 8 + s:9 + s], in0=sm.t[:, s:s + 1], scalar1=-1.0), [sm], [sm])
                            P.op("act", lambda e, s=s: e.activation(out=gts.t[:, s, :], in_=lg.t[:, s, :], func=AF.Exp, bias=sm.t[:, 8 + s:9 + s]), [lg, sm], [gts])
                            P.op("dve", lambda e, s=s: e.scalar_tensor_tensor(out=gts.t[:, s, :], in0=lg.t[:, s, :], scalar=sm.t[:, 4 + s:5 + s], in1=gts.t[:, s, :], op0=ALU.is_ge, op1=ALU.mult), [lg, sm, gts], [gts])
                            P.op("dve", lambda e, s=s: e.tensor_reduce(out=sm.t[:, 12 + s:13 + s], in_=gts.t[:, s, :], axis=AX.X, op=ALU.add), [gts], [sm])
                            P.op("dve", lambda e, s=s: e.reciprocal(out=sm.t[:, 12 + s:13 + s], in_=sm.t[:, 12 + s:13 + s]), [sm], [sm])
                            P.op("dve", lambda e, s=s: e.tensor_scalar_mul(out=gts.t[:, s, :], in0=gts.t[:, s, :], scalar1=sm.t[:, 12 + s:13 + s]), [gts, sm], [gts])
                        ps = psr.next()
                        for s in range(4):
                            P.op("pe", lambda e, s=s, ps=ps: e.transpose(ps.t[0:NE, s * 128:(s + 1) * 128], gts.t[:, s, :], identF.t[:]), [gts, identF], [ps])
                        P.op("dve", lambda e, ps=ps: e.tensor_copy(out=gT.t[:], in_=ps.t[0:NE, :]), [ps], [gT])
                        for e_ in range(NE):
                            ps = psr.next()
                            P.op("pe", lambda e, e_=e_, ps=ps: e.matmul(ps.t[:, :], SEL.t[0:NE, e_, :], gT.t[:], start=True, stop=True), [SEL, gT], [ps])
                            P.op("act", lambda e, e_=e_, ps=ps: e.copy(out=gbt.t[:, e_, :], in_=ps.t[:, :]), [ps], [gbt])
                    for ex in range(NE if moe else 1):
                        exi = (1 + ex) if moe else 0
                        wsb = WSe if moe else WSd
                        for fg in range(FC // 2):
                            wg_t = wgu.next()
                            wu_t = wgu.next()
                            P.dma("sp", wg_t.t[:], WGS[exi, fg].rearrange("p (k n) -> p k n", n=256), [wsb], [wg_t])
                            P.dma("sp", wu_t.t[:], WUS[exi, fg].rearrange("p (k n) -> p k n", n=256), [wsb], [wu_t])
                            for ff in range(2):
                                fc = fg * 2 + ff
                                pg = psr.next()
                                pu = psr.next()
                                for k in range(KC):
                                    P.op("pe", lambda e, k=k, ff=ff, wg_t=wg_t, pg=pg: e.matmul(pg.t[:, :], wg_t.t[:, k, ff * 128:(ff + 1) * 128], hb.t[:, k, :], start=(k == 0), stop=(k == KC - 1)), [wg_t, hb], [pg])
                                for k in range(KC):
                                    P.op("pe", lambda e, k=k, ff=ff, wu_t=wu_t, pu=pu: e.matmul(pu.t[:, :], wu_t.t[:, k, ff * 128:(ff + 1) * 128], hb.t[:, k, :], start=(k == 0), stop=(k == KC - 1)), [wu_t, hb], [pu])
                                tm = tmpr.next()
                                P.op("act", lambda e, pg=pg, tm=tm: e.activation(out=tm.t[:], in_=pg.t[:, :], func=AF.Silu), [pg], [tm])
                                if moe:
                                    tm2 = tmpr.next()
                                    P.op("dve", lambda e, pu=pu, tm=tm, tm2=tm2: e.tensor_tensor(out=tm2.t[:], in0=pu.t[:, :], in1=tm.t[:], op=ALU.mult), [pu, tm], [tm2])
                                    P.op("dve", lambda e, fc=fc, tm2=tm2, ex=ex: e.tensor_tensor(out=act.t[:, fc, :], in0=tm2.t[:], in1=gbt.t[:, ex, :], op=ALU.mult), [tm2, gbt], [act])
                                else:
                                    P.op("dve", lambda e, fc=fc, pu=pu, tm=tm: e.tensor_tensor(out=act.t[:, fc, :], in0=pu.t[:, :], in1=tm.t[:], op=ALU.mult), [pu, tm], [act])
                        for dc in range(KC):
                            wd_t = wdr.next()
                            P.dma("sp", wd_t.t[:], WDS[exi, dc].rearrange("p (f n) -> p f n", n=128), [wsb], [wd_t])
                            ps = psr.next()
                            for fc in range(FC):
                                P.op("pe", lambda e, fc=fc, wd_t=wd_t, ps=ps: e.matmul(ps.t[:, :], wd_t.t[:, fc, :], act.t[:, fc, :], start=(fc == 0), stop=(fc == FC - 1)), [wd_t, act], [ps])
                            P.op("dve", lambda e, dc=dc, ps=ps, seg=seg: e.scalar_tensor_tensor(out=XS.t[:, dc, :], in0=ps.t[:, :], scalar=MOD[l][seg].t[:, 80 + dc:81 + dc], in1=XS.t[:, dc, :],
                                                                                     op0=ALU.mult, op1=ALU.add), [ps, MOD[l][seg], XS], [XS])
                    if not last:
                        P.dma("sp", XT[blk], XS.t[:], [XS], [XTb[blk]])
                    elif seg == 1 or blk * T < OWN:
                        rstd_from_sq([(XS.t[:, k, :], XS) for k in range(KC)], KC, Rb, sqr, EPS * D)
                        go = 24 * DEPTH
                        for k in range(KC):
                            tm = tmpr.next()
                            P.op("dve", lambda e, k=k, tm=tm: e.tensor_tensor(out=tm.t[:], in0=XS.t[:, k, :], in1=Rb.t[:], op=ALU.mult), [XS, Rb], [tm])
                            P.op("act", lambda e, k=k, tm=tm, go=go: e.activation(out=XS.t[:, k, :], in_=tm.t[:], func=AF.Identity, scale=GL.t[:, go + k:go + k + 1]), [tm, GL, XS], [XS])
                        yo = act.t[:, 0:16, :].rearrange("p a b -> p (a b)").bitcast(F32)
                        orow = (blk * T) if seg == 0 else (OWN + blk * T - S_P)
                        for s in range(4):
                            half = s % 2
                            for k4 in range(4):
                                ps = psr.next()
                                for kk in range(4):
                                    k = k4 * 4 + kk
                                    P.op("pe", lambda e, k=k, kk=kk, s=s, ps=ps: e.transpose(ps.t[:, kk * 128:(kk + 1) * 128], XS.t[:, k, s * 128:(s + 1) * 128], identF.t[:]), [XS, identF], [ps])
                                P.op("act" if k4 % 2 else "dve", lambda e, k4=k4, half=half, ps=ps: (e.copy if e is nc.scalar else e.tensor_copy)(out=yo[:, half * 2048 + k4 * 512:half * 2048 + (k4 + 1) * 512], in_=ps.t[:, :]), [ps], [act])
                            P.dma("sp", y_out[orow + s * 128:orow + (s + 1) * 128, :], yo[:, half * 2048:(half + 1) * 2048], [act], [Yb])
                P.end_phase()

        P._wait_all(P.E["sp"], {Yb.dsemname: Yb.dcnt})
    return nc


def _host_tables(S_P, S_S, shift):
    import jax
    import jax.numpy as jnp
    cpu = jax.devices("cpu")[0]
    with jax.default_device(cpu):
        def rope(S):
            pos = jnp.arange(S, dtype=jnp.float32)
            inv = 1.0 / (10000.0 ** (jnp.arange(0, 64, 2, dtype=jnp.float32) / 64))
            ang = pos[:, None] * inv[None, :]
            return np.asarray(jnp.cos(ang)), np.asarray(jnp.sin(ang))
        cP, sP = rope(S_P)
        cS, sS = rope(S_S)
        rel = jnp.arange(-255, 256, dtype=jnp.int32)
        nb = 16
        ret = (rel > 0).astype(jnp.int32) * nb
        n = jnp.abs(rel)
        max_exact = nb // 2
        nf = jnp.maximum(n, 1).astype(jnp.float32)
        large = max_exact + (jnp.log(nf / max_exact) / math.log(128 / max_exact) * (nb - max_exact)).astype(jnp.int32)
        large = jnp.minimum(large, nb - 1)
        bucket = np.asarray(ret + jnp.where(n < max_exact, n, large))
    relv = np.arange(-255, 256)
    idx = (np.arange(S_P) + shift) % S_P
    cos = np.concatenate([cP[idx], cS], 0).T
    sin = np.concatenate([sP[idx], sS], 0).T
    rope_t = np.zeros((64, 2, S_P + S_S), np.float32)
    rope_t[0:32, 0] = cos
    rope_t[32:64, 0] = cos
    rope_t[0:32, 1] = -sin
    rope_t[32:64, 1] = sin
    nbP, nbS = S_P // 128, S_S // 128
    em = np.zeros((128, 2 * (nbP + nbS)), np.float32)
    for b in range(nbP):
        gb = (b + shift // 128) % nbP
        if gb == 0:
            em[:, 2 * b] = NEG
        if gb == nbP - 1:
            em[:, 2 * b + 1] = NEG
    em[:, 2 * nbP] = NEG
    em[:, 2 * (nbP + nbS) - 1] = NEG
    oh = np.zeros((33, 512), np.float32)
    for kp in range(511):
        r = 255 - kp
        if abs(r) <= 128:
            oh[bucket[r + 255], kp] = 1.0
        else:
            oh[32, kp] = 1.0
    oh[32, 511] = 1.0
    return rope_t, em, oh


_PERM = np.concatenate([np.arange(32, 64), np.arange(0, 32)])


def make_in_maps(inputs, cfg, n_cores=8):
    S_P, S_S, OWN = cfg["S_P"], cfg["S_S"], cfg["OWN"]
    f = lambda a: np.ascontiguousarray(np.asarray(a, dtype=np.float32))
    shared = {k: f(inputs[k]) for k in ("rel_bias", "w_ada", "b_ada", "g_norm_mix", "g_norm_ffn", "w_in", "g_q_lat", "w_uq", "g_kv_lat", "w_ukv",
                                         "sink", "g_out_a", "g_out_b", "w_out", "w_gate_d", "w_up_d", "w_down_d", "w_router", "w_gate_e", "w_up_e",
                                         "w_down_e", "g_final")}
    w_in = shared["w_in"]
    shared["w_krs"] = np.ascontiguousarray(w_in[:, :, 1024:1088][:, :, _PERM])
    wq = shared["w_uq"].reshape(w_in.shape[0], 512, 8, 192)
    shared["w_uqs"] = np.ascontiguousarray(wq[:, :, :, 128:192][:, :, :, _PERM].reshape(w_in.shape[0], 512, 512))
    shared["ident_in"] = np.eye(128, dtype=np.float32)
    xp, xs = f(inputs["x_prompt"]), f(inputs["x_sample"])
    cp, csmp = f(inputs["c_prompt"]), f(inputs["c_sample"])
    per_group = n_cores // xp.shape[0]
    maps = []
    for c in range(n_cores):
        b = c // per_group
        r = c % per_group
        shift = r * OWN
        idx = (np.arange(S_P) + shift) % S_P
        rope_t, em, oh = _host_tables(S_P, S_S, shift)
        m = dict(shared)
        m["x_in"] = np.ascontiguousarray(np.concatenate([xp[b][idx], xs[c]], 0))
        m["c_in"] = np.ascontiguousarray(np.stack([cp[b], csmp[c]], 0))
        m["rope_in"] = rope_t
        m["em_in"] = em
        m["oh_in"] = oh
        maps.append(m)
    return maps


CFG_FULL = dict(S_P=16384, S_S=2048, OWN=4096, FF=5632, NE=8, DEPTH=2)


def kernel(**inputs):
    return run(inputs, CFG_FULL)


def run(inputs, cfg):
    nc = build(cfg)
    maps = make_in_maps(inputs, cfg)
    res = run_bass_kernel_spmd(nc, maps, core_ids=list(range(8)))
    S_P, S_S, OWN = cfg["S_P"], cfg["S_S"], cfg["OWN"]
    yp = np.zeros((2, S_P, D), np.float32)
    ys = np.zeros((8, S_S, D), np.float32)
    for c in range(8):
        y = np.asarray(res.results[c]["y_out"])
        b, r = c // 4, c % 4
        yp[b, r * OWN:(r + 1) * OWN] = y[0:OWN]
        ys[c] = y[OWN:OWN + S_S]
    return (yp, ys)
```
